# Optimizing a Trainium2 kernel written in Bass

```python
import jax, jax.numpy as jnp
from jax import lax
import numpy as np

D_MODEL = 2048
BATCH = 4
SEQ = 2048
DEPTH = 1

CHUNK = 64
Q_BLOCK = 128
SB_HEADS = 8
HEAD_DIM = 128
SB_WIDTH = SB_HEADS * HEAD_DIM
MLA_HEADS = 8
MLA_Q_RANK = 512
MLA_KV_RANK = 256
MLA_NOPE = 128
MLA_ROPE = 64
MLA_V = 128
MLA_WIDTH = MLA_HEADS * MLA_V
MIX_WIDTH = SB_WIDTH + MLA_WIDTH
IN_PROJ = 3 * SB_WIDTH + MLA_Q_RANK + MLA_KV_RANK + MLA_ROPE
ROPE_THETA = 10000.0
N_EXPERTS = 32
TOP_K = 4
D_FF = 2048
SWIGLU_LIMIT = 7.0
SWIGLU_ALPHA = 1.702
MOE_BLOCK = 128
LN_EPS = 1e-5
RMS_EPS = 1e-6
DEEPNORM_ALPHA = (2 * DEPTH) ** 0.25
DEEPNORM_BETA = (8 * DEPTH) ** -0.25

kernel_name = 'hybrid_sb_mla_moe_deepnorm'


def layer_norm(x, g, b):
    xf = x.astype(jnp.float32)
    mu = jnp.mean(xf, -1, keepdims=True)
    xc = xf - mu
    var = jnp.mean(xc * xc, -1, keepdims=True)
    return (xc * lax.rsqrt(var + LN_EPS) * g.astype(jnp.float32) + b.astype(jnp.float32)).astype(x.dtype)


def rms_norm(x, g):
    xf = x.astype(jnp.float32)
    ms = jnp.mean(xf * xf, -1, keepdims=True)
    return (xf * lax.rsqrt(ms + RMS_EPS) * g.astype(jnp.float32)).astype(x.dtype)


def rope(x, positions):
    half = x.shape[-1] // 2
    inv_freq = ROPE_THETA ** (-(jnp.arange(half, dtype=jnp.float32) * 2.0 / x.shape[-1]))
    ang = positions[..., None].astype(jnp.float32) * inv_freq
    cos = jnp.cos(ang)[:, :, None, :]
    sin = jnp.sin(ang)[:, :, None, :]
    xf = x.astype(jnp.float32)
    x1, x2 = xf[..., :half], xf[..., half:]
    return jnp.concatenate([x1 * cos - x2 * sin, x2 * cos + x1 * sin], -1).astype(x.dtype)


def stick_breaking_attention(q, k, v):
    S = q.shape[1]
    scale = q.shape[-1] ** -0.5
    outs = []
    for qb in range(S // Q_BLOCK):
        q0 = qb * Q_BLOCK
        kend = q0 + Q_BLOCK
        z = jnp.einsum('bqhd,bkhd->bhqk', q[:, q0:kend], k[:, :kend],
                       preferred_element_type=jnp.float32) * scale
        t_idx = (q0 + jnp.arange(Q_BLOCK))[:, None]
        s_idx = jnp.arange(kend)[None, :]
        past = s_idx < t_idx
        neg_log_keep = jnp.where(past, jax.nn.softplus(z), 0.0)
        between = lax.cumsum(neg_log_keep, axis=3, reverse=True) - neg_log_keep
        w = jnp.where(past, jnp.exp(jax.nn.log_sigmoid(z) - between), 0.0)
        outs.append(jnp.einsum('bhqk,bkhd->bqhd', w.astype(v.dtype), v[:, :kend]))
    return jnp.concatenate(outs, axis=1)


def chunk_causal_softmax_attention(q, k, v):
    S = q.shape[1]
    scale = q.shape[-1] ** -0.5
    outs = []
    for qb in range(S // Q_BLOCK):
        q0 = qb * Q_BLOCK
        kend = q0 + Q_BLOCK
        s = jnp.einsum('bqhd,bkhd->bhqk', q[:, q0:kend], k[:, :kend],
                       preferred_element_type=jnp.float32) * scale
        t_chunk = (q0 + jnp.arange(Q_BLOCK))[:, None] // CHUNK
        s_chunk = jnp.arange(kend)[None, :] // CHUNK
        s = jnp.where(s_chunk <= t_chunk, s, -jnp.inf)
        p = jax.nn.softmax(s, axis=-1)
        outs.append(jnp.einsum('bhqk,bkhd->bqhd', p.astype(v.dtype), v[:, :kend]))
    return jnp.concatenate(outs, axis=1)


def hybrid_mixer(h, positions, w_in, q_a_norm, w_q_b, kv_a_norm, w_kv_b, sb_out_norm, mla_out_norm, w_o):
    B, S, _ = h.shape
    u = h @ w_in
    splits = [SB_WIDTH, 2 * SB_WIDTH, 3 * SB_WIDTH,
              3 * SB_WIDTH + MLA_Q_RANK, 3 * SB_WIDTH + MLA_Q_RANK + MLA_KV_RANK]
    sb_q, sb_k, sb_v, q_lat, kv_lat, k_rope = jnp.split(u, splits, axis=-1)
    hs = (B, S, SB_HEADS, HEAD_DIM)
    sb_o = stick_breaking_attention(sb_q.reshape(hs), sb_k.reshape(hs), sb_v.reshape(hs))
    sb_o = rms_norm(sb_o.reshape(B, S, SB_WIDTH), sb_out_norm)
    q = (rms_norm(q_lat, q_a_norm) @ w_q_b).reshape(B, S, MLA_HEADS, MLA_NOPE + MLA_ROPE)
    q_nope, q_pe = q[..., :MLA_NOPE], rope(q[..., MLA_NOPE:], positions)
    kv = (rms_norm(kv_lat, kv_a_norm) @ w_kv_b).reshape(B, S, MLA_HEADS, MLA_NOPE + MLA_V)
    k_nope, mla_v = kv[..., :MLA_NOPE], kv[..., MLA_NOPE:]
    k_pe = jnp.broadcast_to(rope(k_rope[:, :, None, :], positions), (B, S, MLA_HEADS, MLA_ROPE))
    mla_q = jnp.concatenate([q_nope, q_pe], -1)
    mla_k = jnp.concatenate([k_nope, k_pe], -1)
    mla_o = chunk_causal_softmax_attention(mla_q, mla_k, mla_v)
    mla_o = rms_norm(mla_o.reshape(B, S, MLA_WIDTH), mla_out_norm)
    return jnp.concatenate([sb_o, mla_o], -1) @ w_o


def moe_ffn(h, w_router, b_router, w_gate_up, b_gate_up, w_down, b_down):
    B, S, D = h.shape
    T = B * S
    xf = h.reshape(T, D)
    logits = (xf @ w_router + b_router).astype(jnp.float32)
    top_vals, top_idx = lax.top_k(logits, TOP_K)
    gates = jax.nn.softmax(top_vals, axis=-1)
    n_assign = T * TOP_K
    e_flat = top_idx.reshape(-1)
    tok_flat = jnp.arange(n_assign, dtype=jnp.int32) // TOP_K
    gate_flat = gates.reshape(-1)
    order = jnp.argsort(e_flat)
    e_sorted = e_flat[order]
    counts = jnp.bincount(e_flat, length=N_EXPERTS)
    starts = jnp.cumsum(counts) - counts
    padded = ((counts + MOE_BLOCK - 1) // MOE_BLOCK) * MOE_BLOCK
    padded_ends = jnp.cumsum(padded)
    padded_starts = padded_ends - padded
    rank = jnp.arange(n_assign, dtype=jnp.int32) - starts[e_sorted]
    dest = padded_starts[e_sorted] + rank
    n_rows = n_assign + N_EXPERTS * MOE_BLOCK
    n_blocks = n_rows // MOE_BLOCK
    row_tok = jnp.zeros((n_rows,), jnp.int32).at[dest].set(tok_flat[order])
    row_gate = jnp.zeros((n_rows,), jnp.float32).at[dest].set(gate_flat[order])
    block_expert = jnp.minimum(
        jnp.searchsorted(padded_ends, jnp.arange(n_blocks, dtype=jnp.int32) * MOE_BLOCK, side='right'),
        N_EXPERTS - 1)
    x_rows = xf[row_tok].reshape(n_blocks, MOE_BLOCK, D)

    def expert_block(args):
        xb, e = args
        gu = xb @ w_gate_up[e] + b_gate_up[e]
        g, up = gu[:, :D_FF], gu[:, D_FF:]
        g = jnp.minimum(g, SWIGLU_LIMIT)
        up = jnp.clip(up, -SWIGLU_LIMIT, SWIGLU_LIMIT)
        act = (up + 1.0) * (g * jax.nn.sigmoid(SWIGLU_ALPHA * g))
        return act @ w_down[e] + b_down[e]

    y_rows = lax.map(expert_block, (x_rows, block_expert)).reshape(n_rows, D)
    out = jnp.zeros((T, D), jnp.float32).at[row_tok].add(y_rows.astype(jnp.float32) * row_gate[:, None])
    return out.astype(h.dtype).reshape(B, S, D)


def setup_inputs(seed: int = 0) -> dict:
    key = jax.random.key(seed)
    ks = jax.random.split(key, 24)
    f32 = jnp.float32

    def nrm(k, shape, scale):
        return jax.random.normal(k, shape, f32) * scale

    x = jax.random.normal(ks[0], (BATCH, SEQ, D_MODEL), f32)
    offs = jax.random.randint(ks[1], (BATCH, 1), 0, 64, dtype=jnp.int32) * CHUNK
    positions = offs + jnp.arange(SEQ, dtype=jnp.int32)[None, :]
    ln_in_g = 1.0 + nrm(ks[2], (D_MODEL,), 0.02)
    ln_in_b = nrm(ks[3], (D_MODEL,), 0.02)
    in_scale = jnp.ones((IN_PROJ,), f32).at[2 * SB_WIDTH:3 * SB_WIDTH].set(DEEPNORM_BETA)
    w_in = nrm(ks[4], (DEPTH, D_MODEL, IN_PROJ), D_MODEL ** -0.5) * in_scale
    q_a_norm = 1.0 + nrm(ks[5], (DEPTH, MLA_Q_RANK), 0.02)
    w_q_b = nrm(ks[6], (DEPTH, MLA_Q_RANK, MLA_HEADS * (MLA_NOPE + MLA_ROPE)), MLA_Q_RANK ** -0.5)
    kv_a_norm = 1.0 + nrm(ks[7], (DEPTH, MLA_KV_RANK), 0.02)
    kv_scale = jnp.tile(jnp.concatenate([jnp.ones((MLA_NOPE,), f32),
                                         jnp.full((MLA_V,), DEEPNORM_BETA, f32)]), MLA_HEADS)
    w_kv_b = nrm(ks[8], (DEPTH, MLA_KV_RANK, MLA_HEADS * (MLA_NOPE + MLA_V)), MLA_KV_RANK ** -0.5) * kv_scale
    sb_out_norm = 1.0 + nrm(ks[9], (DEPTH, SB_WIDTH), 0.02)
    mla_out_norm = 1.0 + nrm(ks[10], (DEPTH, MLA_WIDTH), 0.02)
    w_o = nrm(ks[11], (DEPTH, MIX_WIDTH, D_MODEL), MIX_WIDTH ** -0.5 * DEEPNORM_BETA)
    ln_mix_g = 1.0 + nrm(ks[12], (DEPTH, D_MODEL), 0.02)
    ln_mix_b = nrm(ks[13], (DEPTH, D_MODEL), 0.02)
    w_router = nrm(ks[14], (DEPTH, D_MODEL, N_EXPERTS), D_MODEL ** -0.5)
    b_router = nrm(ks[15], (DEPTH, N_EXPERTS), 0.01)
    w_gate_up = nrm(ks[16], (DEPTH, N_EXPERTS, D_MODEL, 2 * D_FF), D_MODEL ** -0.5 * DEEPNORM_BETA)
    b_gate_up = nrm(ks[17], (DEPTH, N_EXPERTS, 2 * D_FF), 0.02)
    w_down = nrm(ks[18], (DEPTH, N_EXPERTS, D_FF, D_MODEL), D_FF ** -0.5 * DEEPNORM_BETA)
    b_down = nrm(ks[19], (DEPTH, N_EXPERTS, D_MODEL), 0.02)
    ln_ffn_g = 1.0 + nrm(ks[20], (DEPTH, D_MODEL), 0.02)
    ln_ffn_b = nrm(ks[21], (DEPTH, D_MODEL), 0.02)
    return {'x': x, 'positions': positions, 'ln_in_g': ln_in_g, 'ln_in_b': ln_in_b,
            'w_in': w_in, 'q_a_norm': q_a_norm, 'w_q_b': w_q_b, 'kv_a_norm': kv_a_norm,
            'w_kv_b': w_kv_b, 'sb_out_norm': sb_out_norm, 'mla_out_norm': mla_out_norm,
            'w_o': w_o, 'ln_mix_g': ln_mix_g, 'ln_mix_b': ln_mix_b,
            'w_router': w_router, 'b_router': b_router, 'w_gate_up': w_gate_up,
            'b_gate_up': b_gate_up, 'w_down': w_down, 'b_down': b_down,
            'ln_ffn_g': ln_ffn_g, 'ln_ffn_b': ln_ffn_b}


def reference(x, positions, ln_in_g, ln_in_b, w_in, q_a_norm, w_q_b, kv_a_norm, w_kv_b,
              sb_out_norm, mla_out_norm, w_o, ln_mix_g, ln_mix_b, w_router, b_router,
              w_gate_up, b_gate_up, w_down, b_down, ln_ffn_g, ln_ffn_b):
    h = layer_norm(x, ln_in_g, ln_in_b)
    for l in range(DEPTH):
        mix = hybrid_mixer(h, positions, w_in[l], q_a_norm[l], w_q_b[l], kv_a_norm[l], w_kv_b[l],
                           sb_out_norm[l], mla_out_norm[l], w_o[l])
        h = layer_norm(DEEPNORM_ALPHA * h + mix, ln_mix_g[l], ln_mix_b[l])
        ffn = moe_ffn(h, w_router[l], b_router[l], w_gate_up[l], b_gate_up[l], w_down[l], b_down[l])
        h = layer_norm(DEEPNORM_ALPHA * h + ffn, ln_ffn_g[l], ln_ffn_b[l])
    return h
```

```python
import numpy as np
import concourse.bass as bass
import concourse.mybir as mybir
from concourse.bass_utils import run_bass_kernel_spmd

F32 = mybir.dt.float32
BF16 = mybir.dt.bfloat16
I32 = mybir.dt.int32
AF = mybir.ActivationFunctionType
ALU = mybir.AluOpType
AX = mybir.AxisListType

D = 2048
S = 2048
NB = 16
NS = 8
H = 8
INP = 3904
E = 32
DFF = 2048
CAP = 256
ALPHA = 2.0 ** 0.25
LN_EPS = 1e-5
RMS_EPS = 1e-6
SEG = 12000
import os as _os
DEBUG_EMIT = bool(_os.environ.get("DEBUG_EMIT"))

QA = [0, 3, 4, 7, 8, 11, 12, 15]
QB = [1, 2, 5, 6, 9, 10, 13, 14]


class Prog:
    ENG = ("pe", "act", "dve", "pool", "sp")

    def __init__(self, nc):
        self.nc = nc
        self.ops = []
        self.lastw = {}
        self.readers = {}
        self.dma_cnt = {}

    def add(self, eng, fn, r=(), w=(), dma=None):
        i = len(self.ops)
        deps = set()
        for b in list(r) + list(w):
            if b in self.lastw:
                deps.add(self.lastw[b])
        for b in w:
            for x in self.readers.get(b, ()):
                deps.add(x)
        for b in r:
            if b.startswith("ps"):
                for x in self.readers.get(b, ()):
                    if self.ops[x]["eng"] != eng:
                        deps.add(x)
        deps.discard(i)
        op = dict(id=i, eng=eng, fn=fn, deps=deps, dma=dma, sig=False, r=tuple(r), w=tuple(w))
        if dma is not None:
            self.dma_cnt[dma] = self.dma_cnt.get(dma, 0) + 1
            op["dval"] = 16 * self.dma_cnt[dma]
        self.ops.append(op)
        for b in w:
            self.lastw[b] = i
            self.readers[b] = []
        for b in r:
            if b not in w:
                self.readers.setdefault(b, []).append(i)
        return i

    def emit(self, stack):
        nc = self.nc
        ops = self.ops
        for op in ops:
            nd = set()
            for d in op["deps"]:
                dop = ops[d]
                if op["fn"] is None:
                    if dop["dma"] is None and dop["eng"] == op["eng"]:
                        continue
                    nd.add(d)
                    continue
                if dop["dma"] is None and dop["eng"] == op["eng"] and op["dma"] is None:
                    if op["eng"] == "pe":
                        continue
                    wr = set(dop["w"])
                    if not (wr & set(op["r"])) and not (wr & set(op["w"])):
                        continue
                nd.add(d)
            op["deps"] = nd
            for d in nd:
                ops[d]["sig"] = True
        cnt = {e: 0 for e in self.ENG}
        for op in ops:
            if op["dma"] is None and op["sig"]:
                cnt[op["eng"]] += 1
                op["sidx"] = cnt[op["eng"]]
        nseg = {e: (cnt[e] + SEG - 1) // SEG for e in self.ENG}
        sems = {}
        for e in self.ENG:
            for k in range(max(1, nseg[e])):
                sems[(e, k)] = stack.enter_context(nc.semaphore(f"s_{e}_{k}"))
        dsem = {}
        for name in self.dma_cnt:
            dsem[name] = stack.enter_context(nc.semaphore(f"d_{name}"))

        def token(op):
            if op["dma"] is not None:
                return ("d", op["dma"]), dsem[op["dma"]], op["dval"]
            k = (op["sidx"] - 1) // SEG
            return (op["eng"], k), sems[(op["eng"], k)], (op["sidx"] - 1) % SEG + 1

        out_dmas = [op for op in ops if op.get("dma") and op["dma"].startswith("out")]
        block = stack.enter_context(nc.Block())
        handles = {"pe": block.tensor, "act": block.scalar, "dve": block.vector,
                   "pool": block.gpsimd, "sp": block.sync}

        def make(engname):
            def body(eng):
                known = {}
                for op in ops:
                    if op["eng"] != engname:
                        continue
                    need = {}
                    for d in op["deps"]:
                        key, sem, val = token(ops[d])
                        if known.get(key, 0) >= val:
                            continue
                        if key not in need or need[key][1] < val:
                            need[key] = (sem, val)
                    for key, (sem, val) in need.items():
                        eng.wait_ge(sem, val)
                        known[key] = val
                        if DEBUG_EMIT:
                            print(f"  [{engname}] wait {key} >= {val}")
                    if DEBUG_EMIT:
                        tk = token(op) if (op["dma"] is not None or op["sig"]) else None
                        print(f"  [{engname}] op{op['id']} r={op['r']} w={op['w']} sig={tk and (tk[0], tk[2])}")
                    if op["fn"] is None:
                        continue
                    ins = op["fn"](eng)
                    if op["dma"] is not None:
                        ins.then_inc(dsem[op["dma"]], 16)
                    elif op["sig"]:
                        _, sem, _ = token(op)
                        ins.then_inc(sem, 1)
                if engname == "sp":
                    last = {}
                    for op in out_dmas:
                        last[op["dma"]] = op["dval"]
                    for name, val in last.items():
                        eng.wait_ge(dsem[name], val)
            return body

        for e in self.ENG:
            handles[e](make(e))

    def fence(self):
        last = {}
        for op in self.ops:
            if op["fn"] is None:
                continue
            k = ("d", op["dma"]) if op["dma"] is not None else op["eng"]
            last[k] = op["id"]
        ids = set(last.values())
        for e in self.ENG:
            i = len(self.ops)
            self.ops.append(dict(id=i, eng=e, fn=None, deps=set(ids), dma=None, sig=False, r=(), w=()))
        self.lastw = {}
        self.readers = {}

    def mm(self, out, lhsT, rhs, start, stop, r, w):
        return self.add("pe", lambda e: e.matmul(out, lhsT, rhs, start=start, stop=stop), r=r, w=w)

    def tr(self, out, in_, ident, r, w):
        return self.add("pe", lambda e: e.transpose(out, in_, ident), r=r, w=w)

    def act(self, out, in_, func, r, w, bias=None, scale=None, accum_out=None, eng="act"):
        kw = {}
        if bias is not None:
            kw["bias"] = bias
        if scale is not None:
            kw["scale"] = scale
        if accum_out is not None:
            kw["accum_out"] = accum_out
        return self.add(eng, lambda e: e.activation(out, in_, func, **kw), r=r, w=w)

    def ts(self, eng, out, in0, s1, s2, op0, op1, r, w, accum_out=None):
        if op1 is None:
            return self.add(eng, lambda e: e.tensor_scalar(out, in0, s1, None, op0), r=r, w=w)
        if accum_out is not None:
            return self.add(eng, lambda e: e.tensor_scalar(out, in0, s1, s2, op0, op1, accum_out=accum_out), r=r, w=w)
        return self.add(eng, lambda e: e.tensor_scalar(out, in0, s1, s2, op0, op1), r=r, w=w)

    def tt(self, eng, out, in0, in1, op, r, w):
        return self.add(eng, lambda e: e.tensor_tensor(out, in0, in1, op), r=r, w=w)

    def stt(self, eng, out, in0, scalar, in1, op0, op1, r, w):
        return self.add(eng, lambda e: e.scalar_tensor_tensor(out, in0, scalar, in1, op0, op1), r=r, w=w)

    def cp(self, eng, out, in_, r, w):
        if eng == "act":
            return self.add(eng, lambda e: e.copy(out, in_), r=r, w=w)
        return self.add(eng, lambda e: e.tensor_copy(out, in_), r=r, w=w)

    def ms(self, eng, ap, val, w):
        return self.add(eng, lambda e: e.memset(ap, val), r=(), w=w)

    def dma(self, stream, out, in_, r, w, eng="sp"):
        return self.add(eng, lambda e: e.dma_start(out=out, in_=in_), r=r, w=w, dma=stream)


class Arena:
    def __init__(self, nc, stack, nbytes):
        self.t = stack.enter_context(nc.sbuf_tensor("arena", [128, nbytes // 4], F32))
        self.f = self.t[:]
        self.b = self.f.bitcast(BF16)
        self.i = self.f.bitcast(I32)

    def f32(self, off, n, p=128):
        return self.f[0:p, off // 4: off // 4 + n]

    def bf(self, off, n, p=128):
        return self.b[0:p, off // 2: off // 2 + n]

    def i32(self, off, n, p=128):
        return self.i[0:p, off // 4: off // 4 + n]


ARENA_BYTES = 210944
O_H0T = 10240
O_MIX = 75776
O_Z = 108544

TWO_PI = 2.0 * np.pi
CW1 = float(np.float32(6.28125))
CW2 = float(np.float32(TWO_PI - 6.28125))


class _Done(Exception):
    pass


def build(nc, stack, n_exp=E, stage="full", stop=None):
    P = Prog(nc)
    try:
        _build(nc, stack, P, n_exp, stage, stop)
    except _Done:
        pass
    return P


def _build(nc, stack, P, n_exp, stage, stop):
    A = Arena(nc, stack, ARENA_BYTES)

    def dram(name, shape, dt=F32, kind="ExternalInput"):
        return nc.dram_tensor(name, list(shape), dt, kind=kind).ap()

    xp = dram("xp", [NB, 128, D])
    posb = dram("posb", [S], I32)
    c_ident = dram("c_ident", [128, 128])
    c_triu = dram("c_triu", [128, 128])
    c_tril = dram("c_tril", [128, 128])
    c_msb = dram("c_msb", [128, 128])
    c_mmla = dram("c_mmla", [128, 128])
    c_pairf = dram("c_pairf", [128, NS * 2 * 128])
    c_small = dram("c_small", [128, 64])
    c_onorm = dram("c_onorm", [128, 16])
    c_iota = dram("c_iota", [128, CAP])
    w_in = dram("w_in", [D, INP])
    w_q_b = dram("w_q_b", [512, 1536])
    w_kv_b = dram("w_kv_b", [256, 2048])
    w_o = dram("w_o", [D, D])
    ln_in_g = dram("ln_in_g", [D])
    ln_in_b = dram("ln_in_b", [D])
    ln_mix_g = dram("ln_mix_g", [D])
    ln_mix_b = dram("ln_mix_b", [D])
    ln_ffn_g = dram("ln_ffn_g", [D])
    ln_ffn_b = dram("ln_ffn_b", [D])
    w_router = dram("w_router", [D, E])
    b_router = dram("b_router", [E])
    w_gu = dram("w_gu", [n_exp, 32, 128, 2048])
    bgu = dram("bgu", [128, n_exp * 32])
    w_dn = dram("w_dn", [n_exp, 16, 128, 2048])
    b_dn = dram("b_dn", [n_exp, D])
    out = dram("out", [NS, 128, D], kind="ExternalOutput")

    ps = [stack.enter_context(nc.psum_tensor(f"ps{i}", [128, 512], F32))[:] for i in range(8)]

    def chk(name, ap=None, n=0, p=128):
        if stop != name:
            return
        P.fence()
        dbg = A.f32(O_Z + 82048, D)
        if ap is not None:
            P.cp("dve", dbg[0:p, 0:n], ap, r=(), w=("dbg",))
            P.dma("out0", out[0][0:p, 0:n], dbg[0:p, 0:n], r=("dbg",), w=())
        else:
            P.ms("dve", dbg[:, 0:8], 1.0, w=("dbg",))
            P.dma("out0", out[0][:, 0:8], dbg[:, 0:8], r=("dbg",), w=())
        P.emit(stack)
        raise _Done()

    psb = [p.bitcast(BF16) for p in ps]

    ident_f = A.f32(0, 128)
    ident_b = A.bf(512, 128)
    triu_b = A.bf(768, 128)
    ones_b = A.bf(1024, 128)
    msb_b = A.bf(1280, 128)
    mmla_b = A.bf(1536, 128)
    pairf_b = A.bf(1792, NS * 2 * 128)
    small = A.f32(5888, 64)
    vis = small[:, 0:8]
    invf = small[0:64, 8:9]
    lng = small[:, 16:32]
    lnb = small[:, 32:48]
    qan = small[:, 48:52]
    kvan = small[:, 52:54]
    onorm = A.f32(6144, 16)
    iota = A.f32(6208, CAP)
    tril_f = A.f32(7232, 128)
    ones_f = A.f32(7744, 128)
    stat = A.f32(8256, 496)

    tmpc = A.f32(O_Z + 49280, 128 * NS * 2)

    def load_const(name, dst_b, src, n, key):
        P.dma("c0", tmpc[:, 0:n], src, r=(), w=("tmpc",))
        P.cp("dve", dst_b, tmpc[:, 0:n], r=("tmpc",), w=(key,))

    P.dma("c1", ident_f, c_ident, r=(), w=("ident_f",))
    P.dma("c2", small, c_small, r=(), w=("small",))
    P.dma("c3", onorm, c_onorm, r=(), w=("onorm",))
    P.dma("c4", iota, c_iota, r=(), w=("iota",))
    P.dma("c5", tril_f, c_tril, r=(), w=("tril_f",))
    P.cp("dve", ident_b, ident_f, r=("ident_f",), w=("ident_b",))
    load_const("triu", triu_b, c_triu, 128, "triu_b")
    load_const("msb", msb_b, c_msb, 128, "msb_b")
    load_const("mmla", mmla_b, c_mmla, 128, "mmla_b")
    load_const("pairf", pairf_b, c_pairf, NS * 2 * 128, "pairf_b")
    P.ms("dve", ones_b, 1.0, w=("ones_b",))
    P.ms("dve", ones_f, 1.0, w=("ones_f",))

    chk("consts", triu_b, 128)
    h0T = A.bf(O_H0T, 16 * S).rearrange("p (c t) -> p c t", c=16)
    mixT = A.bf(O_MIX, 16 * 1024).rearrange("p (c t) -> p c t", c=16)
    cos2 = A.f32(O_Z, S, p=64)
    sin2 = A.f32(O_Z + 8192, S, p=64)
    kropeT = A.bf(O_Z + 16384, S, p=64)
    qlatnT = A.bf(O_Z + 20480, 4 * 1024).rearrange("p (c t) -> p c t", c=4)
    kvlatnT = A.bf(O_Z + 28672, 2 * S).rearrange("p (c t) -> p c t", c=2)
    kT = A.bf(O_Z + 36864, S)
    vt = A.bf(O_Z + 40960, 16 * 132).rearrange("p (b d) -> p b d", b=16)
    qT = A.bf(O_Z + 45184, 1024)
    qpeT = A.bf(O_Z + 47232, 1024, p=64)
    ws = [A.f32(O_Z + 49280 + 8192 * i, 16 * 128).rearrange("p (c n) -> p c n", c=16) for i in range(2)]
    wb = [A.bf(O_Z + 65664 + 4096 * i, 16 * 128).rearrange("p (c n) -> p c n", c=16) for i in range(4)]
    xt = A.f32(O_Z + 82048, D)
    xn = A.bf(O_Z + 90240, D)
    e_t = A.f32(O_Z + 94336, 512)
    sp_t = A.bf(O_Z + 96384, 512)
    t1_t = A.f32(O_Z + 97408, 512)
    w_t = A.bf(O_Z + 99456, 512)
    carry = [A.f32(O_Z + 100480 + 512 * i, 128) for i in range(2)]
    osb = A.bf(O_Z + 101504, 128)
    rinv = A.f32(O_Z + 101760, 1)

    own_tok = lambda ap3, c, g: ap3[:, c, :].rearrange("p (b two t) -> p b two t", two=2, t=128)[:, 4 * g:4 * g + 4, 0, :]

    r_i = A.i32(O_Z + 49280, S, p=64)
    r_a = A.f32(O_Z + 49280 + 8192, S, p=64)
    r_k = A.f32(O_Z + 49280 + 16384, S, p=64)
    r_r = A.f32(O_Z + 49280 + 24576, S, p=64)
    r_m = A.f32(O_Z + 49280 + 32768, S, p=64)
    P.dma("c6", r_i, posb.partition_broadcast(64), r=("tmpc",), w=("r_i", "tmpc"))
    P.cp("dve", r_a, r_i, r=("r_i", "small"), w=("r_a",))
    P.ts("dve", r_a, r_a, invf, None, ALU.mult, None, r=("r_a", "small"), w=("r_a",))
    P.ts("dve", r_i, r_a, 1.0 / TWO_PI, None, ALU.mult, None, r=("r_a",), w=("r_i",))
    P.cp("dve", r_k, r_i, r=("r_i",), w=("r_k",))
    P.stt("dve", r_r, r_k, -CW1, r_a, ALU.mult, ALU.add, r=("r_k", "r_a"), w=("r_r",))
    P.stt("dve", r_r, r_k, -CW2, r_r, ALU.mult, ALU.add, r=("r_k", "r_r"), w=("r_r",))

    def wrap(x):
        P.ts("dve", r_m, x, float(np.pi), -TWO_PI, ALU.is_gt, ALU.mult, r=("r_r", "r_a"), w=("r_m",))
        P.tt("dve", x, x, r_m, ALU.add, r=("r_m",), w=("r_r", "r_a"))
        P.ts("dve", r_m, x, -float(np.pi), TWO_PI, ALU.is_lt, ALU.mult, r=("r_r", "r_a"), w=("r_m",))
        P.tt("dve", x, x, r_m, ALU.add, r=("r_m",), w=("r_r", "r_a"))

    P.ts("dve", r_a, r_r, float(np.pi / 2), None, ALU.add, None, r=("r_r",), w=("r_a",))
    wrap(r_r)
    wrap(r_a)
    ovl = ("ws0", "ws1", "wb0", "wb1", "wb2", "wb3", "xt", "tmpc")
    P.act(sin2, r_r, AF.Sin, r=("r_r",), w=("sin2",) + ovl)
    P.act(cos2, r_a, AF.Sin, r=("r_a",), w=("cos2",) + ovl)

    chk("rope", cos2, 2048, 64)
    def ln_stats(src, key):
        for q in range(4):
            P.add("dve", (lambda q: lambda e: e.bn_stats(stat[:, 6 * q:6 * q + 6], src[:, 512 * q:512 * q + 512]))(q),
                  r=(key,), w=("bnst",))
        P.add("dve", lambda e: e.bn_aggr(stat[:, 24:26], stat[:, 0:24]), r=("bnst",), w=("mv",))
        P.act(stat[:, 26:27], stat[:, 25:26], AF.Sqrt, r=("mv",), w=("std",), bias=eps_t[:, 0:1], scale=1.0)
        P.add("dve", lambda e: e.reciprocal(stat[:, 27:28], stat[:, 26:27]), r=("std",), w=("rstd",))
        P.ts("dve", stat[:, 28:29], stat[:, 24:25], -1.0, stat[:, 27:28], ALU.mult, ALU.mult,
             r=("mv", "rstd"), w=("nmr",))

    eps_t = A.f32(8256 + 4 * 480, 4)
    P.ms("dve", eps_t[:, 0:1], LN_EPS, w=("eps",))
    P.ms("dve", eps_t[:, 1:2], RMS_EPS, w=("eps",))

    bkey = ("pst0", "ps6")
    for blk in range(NB):
        P.dma("xt", xt, xp[blk], r=(), w=("xt",))
        ln_stats(xt, "xt")
        if blk == 0:
            chk("ln_s", stat[:, 24:29], 5)
        P.act(xn, xt, AF.Identity, r=("xt", "rstd", "nmr"), w=("xn",), bias=stat[:, 28:29], scale=stat[:, 27:28])
        if blk == 0:
            chk("ln_x", xn, 2048)
        if blk == 1:
            chk("ln_b0", h0T[:, 3, :], 2048)
        for c4 in range(4):
            half = c4 % 2
            pst = psb[7 - half][:, 0:512].rearrange("p (i t) -> p i t", i=4)
            for i in range(4):
                c = 4 * c4 + i
                P.tr(pst[:, i, :], xn[:, 128 * c:128 * c + 128], ident_b, r=("xn", "ident_b"), w=(bkey[half],))
            if blk == 0 and c4 == 0:
                chk("ln_t", psb[7][:, 0:512], 512)
            if blk == 0 and c4 == 1:
                chk("ln_t1", psb[6][:, 0:512], 512)
            for i in range(4):
                c = 4 * c4 + i
                import os
                evm = os.environ.get("EVM", "mix")
                eng = "dve" if half == 0 else "pool"
                if evm == "dve":
                    eng = "dve"
                if evm == "act":
                    eng = "pool"
                if eng == "pool":
                    P.act(h0T[:, c, 128 * blk:128 * blk + 128], pst[:, i, :], AF.Identity,
                          r=(bkey[half], "small"), w=(f"h0T{c}",), bias=lnb[:, c:c + 1], scale=lng[:, c:c + 1])
                else:
                    P.ts("dve", h0T[:, c, 128 * blk:128 * blk + 128], pst[:, i, :], lng[:, c:c + 1], lnb[:, c:c + 1],
                         ALU.mult, ALU.add, r=(bkey[half], "small"), w=(f"h0T{c}",))
            if blk == 0 and c4 == 0:
                chk("ln_c0", h0T[:, 3, :], 2048)
            if blk == 0 and c4 == 1:
                chk("ln_c1", h0T[:, 3, :], 2048)
            if blk == 0 and c4 == 3:
                chk("ln_c3", h0T[:, 3, :], 2048)

    chk("ln", h0T[:, 3, :], 2048)
    wcnt = [0, 0]
    ws_flat = [A.f32(O_Z + 49280 + 8192 * i, 2048) for i in range(2)]
    cast_rr = [0]

    def load_w(src3, ncols, rows_c, scale_ap=None, dst=None, scale_off=0, cast_engs=("pool",)):
        si = wcnt[0] % 2
        wcnt[0] += 1
        s_v = ws_flat[si][:, 0:rows_c * ncols].rearrange("p (c n) -> p c n", c=rows_c)
        P.dma(f"ws{si}", s_v, src3, r=(), w=(f"ws{si}",))
        if dst is None:
            bi = wcnt[1] % 4
            wcnt[1] += 1
            d_v = wb[bi][:, 0:rows_c, 0:ncols]
            key = (f"wb{bi}",)
        else:
            d_v, key = dst
        if scale_ap is None:
            eng = cast_engs[cast_rr[0] % len(cast_engs)]
            cast_rr[0] += 1
            P.cp(eng, d_v, s_v, r=(f"ws{si}",), w=key)
        else:
            for c in range(rows_c):
                P.ts("pool", d_v[:, c, :], s_v[:, c, :], scale_ap[:, scale_off + c:scale_off + c + 1], None, ALU.mult, None,
                     r=(f"ws{si}", "small", "onorm"), w=key)
        return d_v, key

    def win_cols(c0, n):
        return w_in[:, c0:c0 + n].rearrange("(c p) n -> p c n", p=128)

    sqb = [sp_t, w_t]

    def latent_group(wts, ncol, rhs_of_c, rhs_keys, dst_of_f, dst_key, nfeat):
        for f, (wt, wk) in enumerate(wts):
            for c in range(16):
                P.mm(ps[f], wt[:, c, :], rhs_of_c(c), c == 0, c == 15, r=wk + (f"h0T{c}",), w=(f"ps{f}",))
        for f in range(len(wts)):
            sq = sqb[f % 2]
            P.act(sq, ps[f], AF.Square, r=(f"ps{f}",), w=(f"sq{f % 2}",))
            P.mm(ps[4], ones_b, sq, f == 0, f == len(wts) - 1, r=("ones_b", f"sq{f % 2}"), w=("ps4",))
        P.act(e_t, ps[4], AF.Sqrt, r=("ps4", "eps"), w=("e_t",), bias=eps_t[:, 1:2], scale=1.0 / nfeat)
        P.add("dve", lambda e: e.reciprocal(t1_t, e_t), r=("e_t",), w=("t1_t",))
        for f in range(len(wts)):
            P.tt("dve", dst_of_f(f), ps[f], t1_t, ALU.mult, r=(f"ps{f}", "t1_t"), w=(dst_key,))

    qw = [load_w(win_cols(3072 + 128 * f, 128), 128, 16) for f in range(4)]
    for g in range(2):
        latent_group(qw, 128, lambda c, g=g: own_tok(h0T, c, g), None,
                     lambda f, g=g: qlatnT[:, f, 512 * g:512 * g + 512], "qlatnT", 512.0)
    kvw = [load_w(win_cols(3584 + 128 * f, 128), 128, 16) for f in range(2)]
    for g in range(4):
        latent_group(kvw, 128, lambda c, g=g: h0T[:, c, 512 * g:512 * g + 512], None,
                     lambda f, g=g: kvlatnT[:, f, 512 * g:512 * g + 512], "kvlatnT", 256.0)

    def make_rot(dst, src, nck, key_d, key_s):
        P.ts("pool", dst[:, 0:nck, 0:32], src[:, 0:nck, 32:64], -1.0, None, ALU.mult, None, r=key_s, w=key_d)
        P.cp("pool", dst[:, 0:nck, 32:64], src[:, 0:nck, 0:32], r=key_s, w=key_d)

    def rope_evac(dst, pa, pb, cs, sn, keys_r, key_w):
        P.tt("dve", t1_t[0:64, :], pa, cs, ALU.mult, r=keys_r + ("cos2",), w=("t1_t",))
        P.tt("dve", e_t[0:64, :], pb, sn, ALU.mult, r=keys_r + ("sin2",), w=("e_t",))
        P.tt("pool", dst, t1_t[0:64, :], e_t[0:64, :], ALU.add, r=("t1_t", "e_t"), w=(key_w,))

    krw, krk = load_w(win_cols(3840, 64), 64, 16)
    krot, krotk = wb[wcnt[1] % 4][:, :, 0:64], (f"wb{wcnt[1] % 4}",)
    wcnt[1] += 1
    make_rot(krot, krw, 16, krotk, krk)
    for g in range(4):
        for c in range(16):
            P.mm(ps[0][0:64, :], krw[:, c, :], h0T[:, c, 512 * g:512 * g + 512], c == 0, c == 15,
                 r=krk + (f"h0T{c}",), w=("ps0",))
        for c in range(16):
            P.mm(ps[1][0:64, :], krot[:, c, :], h0T[:, c, 512 * g:512 * g + 512], c == 0, c == 15,
                 r=krotk + (f"h0T{c}",), w=("ps1",))
        rope_evac(kropeT[:, 512 * g:512 * g + 512], ps[0][0:64, :], ps[1][0:64, :],
                  cos2[:, 512 * g:512 * g + 512], sin2[:, 512 * g:512 * g + 512], ("ps0", "ps1"), "kropeT")

    chk("lat", kvlatnT[:, 1, :], 2048)
    chk("krope", kropeT, 2048, 64)
    chk("qlat", qlatnT[:, 2, :], 1024)
    ss_sb = stat[:, 32:40]
    ss_mla = stat[:, 40:48]
    P.ms("dve", stat[:, 32:48], 0.0, w=("ss",))
    P.ms("dve", vt[:, :, 128:132], 1.0, w=("vt",))

    def attn_slot(kind, h, j):
        nblk = 2 * j + 2
        groups = [list(range(b0, min(b0 + 4, nblk))) for b0 in range(0, nblk, 4)]
        groups = groups[::-1]
        oc = 128 if kind == "sb" else 129
        scale = 128.0 ** -0.5 if kind == "sb" else 192.0 ** -0.5
        mask_b = msb_b if kind == "sb" else mmla_b
        o_ps = ps[5][:, 0:oc]
        npv = 0
        tot_pv = nblk
        cur = 0
        for gi, blks in enumerate(groups):
            nb = len(blks)
            cols = nb * 128
            zb = gi % 2
            z = ps[zb]
            bt = ps[2 + zb]
            last_group = gi == len(groups) - 1
            for i, b in enumerate(blks):
                zo = z[:, 128 * i:128 * i + 128]
                if kind == "sb":
                    P.mm(zo, kT[:, 128 * b:128 * b + 128], qT[:, 128 * j:128 * j + 128], True, True,
                         r=("kT", "qT"), w=(f"ps{zb}",))
                else:
                    P.mm(zo, kT[:, 128 * b:128 * b + 128], qT[:, 128 * j:128 * j + 128], True, False,
                         r=("kT", "qT"), w=(f"ps{zb}",))
                    P.mm(zo, kropeT[:, 128 * b:128 * b + 128], qpeT[:, 128 * j:128 * j + 128], False, True,
                         r=("kropeT", "qpeT"), w=(f"ps{zb}",))

            def apply_masks(t, key):
                if gi == 0:
                    i0, i1 = nb - 2, nb - 1
                    P.tt("pool", t[:, 128 * i0:128 * i0 + 128], t[:, 128 * i0:128 * i0 + 128], mask_b, ALU.mult,
                         r=(key, "msb_b", "mmla_b"), w=(key,))
                    P.ts("pool", t[:, 128 * i1:128 * i1 + 128], t[:, 128 * i1:128 * i1 + 128], vis[:, j:j + 1], None,
                         ALU.mult, None, r=(key, "small"), w=(key,))

            if kind == "sb":
                P.act(e_t[:, 0:cols], z[:, 0:cols], AF.Exp, r=(f"ps{zb}",), w=("e_t",), scale=scale)
                P.act(sp_t[:, 0:cols], e_t[:, 0:cols], AF.Ln, r=("e_t", "ones_f"), w=("sp_t",), bias=ones_f[:, 0:1], scale=1.0)
                apply_masks(sp_t, "sp_t")
                P.stt("dve", t1_t[:, 0:cols], z[:, 0:cols], scale, sp_t[:, 0:cols], ALU.mult, ALU.subtract,
                      r=(f"ps{zb}", "sp_t"), w=("t1_t",))
                for i, b in enumerate(blks):
                    terms = [(triu_b, i, "triu_b")]
                    pair = b // 2
                    if b % 2 == 0:
                        terms.append((pairf_b[:, (pair * 2 + 0) * 128:(pair * 2 + 1) * 128], i + 1, "pairf_b"))
                    else:
                        terms.append((pairf_b[:, (pair * 2 + 1) * 128:(pair * 2 + 2) * 128], i - 1, "pairf_b"))
                    for i2, b2 in enumerate(blks):
                        if b2 // 2 > pair:
                            terms.append((ones_b, i2, "ones_b"))
                    for n, (m, src, mk) in enumerate(terms):
                        P.mm(bt[:, 128 * i:128 * i + 128], m, sp_t[:, 128 * src:128 * src + 128], n == 0,
                             n == len(terms) - 1, r=(mk, "sp_t"), w=(f"ps{2 + zb}",))
                if not last_group:
                    for i in range(nb):
                        P.mm(ps[4][:, 0:128], ones_b, sp_t[:, 128 * i:128 * i + 128], i == 0, i == nb - 1,
                             r=("ones_b", "sp_t"), w=("ps4",))
                P.tt("dve", t1_t[:, 0:cols], t1_t[:, 0:cols], bt[:, 0:cols], ALU.subtract, r=("t1_t", f"ps{2 + zb}"), w=("t1_t",))
                if gi > 0:
                    t3 = t1_t[:, 0:cols].rearrange("p (b t) -> p b t", b=nb)
                    P.tt("pool", t3, t3, carry[cur].unsqueeze(1).to_broadcast([128, nb, 128]), ALU.subtract,
                         r=("t1_t", f"carry{cur}"), w=("t1_t",))
                if not last_group:
                    if gi == 0:
                        P.cp("dve", carry[0], ps[4][:, 0:128], r=("ps4",), w=("carry0",))
                        cur = 0
                    else:
                        P.tt("dve", carry[1 - cur], carry[cur], ps[4][:, 0:128], ALU.add, r=(f"carry{cur}", "ps4"),
                             w=(f"carry{1 - cur}",))
                        cur = 1 - cur
                P.act(w_t[:, 0:cols], t1_t[:, 0:cols], AF.Exp, r=("t1_t",), w=("w_t",))
            else:
                P.act(w_t[:, 0:cols], z[:, 0:cols], AF.Exp, r=(f"ps{zb}",), w=("w_t",), scale=scale)
            apply_masks(w_t, "w_t")
            for i, b in enumerate(blks):
                P.mm(o_ps, w_t[:, 128 * i:128 * i + 128], vt[:, b, 0:oc], npv == 0, npv == tot_pv - 1,
                     r=("w_t", "vt"), w=("ps5",))
                npv += 1
        hc = h if kind == "sb" else 8 + h
        ssd = ss_sb if kind == "sb" else ss_mla
        if kind == "sb":
            P.act(e_t[:, 0:128], o_ps[:, 0:128], AF.Square, r=("ps5",), w=("e_t", "ssacc"), accum_out=stat[:, 64:65])
            P.cp("dve", osb, o_ps[:, 0:128], r=("ps5",), w=("osb",))
        else:
            P.add("dve", lambda e: e.reciprocal(rinv, o_ps[:, 128:129]), r=("ps5",), w=("rinv",))
            P.act(e_t[:, 0:128], o_ps[:, 0:128], AF.Square, r=("ps5", "rinv"), w=("e_t", "ssacc"), scale=rinv,
                  accum_out=stat[:, 64:65])
            P.ts("dve", osb, o_ps[:, 0:128], rinv, None, ALU.mult, None, r=("ps5", "rinv"), w=("osb",))
        P.tt("dve", ssd[:, j:j + 1], ssd[:, j:j + 1], stat[:, 64:65], ALU.add, r=("ssacc", "ss"), w=("ss",))
        P.tr(psb[7][:, 0:128], osb, ident_b, r=("osb", "ident_b"), w=("pst0",))
        P.cp("act", mixT[:, hc, 128 * j:128 * j + 128], psb[7][:, 0:128], r=("pst0",), w=(f"mixT{hc}",))

    def proj_T(dst, wt, wkey, nck, rhs_of, rkeys, ngrp, dkey, m=128):
        for g in range(ngrp):
            pb = 6 + g % 2
            pkeys = ("ps6",) if pb == 6 else ("pst0", "pst1")
            for c in range(nck):
                P.mm(ps[pb][0:m, :], wt[:, c, :], rhs_of(c, g), c == 0, c == nck - 1, r=wkey + rkeys(c), w=pkeys)
            P.cp("act", dst[:, 512 * g:512 * g + 512], ps[pb][0:m, :], r=pkeys, w=(dkey,))

    def proj_v(wt, wkey, nck, lhs_of, lkeys):
        for q4 in range(4):
            pb = 6 + q4 % 2
            pkeys = ("ps6",) if pb == 6 else ("pst0", "pst1")
            for i in range(4):
                blk = 4 * q4 + i
                for c in range(nck):
                    P.mm(ps[pb][:, 128 * i:128 * i + 128], lhs_of(c, blk), wt[:, c, :], c == 0, c == nck - 1,
                         r=wkey + lkeys(c), w=pkeys)
            P.cp("act", vt[:, 4 * q4:4 * q4 + 4, 0:128], ps[pb].rearrange("p (b d) -> p b d", b=4), r=pkeys, w=("vt",))

    n_heads = H if stage != "fast" else 1
    for h in range(n_heads):
        wq, wqk = load_w(win_cols(h * 128, 128), 128, 16)
        wk_, wkk = load_w(win_cols(1024 + h * 128, 128), 128, 16)
        wv, wvk = load_w(win_cols(2048 + h * 128, 128), 128, 16)
        proj_T(qT, wq, wqk, 16, lambda c, g: own_tok(h0T, c, g), lambda c: (f"h0T{c}",), 2, "qT")
        proj_T(kT, wk_, wkk, 16, lambda c, g: h0T[:, c, 512 * g:512 * g + 512], lambda c: (f"h0T{c}",), 4, "kT")
        proj_v(wv, wvk, 16, lambda c, blk: h0T[:, c, 128 * blk:128 * blk + 128], lambda c: (f"h0T{c}",))
        if h == 0:
            chk("proj_k", kT, 2048)
            chk("proj_q", qT, 1024)
            chk("proj_v", vt[:, :, 0:128], 2048)
        for j in range(NS):
            attn_slot("sb", h, j)
            if h == 0 and j == 1:
                chk("sb01", mixT[:, 0, :], 256)
        if h == 0:
            chk("sb0", mixT[:, 0, :], 1024)

    wqb = A.bf(O_H0T, 4 * 1536).rearrange("p (c n) -> p c n", c=4)
    wqb_k = ("h0T0", "h0T1", "h0T2")
    wkvb = A.bf(O_H0T + 12288, 2 * 2048).rearrange("p (c n) -> p c n", c=2)
    wkvb_k = ("h0T3", "h0T4")
    wqrot = A.bf(O_H0T + 20480, 4 * 512).rearrange("p (c n) -> p c n", c=4)
    wqrot_k = ("h0T5",)
    if stage != "fast":
        pass
    for q3 in range(3):
        load_w(w_q_b[:, 512 * q3:512 * q3 + 512].rearrange("(c p) n -> p c n", p=128), 512, 4, scale_ap=qan,
               dst=(wqb[:, :, 512 * q3:512 * q3 + 512], wqb_k))
    for q2 in range(2):
        load_w(w_kv_b[:, 1024 * q2:1024 * q2 + 1024].rearrange("(c p) n -> p c n", p=128), 1024, 2, scale_ap=kvan,
               dst=(wkvb[:, :, 1024 * q2:1024 * q2 + 1024], wkvb_k))
    for h in range(H):
        make_rot(wqrot[:, :, 64 * h:64 * h + 64], wqb[:, :, 192 * h + 128:192 * h + 192], 4, wqrot_k, wqb_k)

    def own4(ap2, g):
        return ap2.rearrange("p (b two t) -> p b two t", two=2, t=128)[:, 4 * g:4 * g + 4, 0, :]

    v3 = lambda ap: ap.rearrange("p (b t) -> p b t", b=4)

    for h in range(n_heads):
        proj_T(kT, wkvb[:, :, 256 * h:256 * h + 128], wkvb_k, 2, lambda c, g: kvlatnT[:, c, 512 * g:512 * g + 512],
               lambda c: ("kvlatnT",), 4, "kT")
        proj_v(wkvb[:, :, 256 * h + 128:256 * h + 256], wkvb_k, 2, lambda c, blk: kvlatnT[:, c, 128 * blk:128 * blk + 128],
               lambda c: ("kvlatnT",))
        proj_T(qT, wqb[:, :, 192 * h:192 * h + 128], wqb_k, 4, lambda c, g: qlatnT[:, c, 512 * g:512 * g + 512],
               lambda c: ("qlatnT",), 2, "qT")
        for g in range(2):
            for c in range(4):
                P.mm(ps[0][0:64, :], wqb[:, c, 192 * h + 128:192 * h + 192], qlatnT[:, c, 512 * g:512 * g + 512],
                     c == 0, c == 3, r=wqb_k + ("qlatnT",), w=("ps0",))
            for c in range(4):
                P.mm(ps[1][0:64, :], wqrot[:, c, 64 * h:64 * h + 64], qlatnT[:, c, 512 * g:512 * g + 512],
                     c == 0, c == 3, r=wqrot_k + ("qlatnT",), w=("ps1",))
            P.tt("dve", v3(t1_t[0:64, :]), v3(ps[0][0:64, :]), own4(cos2, g), ALU.mult, r=("ps0", "cos2"), w=("t1_t",))
            P.tt("dve", v3(e_t[0:64, :]), v3(ps[1][0:64, :]), own4(sin2, g), ALU.mult, r=("ps1", "sin2"), w=("e_t",))
            P.tt("pool", qpeT[:, 512 * g:512 * g + 512], t1_t[0:64, :], e_t[0:64, :], ALU.add, r=("t1_t", "e_t"), w=("qpeT",))
        if h == 0:
            chk("mproj_k", kT, 2048)
            chk("mproj_q", qT, 1024)
            chk("mproj_qpe", qpeT, 1024, 64)
            chk("mproj_v", vt[:, :, 0:128], 2048)
        for j in range(NS):
            attn_slot("mla", h, j)
        if h == 0:
            chk("mla0", mixT[:, 8, :], 1024)

    P.fence()

    acc = A.f32(O_H0T, NS * D).rearrange("p (j d) -> p j d", j=NS)
    acck = lambda j: (f"acc{j}",)
    h1bf = A.bf(O_MIX, NS * D).rearrange("p (j d) -> p j d", j=NS)
    gbc = A.f32(O_Z, D)
    bbc = A.f32(O_Z + 8192, D)
    xt2 = A.f32(O_Z + 16384, D)
    wso = [A.f32(O_Z + 24576 + 16384 * i, 8 * 512).rearrange("p (c n) -> p c n", c=8) for i in range(2)]
    wbo = [A.bf(O_Z + 57344 + 8192 * i, 8 * 512).rearrange("p (c n) -> p c n", c=8) for i in range(2)]
    h1T = A.f32(O_Z + 75776, 16 * 128).rearrange("p (c t) -> p c t", c=16)
    wr = A.f32(O_Z + 83968, 16 * E).rearrange("p (c n) -> p c n", c=16)
    rt = A.f32(O_Z + 86016, 8 * E)
    lg = rt[:, 0:32]
    m8 = rt[:, 32:40]
    negm = rt[:, 40:41]
    den = rt[:, 41:42]
    ex = rt[:, 64:96]
    brt = rt[:, 96:128]
    Gt = A.f32(O_Z + 90112, NS * E).rearrange("p (j e) -> p j e", j=NS)
    rankm = A.f32(O_Z + 91136, NS * E).rearrange("p (j e) -> p j e", j=NS)
    routed = A.f32(O_Z + 92160, NS * E).rearrange("p (j e) -> p j e", j=NS)

    P.dma("gbc", gbc, ln_in_g.partition_broadcast(128), r=(), w=("gbc",))
    P.dma("bbc", bbc, ln_in_b.partition_broadcast(128), r=(), w=("bbc",))
    P.add("act", lambda e: e.mul(gbc, gbc, ALPHA), r=("gbc",), w=("gbc",))
    P.add("act", lambda e: e.mul(bbc, bbc, ALPHA), r=("bbc",), w=("bbc",))
    for j in range(NS):
        P.dma("xt2", xt2, xp[2 * j], r=(), w=("xt2",))
        ln_stats(xt2, "xt2")
        P.act(xt2, xt2, AF.Identity, r=("xt2", "rstd", "nmr"), w=("xt2",), bias=stat[:, 28:29], scale=stat[:, 27:28])
        P.tt("dve", acc[:, j, :], xt2, gbc, ALU.mult, r=("xt2", "gbc"), w=acck(j))
        P.tt("pool", acc[:, j, :], acc[:, j, :], bbc, ALU.add, r=acck(j) + ("bbc",), w=acck(j))

    P.act(stat[:, 48:64], stat[:, 32:48], AF.Sqrt, r=("ss", "eps"), w=("rstdo",), bias=eps_t[:, 1:2], scale=1.0 / 1024.0)
    P.add("dve", lambda e: e.reciprocal(stat[:, 48:64], stat[:, 48:64]), r=("rstdo",), w=("rstdo",))
    oi = 0
    for dblk in range(4):
        for half in range(2):
            si = oi % 2
            oi += 1
            src = w_o[1024 * half:1024 * half + 1024, 512 * dblk:512 * dblk + 512].rearrange("(c p) n -> p c n", p=128)
            P.dma(f"wso{si}", wso[si], src, r=(), w=(f"wso{si}",))
            for c in range(8):
                P.ts("pool", wbo[si][:, c, :], wso[si][:, c, :], onorm[:, 8 * half + c:8 * half + c + 1], None, ALU.mult, None,
                     r=(f"wso{si}", "onorm"), w=(f"wbo{si}",))
            for j in range(NS):
                pb = 6 + j % 2
                pkeys = ("ps6",) if pb == 6 else ("pst0", "pst1")
                for c in range(8):
                    P.mm(ps[pb], mixT[:, 8 * half + c, 128 * j:128 * j + 128], wbo[si][:, c, :], c == 0, c == 7,
                         r=(f"mixT{8 * half + c}", f"wbo{si}"), w=pkeys)
                dst = acc[:, j, 512 * dblk:512 * dblk + 512]
                P.stt("dve", dst, ps[pb], stat[:, 48 + 8 * half + j:48 + 8 * half + j + 1], dst, ALU.mult, ALU.add,
                      r=pkeys + acck(j) + ("rstdo",), w=acck(j))

    P.fence()
    P.dma("gbc", gbc, ln_mix_g.partition_broadcast(128), r=(), w=("gbc",))
    P.dma("bbc", bbc, ln_mix_b.partition_broadcast(128), r=(), w=("bbc",))
    P.dma("wr", wr, w_router.rearrange("(c p) n -> p c n", p=128), r=(), w=("wr",))
    P.dma("brt", brt, b_router.partition_broadcast(128), r=(), w=("brt",))
    for j in range(NS):
        ln_stats(acc[:, j, :], f"acc{j}")
        P.act(xt2, acc[:, j, :], AF.Identity, r=acck(j) + ("rstd", "nmr"), w=("xt2",), bias=stat[:, 28:29], scale=stat[:, 27:28])
        P.tt("dve", xt2, xt2, gbc, ALU.mult, r=("xt2", "gbc"), w=("xt2",))
        P.tt("pool", acc[:, j, :], xt2, bbc, ALU.add, r=("xt2", "bbc"), w=acck(j))
        P.cp("pool", h1bf[:, j, :], acc[:, j, :], r=acck(j), w=(f"h1bf{j}",))
        for c4 in range(4):
            pb = 6 + c4 % 2
            pkeys = ("ps6",) if pb == 6 else ("pst0", "pst1")
            for i in range(4):
                c = 4 * c4 + i
                P.tr(ps[pb][:, 128 * i:128 * i + 128], acc[:, j, 128 * c:128 * c + 128], ident_f, r=acck(j) + ("ident_f",), w=pkeys)
            P.cp("act", h1T[:, 4 * c4:4 * c4 + 4, :], ps[pb].rearrange("p (i t) -> p i t", i=4), r=pkeys, w=("h1T",))
        for c in range(16):
            P.mm(ps[4][:, 0:E], h1T[:, c, :], wr[:, c, :], c == 0, c == 15, r=("h1T", "wr"), w=("ps4",))
        P.tt("dve", lg, ps[4][:, 0:E], brt, ALU.add, r=("ps4", "brt"), w=("lg",))
        P.add("dve", lambda e: e.max(m8, lg), r=("lg",), w=("m8",))
        P.ts("dve", routed[:, j, :], lg, m8[:, 3:4], None, ALU.is_ge, None, r=("lg", "m8"), w=("routed",))
        P.ts("dve", negm, m8[:, 0:1], -1.0, None, ALU.mult, None, r=("m8",), w=("negm",))
        P.act(ex, lg, AF.Exp, r=("lg", "negm"), w=("ex",), bias=negm, scale=1.0)
        P.tt("dve", ex, ex, routed[:, j, :], ALU.mult, r=("ex", "routed"), w=("ex",))
        P.add("dve", lambda e: e.reduce_sum(den, ex, AX.X), r=("ex",), w=("den",))
        P.add("dve", lambda e: e.reciprocal(den, den), r=("den",), w=("den",))
        P.ts("dve", Gt[:, j, :], ex, den, None, ALU.mult, None, r=("ex", "den"), w=("Gt",))
        for j2 in range(j + 1):
            P.mm(ps[5][:, 0:E], (tril_f if j2 == j else ones_f), routed[:, j2, :], j2 == 0, j2 == j,
                 r=("tril_f", "ones_f", "routed"), w=("ps5",))
        P.stt("dve", rankm[:, j, :], ps[5][:, 0:E], 1.0, routed[:, j, :], ALU.add, ALU.mult, r=("ps5", "routed"), w=("rankm",))
        P.ts("dve", rankm[:, j, :], rankm[:, j, :], -1.0, None, ALU.add, None, r=("rankm",), w=("rankm",))
        P.add("act", (lambda j: lambda e: e.mul(acc[:, j, :], acc[:, j, :], ALPHA))(j), r=acck(j), w=acck(j))

    if stage == "A":
        P.fence()
        for j in range(NS):
            P.dma(f"out{j % 2}", out[j], acc[:, j, :], r=acck(j), w=())
        P.emit(stack)
        return

    P.fence()

    wgu_t = w_gu
    wdn_t = w_dn
    wsM = [A.f32(O_Z + 8192 * i, 2048).rearrange("p (c n) -> p c n", c=16) for i in range(2)]
    wbM = [A.bf(O_Z + 16384 + 4096 * i, 2048).rearrange("p (c n) -> p c n", c=16) for i in range(3)]
    Sel = A.bf(O_Z + 28672, NS * CAP).rearrange("p (j r) -> p j r", j=NS)
    SelG = A.bf(O_Z + 32768, NS * CAP).rearrange("p (j r) -> p j r", j=NS)
    SelGT = A.bf(O_Z + 36864, 2 * 1024).rearrange("p (c t) -> p c t", c=2)
    xT = A.bf(O_Z + 40960, 16 * CAP).rearrange("p (c r) -> p c r", c=16)
    gs = A.bf(O_Z + 49152, 16 * CAP).rearrange("p (c r) -> p c r", c=16)
    actT = A.bf(O_Z + 57344, 16 * CAP).rearrange("p (c r) -> p c r", c=16)
    yb = A.bf(O_Z + 65536, 2 * D).rearrange("p (c d) -> p c d", c=2)
    bdn = A.f32(O_Z + 73728, D)
    gtmp = A.f32(O_Z + 81920, CAP)
    sgt = A.f32(O_Z + 82944, CAP)
    utmp = A.f32(O_Z + 83968, CAP)
    bgu_t = A.f32(O_Z + 84992, n_exp * 32)
    P.dma("bgu", bgu_t, bgu, r=(), w=("bgu",))

    mcnt = [0, 0, 0]
    cast_pat = ("pool", "act", "pool", "act", "pool", "dve")

    def load_wM(src2):
        si = mcnt[0] % 2
        bi = mcnt[1] % 3
        mcnt[0] += 1
        mcnt[1] += 1
        P.dma(f"wsM{si}", wsM[si], src2.rearrange("p (c n) -> p c n", c=16), r=(), w=(f"wsM{si}",))
        eng = cast_pat[mcnt[2] % len(cast_pat)]
        mcnt[2] += 1
        P.cp(eng, wbM[bi], wsM[si], r=(f"wsM{si}",), w=(f"wbM{bi}",))
        return wbM[bi], f"wbM{bi}"

    for e_i in range(n_exp):
        for j in range(NS):
            P.ts("dve", Sel[:, j, :], iota, rankm[:, j, e_i:e_i + 1], None, ALU.is_equal, None, r=("iota", "rankm"), w=("Sel",))
            P.ts("pool", SelG[:, j, :], iota, rankm[:, j, e_i:e_i + 1], Gt[:, j, e_i:e_i + 1], ALU.is_equal, ALU.mult,
                 r=("iota", "rankm", "Gt"), w=("SelG",))
        P.dma("bdn", bdn, b_dn[e_i].partition_broadcast(128), r=(), w=("bdn",))
        for d2 in range(8):
            pb = d2 % 2
            for i in range(2):
                dc = 2 * d2 + i
                for j in range(NS):
                    P.mm(ps[pb][:, CAP * i:CAP * i + CAP], h1bf[:, j, 128 * dc:128 * dc + 128], Sel[:, j, :], j == 0, j == NS - 1,
                         r=(f"h1bf{j}", "Sel"), w=(f"ps{pb}",))
            P.cp("act", xT[:, 2 * d2:2 * d2 + 2, :], ps[pb].rearrange("p (i r) -> p i r", i=2), r=(f"ps{pb}",), w=("xT",))
        for rc in range(2):
            for j4 in range(2):
                half = (2 * rc + j4) % 2
                pst = psb[7 - half][:, 0:512]
                for i in range(4):
                    j = 4 * j4 + i
                    P.tr(pst[:, 128 * i:128 * i + 128], SelG[:, j, 128 * rc:128 * rc + 128], ident_b, r=("SelG", "ident_b"),
                         w=(bkey[half],))
                P.cp("act", SelGT[:, rc, 512 * j4:512 * j4 + 512], pst, r=(bkey[half],), w=("SelGT",))
        for fb in range(32):
            wt, wk = load_wM(wgu_t[e_i, fb])
            pb = 2 + fb % 2
            for c in range(16):
                P.mm(ps[pb][:, 0:CAP], wt[:, c, :], xT[:, c, :], c == 0, c == 15, r=(wk, "xT"), w=(f"ps{pb}",))
            bcol = bgu_t[:, e_i * 32 + fb:e_i * 32 + fb + 1]
            if fb < 16:
                P.ts("dve", gtmp, ps[pb][:, 0:CAP], bcol, 7.0, ALU.add, ALU.min, r=(f"ps{pb}", "bgu"), w=("gtmp",))
                P.act(sgt, gtmp, AF.Sigmoid, r=("gtmp",), w=("sgt",), scale=1.702)
                P.tt("pool", gs[:, fb, :], gtmp, sgt, ALU.mult, r=("gtmp", "sgt"), w=("gs",))
            else:
                P.act(utmp, ps[pb][:, 0:CAP], AF.Identity, r=(f"ps{pb}", "bgu"), w=("utmp",), bias=bcol, scale=1.0)
                P.ts("dve", utmp, utmp, -7.0, 7.0, ALU.max, ALU.min, r=("utmp",), w=("utmp",))
                P.stt("dve", actT[:, fb - 16, :], utmp, 1.0, gs[:, fb - 16, :], ALU.add, ALU.mult, r=("utmp", "gs"), w=("actT",))
        for db in range(16):
            wt, wk = load_wM(wdn_t[e_i, db])
            q = db // 4
            for rc in range(2):
                pb = 4 + rc
                for c in range(16):
                    P.mm(ps[pb][:, 128 * (db % 4):128 * (db % 4) + 128], actT[:, c, 128 * rc:128 * rc + 128], wt[:, c, :],
                         c == 0, c == 15, r=(wk, "actT"), w=(f"ps{pb}",))
            if db % 4 == 3:
                for rc in range(2):
                    P.tt("dve", yb[:, rc, 512 * q:512 * q + 512], ps[4 + rc], bdn[:, 512 * q:512 * q + 512], ALU.add,
                         r=(f"ps{4 + rc}", "bdn"), w=("yb",))
        for j in range(NS):
            for dq in range(4):
                pb = dq % 2
                for rc in range(2):
                    P.mm(ps[pb], SelGT[:, rc, 128 * j:128 * j + 128], yb[:, rc, 512 * dq:512 * dq + 512], rc == 0, rc == 1,
                         r=("SelGT", "yb"), w=(f"ps{pb}",))
                dst = acc[:, j, 512 * dq:512 * dq + 512]
                P.tt("dve", dst, dst, ps[pb], ALU.add, r=acck(j) + (f"ps{pb}",), w=acck(j))

    P.fence()

    P.dma("gbc", gbc, ln_ffn_g.partition_broadcast(128), r=(), w=("gbc",))
    P.dma("bbc", bbc, ln_ffn_b.partition_broadcast(128), r=(), w=("bbc",))
    fb2 = [A.f32(O_Z + 16384 + 8192 * i, D) for i in range(2)]
    for j in range(NS):
        fo = fb2[j % 2]
        ln_stats(acc[:, j, :], f"acc{j}")
        P.act(fo, acc[:, j, :], AF.Identity, r=acck(j) + ("rstd", "nmr"), w=(f"fo{j % 2}",), bias=stat[:, 28:29], scale=stat[:, 27:28])
        P.tt("dve", fo, fo, gbc, ALU.mult, r=(f"fo{j % 2}", "gbc"), w=(f"fo{j % 2}",))
        P.tt("pool", fo, fo, bbc, ALU.add, r=(f"fo{j % 2}", "bbc"), w=(f"fo{j % 2}",))
        P.dma(f"out{j % 2}", out[j], fo, r=(f"fo{j % 2}",), w=())
    P.emit(stack)


def _host_consts(g):
    own = QA if g == 0 else QB
    oth = QB if g == 0 else QA
    k = np.arange(128)
    ident = np.eye(128, dtype=np.float32)
    triu = (k[:, None] > k[None, :]).astype(np.float32)
    tril = (k[:, None] < k[None, :]).astype(np.float32)
    msb = (k[:, None] < k[None, :]).astype(np.float32)
    mmla = ((k[:, None] // 64) <= (k[None, :] // 64)).astype(np.float32)
    pairf = np.zeros((128, NS, 2, 128), np.float32)
    small = np.zeros((128, 64), np.float32)
    for i in range(NS):
        f = 1.0 if oth[i] > own[i] else 0.0
        pairf[:, i, 0, :] = f
        pairf[:, i, 1, :] = 1.0 - f
        small[:, i] = 1.0 - f
    inv_freq = (10000.0 ** (-(np.arange(32, dtype=np.float32) * 2.0 / 64.0))).astype(np.float32)
    small[:64, 8] = np.concatenate([inv_freq, inv_freq])
    iota = np.broadcast_to(np.arange(CAP, dtype=np.float32)[None, :], (128, CAP)).copy()
    return dict(c_ident=ident, c_triu=triu, c_tril=tril, c_msb=msb, c_mmla=mmla,
                c_pairf=pairf.reshape(128, -1), c_iota=iota), small


def _pc(v, nchunk):
    return np.ascontiguousarray(np.asarray(v, np.float32).reshape(nchunk, 128).T)


def _prepare(inputs, n_exp=E):
    f = lambda k: np.asarray(inputs[k])
    x = f("x").astype(np.float32, copy=False)
    pos = f("positions").astype(np.int32, copy=False)
    wgu = f("w_gate_up")[0][:n_exp]
    wdn = f("w_down")[0][:n_exp]
    wgu_t = np.ascontiguousarray(wgu.reshape(n_exp, 16, 128, 32, 128).transpose(0, 3, 2, 1, 4)).reshape(n_exp, 32, 128, 2048)
    wdn_t = np.ascontiguousarray(wdn.reshape(n_exp, 16, 128, 16, 128).transpose(0, 3, 2, 1, 4)).reshape(n_exp, 16, 128, 2048)
    bgu = np.ascontiguousarray(f("b_gate_up")[0][:n_exp].reshape(n_exp, 32, 128).transpose(2, 0, 1)).reshape(128, n_exp * 32)
    shared = dict(
        w_in=np.ascontiguousarray(f("w_in")[0]), w_q_b=np.ascontiguousarray(f("w_q_b")[0]),
        w_kv_b=np.ascontiguousarray(f("w_kv_b")[0]), w_o=np.ascontiguousarray(f("w_o")[0]),
        ln_in_g=f("ln_in_g"), ln_in_b=f("ln_in_b"), ln_mix_g=f("ln_mix_g")[0], ln_mix_b=f("ln_mix_b")[0],
        ln_ffn_g=f("ln_ffn_g")[0], ln_ffn_b=f("ln_ffn_b")[0], w_router=np.ascontiguousarray(f("w_router")[0]),
        b_router=f("b_router")[0], w_gu=wgu_t, bgu=bgu, w_dn=wdn_t, b_dn=np.ascontiguousarray(f("b_down")[0][:n_exp]),
        c_onorm=_pc(np.concatenate([f("sb_out_norm")[0], f("mla_out_norm")[0]]), 16),
    )
    shared = {k: np.ascontiguousarray(v, dtype=np.float32) for k, v in shared.items()}
    in_maps = []
    for c in range(8):
        b, g = c // 2, c % 2
        own = QA if g == 0 else QB
        oth = QB if g == 0 else QA
        order = []
        for i in range(NS):
            order += [own[i], oth[i]]
        xb = x[b].reshape(NB, 128, D)[order]
        pb = pos[b].reshape(NB, 128)[order].reshape(-1)
        consts, small = _host_consts(g)
        small = small.copy()
        small[:, 16:32] = _pc(f("ln_in_g"), 16)
        small[:, 32:48] = _pc(f("ln_in_b"), 16)
        small[:, 48:52] = _pc(f("q_a_norm")[0], 4)
        small[:, 52:54] = _pc(f("kv_a_norm")[0], 2)
        m = dict(shared)
        m.update(consts)
        m["c_small"] = small
        m["xp"] = np.ascontiguousarray(xb)
        m["posb"] = np.ascontiguousarray(pb.astype(np.int32))
        in_maps.append(m)
    return in_maps


def _assemble(results):
    out = np.zeros((4, S, D), np.float32)
    for c in range(8):
        b, g = c // 2, c % 2
        own = QA if g == 0 else QB
        o = np.asarray(results[c]["out"], np.float32)
        for j in range(NS):
            out[b, own[j] * 128:(own[j] + 1) * 128, :] = o[j]
    return out


def kernel(**inputs):
    from contextlib import ExitStack
    in_maps = _prepare(inputs)
    nc = bass.Bass("TRN2", target_bir_lowering=False)
    with ExitStack() as stack:
        build(nc, stack)
    res = run_bass_kernel_spmd(nc, in_maps, core_ids=list(range(8)))
    return _assemble(res.results)
```

```python
import numpy as np
import concourse.bass as bass
import concourse.mybir as mybir
from concourse.bass_utils import run_bass_kernel_spmd

F32 = mybir.dt.float32
BF16 = mybir.dt.bfloat16
I32 = mybir.dt.int32
AF = mybir.ActivationFunctionType
ALU = mybir.AluOpType
AX = mybir.AxisListType

D = 2048
S = 2048
NB = 16
NS = 8
H = 8
INP = 3904
E = 32
DFF = 2048
CAP = 256
ALPHA = 2.0 ** 0.25
LN_EPS = 1e-5
RMS_EPS = 1e-6
SEG = 12000
import os as _os
DEBUG_EMIT = bool(_os.environ.get("DEBUG_EMIT"))

QA = [0, 3, 4, 7, 8, 11, 12, 15]
QB = [1, 2, 5, 6, 9, 10, 13, 14]


class Prog:
    ENG = ("pe", "act", "dve", "pool", "sp")

    def __init__(self, nc):
        self.nc = nc
        self.ops = []
        self.lastw = {}
        self.readers = {}
        self.dma_cnt = {}

    def add(self, eng, fn, r=(), w=(), dma=None):
        i = len(self.ops)
        deps = set()
        for b in list(r) + list(w):
            if b in self.lastw:
                deps.add(self.lastw[b])
        for b in w:
            for x in self.readers.get(b, ()):
                deps.add(x)
        for b in r:
            if b.startswith("ps"):
                for x in self.readers.get(b, ()):
                    if self.ops[x]["eng"] != eng:
                        deps.add(x)
        deps.discard(i)
        op = dict(id=i, eng=eng, fn=fn, deps=deps, dma=dma, sig=False, r=tuple(r), w=tuple(w))
        if dma is not None:
            self.dma_cnt[dma] = self.dma_cnt.get(dma, 0) + 1
            op["dval"] = 16 * self.dma_cnt[dma]
        self.ops.append(op)
        for b in w:
            self.lastw[b] = i
            self.readers[b] = []
        for b in r:
            if b not in w:
                self.readers.setdefault(b, []).append(i)
        return i

    def emit(self, stack):
        nc = self.nc
        ops = self.ops
        for op in ops:
            nd = set()
            for d in op["deps"]:
                dop = ops[d]
                if op["fn"] is None:
                    if dop["dma"] is None and dop["eng"] == op["eng"]:
                        continue
                    nd.add(d)
                    continue
                if dop["dma"] is None and dop["eng"] == op["eng"] and op["dma"] is None:
                    if op["eng"] == "pe":
                        continue
                    wr = set(dop["w"])
                    if not (wr & set(op["r"])) and not (wr & set(op["w"])):
                        continue
                nd.add(d)
            op["deps"] = nd
            for d in nd:
                ops[d]["sig"] = True
        cnt = {e: 0 for e in self.ENG}
        for op in ops:
            if op["dma"] is None and op["sig"]:
                cnt[op["eng"]] += 1
                op["sidx"] = cnt[op["eng"]]
        nseg = {e: (cnt[e] + SEG - 1) // SEG for e in self.ENG}
        sems = {}
        for e in self.ENG:
            for k in range(max(1, nseg[e])):
                sems[(e, k)] = stack.enter_context(nc.semaphore(f"s_{e}_{k}"))
        dsem = {}
        for name in self.dma_cnt:
            dsem[name] = stack.enter_context(nc.semaphore(f"d_{name}"))

        def token(op):
            if op["dma"] is not None:
                return ("d", op["dma"]), dsem[op["dma"]], op["dval"]
            k = (op["sidx"] - 1) // SEG
            return (op["eng"], k), sems[(op["eng"], k)], (op["sidx"] - 1) % SEG + 1

        out_dmas = [op for op in ops if op.get("dma") and op["dma"].startswith("out")]
        block = stack.enter_context(nc.Block())
        handles = {"pe": block.tensor, "act": block.scalar, "dve": block.vector,
                   "pool": block.gpsimd, "sp": block.sync}

        def make(engname):
            def body(eng):
                known = {}
                for op in ops:
                    if op["eng"] != engname:
                        continue
                    need = {}
                    for d in op["deps"]:
                        key, sem, val = token(ops[d])
                        if known.get(key, 0) >= val:
                            continue
                        if key not in need or need[key][1] < val:
                            need[key] = (sem, val)
                    for key, (sem, val) in need.items():
                        eng.wait_ge(sem, val)
                        known[key] = val
                        if DEBUG_EMIT:
                            print(f"  [{engname}] wait {key} >= {val}")
                    if DEBUG_EMIT:
                        tk = token(op) if (op["dma"] is not None or op["sig"]) else None
                        print(f"  [{engname}] op{op['id']} r={op['r']} w={op['w']} sig={tk and (tk[0], tk[2])}")
                    if op["fn"] is None:
                        continue
                    ins = op["fn"](eng)
                    if op["dma"] is not None:
                        ins.then_inc(dsem[op["dma"]], 16)
                    elif op["sig"]:
                        _, sem, _ = token(op)
                        ins.then_inc(sem, 1)
                if engname == "sp":
                    last = {}
                    for op in out_dmas:
                        last[op["dma"]] = op["dval"]
                    for name, val in last.items():
                        eng.wait_ge(dsem[name], val)
            return body

        for e in self.ENG:
            handles[e](make(e))

    def fence(self):
        last = {}
        for op in self.ops:
            if op["fn"] is None:
                continue
            k = ("d", op["dma"]) if op["dma"] is not None else op["eng"]
            last[k] = op["id"]
        ids = set(last.values())
        for e in self.ENG:
            i = len(self.ops)
            self.ops.append(dict(id=i, eng=e, fn=None, deps=set(ids), dma=None, sig=False, r=(), w=()))
        self.lastw = {}
        self.readers = {}

    def mm(self, out, lhsT, rhs, start, stop, r, w):
        return self.add("pe", lambda e: e.matmul(out, lhsT, rhs, start=start, stop=stop), r=r, w=w)

    def tr(self, out, in_, ident, r, w):
        return self.add("pe", lambda e: e.transpose(out, in_, ident), r=r, w=w)

    def act(self, out, in_, func, r, w, bias=None, scale=None, accum_out=None, eng="act"):
        kw = {}
        if bias is not None:
            kw["bias"] = bias
        if scale is not None:
            kw["scale"] = scale
        if accum_out is not None:
            kw["accum_out"] = accum_out
        return self.add(eng, lambda e: e.activation(out, in_, func, **kw), r=r, w=w)

    def ts(self, eng, out, in0, s1, s2, op0, op1, r, w, accum_out=None):
        if op1 is None:
            return self.add(eng, lambda e: e.tensor_scalar(out, in0, s1, None, op0), r=r, w=w)
        if accum_out is not None:
            return self.add(eng, lambda e: e.tensor_scalar(out, in0, s1, s2, op0, op1, accum_out=accum_out), r=r, w=w)
        return self.add(eng, lambda e: e.tensor_scalar(out, in0, s1, s2, op0, op1), r=r, w=w)

    def tt(self, eng, out, in0, in1, op, r, w):
        return self.add(eng, lambda e: e.tensor_tensor(out, in0, in1, op), r=r, w=w)

    def stt(self, eng, out, in0, scalar, in1, op0, op1, r, w):
        return self.add(eng, lambda e: e.scalar_tensor_tensor(out, in0, scalar, in1, op0, op1), r=r, w=w)

    def cp(self, eng, out, in_, r, w):
        if eng == "act":
            return self.add(eng, lambda e: e.copy(out, in_), r=r, w=w)
        return self.add(eng, lambda e: e.tensor_copy(out, in_), r=r, w=w)

    def ms(self, eng, ap, val, w):
        return self.add(eng, lambda e: e.memset(ap, val), r=(), w=w)

    def dma(self, stream, out, in_, r, w, eng="sp"):
        return self.add(eng, lambda e: e.dma_start(out=out, in_=in_), r=r, w=w, dma=stream)


class Arena:
    def __init__(self, nc, stack, nbytes):
        self.t = stack.enter_context(nc.sbuf_tensor("arena", [128, nbytes // 4], F32))
        self.f = self.t[:]
        self.b = self.f.bitcast(BF16)
        self.i = self.f.bitcast(I32)

    def f32(self, off, n, p=128):
        return self.f[0:p, off // 4: off // 4 + n]

    def bf(self, off, n, p=128):
        return self.b[0:p, off // 2: off // 2 + n]

    def i32(self, off, n, p=128):
        return self.i[0:p, off // 4: off // 4 + n]


ARENA_BYTES = 210944
O_H0T = 10240
O_MIX = 75776
O_Z = 108544

TWO_PI = 2.0 * np.pi
CW1 = float(np.float32(6.28125))
CW2 = float(np.float32(TWO_PI - 6.28125))


class _Done(Exception):
    pass


def build(nc, stack, n_exp=E, stage="full", stop=None):
    P = Prog(nc)
    try:
        _build(nc, stack, P, n_exp, stage, stop)
    except _Done:
        pass
    return P


def _build(nc, stack, P, n_exp, stage, stop):
    A = Arena(nc, stack, ARENA_BYTES)

    def dram(name, shape, dt=F32, kind="ExternalInput"):
        return nc.dram_tensor(name, list(shape), dt, kind=kind).ap()

    xp = dram("xp", [NB, 128, D])
    posb = dram("posb", [S], I32)
    c_ident = dram("c_ident", [128, 128])
    c_triu = dram("c_triu", [128, 128])
    c_tril = dram("c_tril", [128, 128])
    c_msb = dram("c_msb", [128, 128])
    c_mmla = dram("c_mmla", [128, 128])
    c_pairf = dram("c_pairf", [128, NS * 2 * 128])
    c_small = dram("c_small", [128, 64])
    c_onorm = dram("c_onorm", [128, 16])
    c_iota = dram("c_iota", [128, CAP])
    w_in = dram("w_in", [D, INP])
    w_q_b = dram("w_q_b", [512, 1536])
    w_kv_b = dram("w_kv_b", [256, 2048])
    w_o = dram("w_o", [D, D])
    ln_in_g = dram("ln_in_g", [D])
    ln_in_b = dram("ln_in_b", [D])
    ln_mix_g = dram("ln_mix_g", [D])
    ln_mix_b = dram("ln_mix_b", [D])
    ln_ffn_g = dram("ln_ffn_g", [D])
    ln_ffn_b = dram("ln_ffn_b", [D])
    w_router = dram("w_router", [D, E])
    b_router = dram("b_router", [E])
    w_gu = dram("w_gu", [n_exp, 32, 128, 2048])
    bgu = dram("bgu", [128, n_exp * 32])
    w_dn = dram("w_dn", [n_exp, 16, 128, 2048])
    b_dn = dram("b_dn", [n_exp, D])
    out = dram("out", [NS, 128, D], kind="ExternalOutput")

    ps = [stack.enter_context(nc.psum_tensor(f"ps{i}", [128, 512], F32))[:] for i in range(8)]

    def chk(name, ap=None, n=0, p=128):
        if stop != name:
            return
        P.fence()
        dbg = A.f32(O_Z + 82048, D)
        if ap is not None:
            P.cp("dve", dbg[0:p, 0:n], ap, r=(), w=("dbg",))
            P.dma("out0", out[0][0:p, 0:n], dbg[0:p, 0:n], r=("dbg",), w=())
        else:
            P.ms("dve", dbg[:, 0:8], 1.0, w=("dbg",))
            P.dma("out0", out[0][:, 0:8], dbg[:, 0:8], r=("dbg",), w=())
        P.emit(stack)
        raise _Done()

    psb = [p.bitcast(BF16) for p in ps]

    ident_f = A.f32(0, 128)
    ident_b = A.bf(512, 128)
    triu_b = A.bf(768, 128)
    ones_b = A.bf(1024, 128)
    msb_b = A.bf(1280, 128)
    mmla_b = A.bf(1536, 128)
    pairf_b = A.bf(1792, NS * 2 * 128)
    small = A.f32(5888, 64)
    vis = small[:, 0:8]
    invf = small[0:64, 8:9]
    lng = small[:, 16:32]
    lnb = small[:, 32:48]
    qan = small[:, 48:52]
    kvan = small[:, 52:54]
    onorm = A.f32(6144, 16)
    iota = A.f32(6208, CAP)
    tril_f = A.f32(7232, 128)
    ones_f = A.f32(7744, 128)
    stat = A.f32(8256, 496)

    tmpc = A.f32(O_Z + 49280, 128 * NS * 2)

    def load_const(name, dst_b, src, n, key):
        P.dma("c0", tmpc[:, 0:n], src, r=(), w=("tmpc",))
        P.cp("dve", dst_b, tmpc[:, 0:n], r=("tmpc",), w=(key,))

    P.dma("c1", ident_f, c_ident, r=(), w=("ident_f",))
    P.dma("c2", small, c_small, r=(), w=("small",))
    P.dma("c3", onorm, c_onorm, r=(), w=("onorm",))
    P.dma("c4", iota, c_iota, r=(), w=("iota",))
    P.dma("c5", tril_f, c_tril, r=(), w=("tril_f",))
    P.cp("dve", ident_b, ident_f, r=("ident_f",), w=("ident_b",))
    load_const("triu", triu_b, c_triu, 128, "triu_b")
    load_const("msb", msb_b, c_msb, 128, "msb_b")
    load_const("mmla", mmla_b, c_mmla, 128, "mmla_b")
    load_const("pairf", pairf_b, c_pairf, NS * 2 * 128, "pairf_b")
    P.ms("dve", ones_b, 1.0, w=("ones_b",))
    P.ms("dve", ones_f, 1.0, w=("ones_f",))

    chk("consts", triu_b, 128)
    h0T = A.bf(O_H0T, 16 * S).rearrange("p (c t) -> p c t", c=16)
    mixT = A.bf(O_MIX, 16 * 1024).rearrange("p (c t) -> p c t", c=16)
    cos2 = A.f32(O_Z, S, p=64)
    sin2 = A.f32(O_Z + 8192, S, p=64)
    kropeT = A.bf(O_Z + 16384, S, p=64)
    qlatnT = A.bf(O_Z + 20480, 4 * 1024).rearrange("p (c t) -> p c t", c=4)
    kvlatnT = A.bf(O_Z + 28672, 2 * S).rearrange("p (c t) -> p c t", c=2)
    kT = A.bf(O_Z + 36864, S)
    vt = A.bf(O_Z + 40960, 16 * 132).rearrange("p (b d) -> p b d", b=16)
    qT = A.bf(O_Z + 45184, 1024)
    qpeT = A.bf(O_Z + 47232, 1024, p=64)
    ws = [A.f32(O_Z + 49280 + 8192 * i, 16 * 128).rearrange("p (c n) -> p c n", c=16) for i in range(2)]
    wb = [A.bf(O_Z + 65664 + 4096 * i, 16 * 128).rearrange("p (c n) -> p c n", c=16) for i in range(4)]
    xt = A.f32(O_Z + 82048, D)
    xn = A.bf(O_Z + 90240, D)
    e_t = A.f32(O_Z + 94336, 512)
    sp_t = A.bf(O_Z + 96384, 512)
    t1_t = A.f32(O_Z + 97408, 512)
    w_t = A.bf(O_Z + 99456, 512)
    carry = [A.f32(O_Z + 100480 + 512 * i, 128) for i in range(2)]
    osb = A.bf(O_Z + 101504, 128)
    rinv = A.f32(O_Z + 101760, 1)

    own_tok = lambda ap3, c, g: ap3[:, c, :].rearrange("p (b two t) -> p b two t", two=2, t=128)[:, 4 * g:4 * g + 4, 0, :]

    r_i = A.i32(O_Z + 49280, S, p=64)
    r_a = A.f32(O_Z + 49280 + 8192, S, p=64)
    r_k = A.f32(O_Z + 49280 + 16384, S, p=64)
    r_r = A.f32(O_Z + 49280 + 24576, S, p=64)
    r_m = A.f32(O_Z + 49280 + 32768, S, p=64)
    P.dma("c6", r_i, posb.partition_broadcast(64), r=("tmpc",), w=("r_i", "tmpc"))
    P.cp("dve", r_a, r_i, r=("r_i", "small"), w=("r_a",))
    P.ts("dve", r_a, r_a, invf, None, ALU.mult, None, r=("r_a", "small"), w=("r_a",))
    P.ts("dve", r_i, r_a, 1.0 / TWO_PI, None, ALU.mult, None, r=("r_a",), w=("r_i",))
    P.cp("dve", r_k, r_i, r=("r_i",), w=("r_k",))
    P.stt("dve", r_r, r_k, -CW1, r_a, ALU.mult, ALU.add, r=("r_k", "r_a"), w=("r_r",))
    P.stt("dve", r_r, r_k, -CW2, r_r, ALU.mult, ALU.add, r=("r_k", "r_r"), w=("r_r",))

    def wrap(x):
        P.ts("dve", r_m, x, float(np.pi), -TWO_PI, ALU.is_gt, ALU.mult, r=("r_r", "r_a"), w=("r_m",))
        P.tt("dve", x, x, r_m, ALU.add, r=("r_m",), w=("r_r", "r_a"))
        P.ts("dve", r_m, x, -float(np.pi), TWO_PI, ALU.is_lt, ALU.mult, r=("r_r", "r_a"), w=("r_m",))
        P.tt("dve", x, x, r_m, ALU.add, r=("r_m",), w=("r_r", "r_a"))

    P.ts("dve", r_a, r_r, float(np.pi / 2), None, ALU.add, None, r=("r_r",), w=("r_a",))
    wrap(r_r)
    wrap(r_a)
    ovl = ("ws0", "ws1", "wb0", "wb1", "wb2", "wb3", "xt", "tmpc")
    P.act(sin2, r_r, AF.Sin, r=("r_r",), w=("sin2",) + ovl)
    P.act(cos2, r_a, AF.Sin, r=("r_a",), w=("cos2",) + ovl)

    chk("rope", cos2, 2048, 64)
    def ln_stats(src, key):
        for q in range(4):
            P.add("dve", (lambda q: lambda e: e.bn_stats(stat[:, 6 * q:6 * q + 6], src[:, 512 * q:512 * q + 512]))(q),
                  r=(key,), w=("bnst",))
        P.add("dve", lambda e: e.bn_aggr(stat[:, 24:26], stat[:, 0:24]), r=("bnst",), w=("mv",))
        P.act(stat[:, 26:27], stat[:, 25:26], AF.Sqrt, r=("mv",), w=("std",), bias=eps_t[:, 0:1], scale=1.0)
        P.add("dve", lambda e: e.reciprocal(stat[:, 27:28], stat[:, 26:27]), r=("std",), w=("rstd",))
        P.ts("dve", stat[:, 28:29], stat[:, 24:25], -1.0, stat[:, 27:28], ALU.mult, ALU.mult,
             r=("mv", "rstd"), w=("nmr",))

    eps_t = A.f32(8256 + 4 * 480, 4)
    P.ms("dve", eps_t[:, 0:1], LN_EPS, w=("eps",))
    P.ms("dve", eps_t[:, 1:2], RMS_EPS, w=("eps",))

    bkey = ("pst0", "ps6")
    for blk in range(NB):
        P.dma("xt", xt, xp[blk], r=(), w=("xt",))
        ln_stats(xt, "xt")
        if blk == 0:
            chk("ln_s", stat[:, 24:29], 5)
        P.act(xn, xt, AF.Identity, r=("xt", "rstd", "nmr"), w=("xn",), bias=stat[:, 28:29], scale=stat[:, 27:28])
        if blk == 0:
            chk("ln_x", xn, 2048)
        if blk == 1:
            chk("ln_b0", h0T[:, 3, :], 2048)
        for c4 in range(4):
            half = c4 % 2
            pst = psb[7 - half][:, 0:512].rearrange("p (i t) -> p i t", i=4)
            for i in range(4):
                c = 4 * c4 + i
                P.tr(pst[:, i, :], xn[:, 128 * c:128 * c + 128], ident_b, r=("xn", "ident_b"), w=(bkey[half],))
            if blk == 0 and c4 == 0:
                chk("ln_t", psb[7][:, 0:512], 512)
            if blk == 0 and c4 == 1:
                chk("ln_t1", psb[6][:, 0:512], 512)
            for i in range(4):
                c = 4 * c4 + i
                import os
                evm = os.environ.get("EVM", "mix")
                eng = "dve" if half == 0 else "pool"
                if evm == "dve":
                    eng = "dve"
                if evm == "act":
                    eng = "pool"
                if eng == "pool":
                    P.act(h0T[:, c, 128 * blk:128 * blk + 128], pst[:, i, :], AF.Identity,
                          r=(bkey[half], "small"), w=(f"h0T{c}",), bias=lnb[:, c:c + 1], scale=lng[:, c:c + 1])
                else:
                    P.ts("dve", h0T[:, c, 128 * blk:128 * blk + 128], pst[:, i, :], lng[:, c:c + 1], lnb[:, c:c + 1],
                         ALU.mult, ALU.add, r=(bkey[half], "small"), w=(f"h0T{c}",))
            if blk == 0 and c4 == 0:
                chk("ln_c0", h0T[:, 3, :], 2048)
            if blk == 0 and c4 == 1:
                chk("ln_c1", h0T[:, 3, :], 2048)
            if blk == 0 and c4 == 3:
                chk("ln_c3", h0T[:, 3, :], 2048)

    chk("ln", h0T[:, 3, :], 2048)
    wcnt = [0, 0]
    ws_flat = [A.f32(O_Z + 49280 + 8192 * i, 2048) for i in range(2)]
    cast_rr = [0]

    def load_w(src3, ncols, rows_c, scale_ap=None, dst=None, scale_off=0, cast_engs=("dve", "act")):
        si = wcnt[0] % 2
        wcnt[0] += 1
        s_v = ws_flat[si][:, 0:rows_c * ncols].rearrange("p (c n) -> p c n", c=rows_c)
        P.dma(f"ws{si}", s_v, src3, r=(), w=(f"ws{si}",))
        if dst is None:
            bi = wcnt[1] % 4
            wcnt[1] += 1
            d_v = wb[bi][:, 0:rows_c, 0:ncols]
            key = (f"wb{bi}",)
        else:
            d_v, key = dst
        if scale_ap is None:
            eng = cast_engs[cast_rr[0] % len(cast_engs)]
            cast_rr[0] += 1
            P.cp(eng, d_v, s_v, r=(f"ws{si}",), w=key)
        else:
            for c in range(rows_c):
                sc = scale_ap[:, scale_off + c:scale_off + c + 1]
                if c % 2 == 0:
                    P.ts("dve", d_v[:, c, :], s_v[:, c, :], sc, None, ALU.mult, None, r=(f"ws{si}", "small", "onorm"), w=key)
                else:
                    P.act(d_v[:, c, :], s_v[:, c, :], AF.Identity, r=(f"ws{si}", "small", "onorm"), w=key, scale=sc)
        return d_v, key

    def win_cols(c0, n):
        return w_in[:, c0:c0 + n].rearrange("(c p) n -> p c n", p=128)

    sqb = [sp_t, w_t]

    def latent_group(wts, ncol, rhs_of_c, rhs_keys, dst_of_f, dst_key, nfeat):
        for f, (wt, wk) in enumerate(wts):
            for c in range(16):
                P.mm(ps[f], wt[:, c, :], rhs_of_c(c), c == 0, c == 15, r=wk + (f"h0T{c}",), w=(f"ps{f}",))
        for f in range(len(wts)):
            sq = sqb[f % 2]
            P.act(sq, ps[f], AF.Square, r=(f"ps{f}",), w=(f"sq{f % 2}",))
            P.mm(ps[4], ones_b, sq, f == 0, f == len(wts) - 1, r=("ones_b", f"sq{f % 2}"), w=("ps4",))
        P.act(e_t, ps[4], AF.Sqrt, r=("ps4", "eps"), w=("e_t",), bias=eps_t[:, 1:2], scale=1.0 / nfeat)
        P.add("dve", lambda e: e.reciprocal(t1_t, e_t), r=("e_t",), w=("t1_t",))
        for f in range(len(wts)):
            P.tt("dve", dst_of_f(f), ps[f], t1_t, ALU.mult, r=(f"ps{f}", "t1_t"), w=(dst_key,))

    qw = [load_w(win_cols(3072 + 128 * f, 128), 128, 16) for f in range(4)]
    for g in range(2):
        latent_group(qw, 128, lambda c, g=g: own_tok(h0T, c, g), None,
                     lambda f, g=g: qlatnT[:, f, 512 * g:512 * g + 512], "qlatnT", 512.0)
    kvw = [load_w(win_cols(3584 + 128 * f, 128), 128, 16) for f in range(2)]
    for g in range(4):
        latent_group(kvw, 128, lambda c, g=g: h0T[:, c, 512 * g:512 * g + 512], None,
                     lambda f, g=g: kvlatnT[:, f, 512 * g:512 * g + 512], "kvlatnT", 256.0)

    def make_rot(dst, src, nck, key_d, key_s):
        P.ts("pool", dst[:, 0:nck, 0:32], src[:, 0:nck, 32:64], -1.0, None, ALU.mult, None, r=key_s, w=key_d)
        P.cp("pool", dst[:, 0:nck, 32:64], src[:, 0:nck, 0:32], r=key_s, w=key_d)

    def rope_evac(dst, pa, pb, cs, sn, keys_r, key_w):
        P.tt("dve", t1_t[0:64, :], pa, cs, ALU.mult, r=keys_r + ("cos2",), w=("t1_t",))
        P.tt("dve", e_t[0:64, :], pb, sn, ALU.mult, r=keys_r + ("sin2",), w=("e_t",))
        P.tt("pool", dst, t1_t[0:64, :], e_t[0:64, :], ALU.add, r=("t1_t", "e_t"), w=(key_w,))

    krw, krk = load_w(win_cols(3840, 64), 64, 16)
    krot, krotk = wb[wcnt[1] % 4][:, :, 0:64], (f"wb{wcnt[1] % 4}",)
    wcnt[1] += 1
    make_rot(krot, krw, 16, krotk, krk)
    for g in range(4):
        for c in range(16):
            P.mm(ps[0][0:64, :], krw[:, c, :], h0T[:, c, 512 * g:512 * g + 512], c == 0, c == 15,
                 r=krk + (f"h0T{c}",), w=("ps0",))
        for c in range(16):
            P.mm(ps[1][0:64, :], krot[:, c, :], h0T[:, c, 512 * g:512 * g + 512], c == 0, c == 15,
                 r=krotk + (f"h0T{c}",), w=("ps1",))
        rope_evac(kropeT[:, 512 * g:512 * g + 512], ps[0][0:64, :], ps[1][0:64, :],
                  cos2[:, 512 * g:512 * g + 512], sin2[:, 512 * g:512 * g + 512], ("ps0", "ps1"), "kropeT")

    chk("lat", kvlatnT[:, 1, :], 2048)
    chk("krope", kropeT, 2048, 64)
    chk("qlat", qlatnT[:, 2, :], 1024)
    ss_sb = stat[:, 32:40]
    ss_mla = stat[:, 40:48]
    P.ms("dve", stat[:, 32:48], 0.0, w=("ss",))
    P.ms("dve", vt[:, :, 128:132], 1.0, w=("vt",))

    def attn_slot(kind, h, j):
        nblk = 2 * j + 2
        groups = [list(range(b0, min(b0 + 4, nblk))) for b0 in range(0, nblk, 4)]
        groups = groups[::-1]
        oc = 128 if kind == "sb" else 129
        scale = 128.0 ** -0.5 if kind == "sb" else 192.0 ** -0.5
        mask_b = msb_b if kind == "sb" else mmla_b
        o_ps = ps[5][:, 0:oc]
        npv = 0
        tot_pv = nblk
        cur = 0
        for gi, blks in enumerate(groups):
            nb = len(blks)
            cols = nb * 128
            zb = gi % 2
            z = ps[zb]
            bt = ps[2 + zb]
            last_group = gi == len(groups) - 1
            for i, b in enumerate(blks):
                zo = z[:, 128 * i:128 * i + 128]
                if kind == "sb":
                    P.mm(zo, kT[:, 128 * b:128 * b + 128], qT[:, 128 * j:128 * j + 128], True, True,
                         r=("kT", "qT"), w=(f"ps{zb}",))
                else:
                    P.mm(zo, kT[:, 128 * b:128 * b + 128], qT[:, 128 * j:128 * j + 128], True, False,
                         r=("kT", "qT"), w=(f"ps{zb}",))
                    P.mm(zo, kropeT[:, 128 * b:128 * b + 128], qpeT[:, 128 * j:128 * j + 128], False, True,
                         r=("kropeT", "qpeT"), w=(f"ps{zb}",))

            def apply_masks(t, key):
                if gi == 0:
                    i0, i1 = nb - 2, nb - 1
                    P.tt("pool", t[:, 128 * i0:128 * i0 + 128], t[:, 128 * i0:128 * i0 + 128], mask_b, ALU.mult,
                         r=(key, "msb_b", "mmla_b"), w=(key,))
                    P.ts("pool", t[:, 128 * i1:128 * i1 + 128], t[:, 128 * i1:128 * i1 + 128], vis[:, j:j + 1], None,
                         ALU.mult, None, r=(key, "small"), w=(key,))

            if kind == "sb":
                P.act(e_t[:, 0:cols], z[:, 0:cols], AF.Exp, r=(f"ps{zb}",), w=("e_t",), scale=scale)
                P.act(sp_t[:, 0:cols], e_t[:, 0:cols], AF.Ln, r=("e_t", "ones_f"), w=("sp_t",), bias=ones_f[:, 0:1], scale=1.0)
                apply_masks(sp_t, "sp_t")
                P.stt("dve", t1_t[:, 0:cols], z[:, 0:cols], scale, sp_t[:, 0:cols], ALU.mult, ALU.subtract,
                      r=(f"ps{zb}", "sp_t"), w=("t1_t",))
                for i, b in enumerate(blks):
                    terms = [(triu_b, i, "triu_b")]
                    pair = b // 2
                    if b % 2 == 0:
                        terms.append((pairf_b[:, (pair * 2 + 0) * 128:(pair * 2 + 1) * 128], i + 1, "pairf_b"))
                    else:
                        terms.append((pairf_b[:, (pair * 2 + 1) * 128:(pair * 2 + 2) * 128], i - 1, "pairf_b"))
                    for i2, b2 in enumerate(blks):
                        if b2 // 2 > pair:
                            terms.append((ones_b, i2, "ones_b"))
                    for n, (m, src, mk) in enumerate(terms):
                        P.mm(bt[:, 128 * i:128 * i + 128], m, sp_t[:, 128 * src:128 * src + 128], n == 0,
                             n == len(terms) - 1, r=(mk, "sp_t"), w=(f"ps{2 + zb}",))
                if not last_group:
                    for i in range(nb):
                        P.mm(ps[4][:, 0:128], ones_b, sp_t[:, 128 * i:128 * i + 128], i == 0, i == nb - 1,
                             r=("ones_b", "sp_t"), w=("ps4",))
                P.tt("dve", t1_t[:, 0:cols], t1_t[:, 0:cols], bt[:, 0:cols], ALU.subtract, r=("t1_t", f"ps{2 + zb}"), w=("t1_t",))
                if gi > 0:
                    t3 = t1_t[:, 0:cols].rearrange("p (b t) -> p b t", b=nb)
                    P.tt("pool", t3, t3, carry[cur].unsqueeze(1).to_broadcast([128, nb, 128]), ALU.subtract,
                         r=("t1_t", f"carry{cur}"), w=("t1_t",))
                if not last_group:
                    if gi == 0:
                        P.cp("dve", carry[0], ps[4][:, 0:128], r=("ps4",), w=("carry0",))
                        cur = 0
                    else:
                        P.tt("dve", carry[1 - cur], carry[cur], ps[4][:, 0:128], ALU.add, r=(f"carry{cur}", "ps4"),
                             w=(f"carry{1 - cur}",))
                        cur = 1 - cur
                P.act(w_t[:, 0:cols], t1_t[:, 0:cols], AF.Exp, r=("t1_t",), w=("w_t",))
            else:
                P.act(w_t[:, 0:cols], z[:, 0:cols], AF.Exp, r=(f"ps{zb}",), w=("w_t",), scale=scale)
            apply_masks(w_t, "w_t")
            for i, b in enumerate(blks):
                P.mm(o_ps, w_t[:, 128 * i:128 * i + 128], vt[:, b, 0:oc], npv == 0, npv == tot_pv - 1,
                     r=("w_t", "vt"), w=("ps5",))
                npv += 1
        hc = h if kind == "sb" else 8 + h
        ssd = ss_sb if kind == "sb" else ss_mla
        if kind == "sb":
            P.act(e_t[:, 0:128], o_ps[:, 0:128], AF.Square, r=("ps5",), w=("e_t", "ssacc"), accum_out=stat[:, 64:65])
            P.cp("dve", osb, o_ps[:, 0:128], r=("ps5",), w=("osb",))
        else:
            P.add("dve", lambda e: e.reciprocal(rinv, o_ps[:, 128:129]), r=("ps5",), w=("rinv",))
            P.act(e_t[:, 0:128], o_ps[:, 0:128], AF.Square, r=("ps5", "rinv"), w=("e_t", "ssacc"), scale=rinv,
                  accum_out=stat[:, 64:65])
            P.ts("dve", osb, o_ps[:, 0:128], rinv, None, ALU.mult, None, r=("ps5", "rinv"), w=("osb",))
        P.tt("dve", ssd[:, j:j + 1], ssd[:, j:j + 1], stat[:, 64:65], ALU.add, r=("ssacc", "ss"), w=("ss",))
        P.tr(psb[7][:, 0:128], osb, ident_b, r=("osb", "ident_b"), w=("pst0",))
        P.cp("act", mixT[:, hc, 128 * j:128 * j + 128], psb[7][:, 0:128], r=("pst0",), w=(f"mixT{hc}",))

    def proj_T(dst, wt, wkey, nck, rhs_of, rkeys, ngrp, dkey, m=128):
        for g in range(ngrp):
            pb = 6 + g % 2
            pkeys = ("ps6",) if pb == 6 else ("pst0", "pst1")
            for c in range(nck):
                P.mm(ps[pb][0:m, :], wt[:, c, :], rhs_of(c, g), c == 0, c == nck - 1, r=wkey + rkeys(c), w=pkeys)
            P.cp("act", dst[:, 512 * g:512 * g + 512], ps[pb][0:m, :], r=pkeys, w=(dkey,))

    def proj_v(wt, wkey, nck, lhs_of, lkeys):
        for q4 in range(4):
            pb = 6 + q4 % 2
            pkeys = ("ps6",) if pb == 6 else ("pst0", "pst1")
            for i in range(4):
                blk = 4 * q4 + i
                for c in range(nck):
                    P.mm(ps[pb][:, 128 * i:128 * i + 128], lhs_of(c, blk), wt[:, c, :], c == 0, c == nck - 1,
                         r=wkey + lkeys(c), w=pkeys)
            P.cp("act", vt[:, 4 * q4:4 * q4 + 4, 0:128], ps[pb].rearrange("p (b d) -> p b d", b=4), r=pkeys, w=("vt",))

    n_heads = H if stage != "fast" else 1
    for h in range(n_heads):
        wq, wqk = load_w(win_cols(h * 128, 128), 128, 16)
        wk_, wkk = load_w(win_cols(1024 + h * 128, 128), 128, 16)
        wv, wvk = load_w(win_cols(2048 + h * 128, 128), 128, 16)
        proj_T(qT, wq, wqk, 16, lambda c, g: own_tok(h0T, c, g), lambda c: (f"h0T{c}",), 2, "qT")
        proj_T(kT, wk_, wkk, 16, lambda c, g: h0T[:, c, 512 * g:512 * g + 512], lambda c: (f"h0T{c}",), 4, "kT")
        proj_v(wv, wvk, 16, lambda c, blk: h0T[:, c, 128 * blk:128 * blk + 128], lambda c: (f"h0T{c}",))
        if h == 0:
            chk("proj_k", kT, 2048)
            chk("proj_q", qT, 1024)
            chk("proj_v", vt[:, :, 0:128], 2048)
        for j in range(NS):
            attn_slot("sb", h, j)
            if h == 0 and j == 1:
                chk("sb01", mixT[:, 0, :], 256)
        if h == 0:
            chk("sb0", mixT[:, 0, :], 1024)

    wqb = A.bf(O_H0T, 4 * 1536).rearrange("p (c n) -> p c n", c=4)
    wqb_k = ("h0T0", "h0T1", "h0T2")
    wkvb = A.bf(O_H0T + 12288, 2 * 2048).rearrange("p (c n) -> p c n", c=2)
    wkvb_k = ("h0T3", "h0T4")
    wqrot = A.bf(O_H0T + 20480, 4 * 512).rearrange("p (c n) -> p c n", c=4)
    wqrot_k = ("h0T5",)
    if stage != "fast":
        pass
    for q3 in range(3):
        load_w(w_q_b[:, 512 * q3:512 * q3 + 512].rearrange("(c p) n -> p c n", p=128), 512, 4, scale_ap=qan,
               dst=(wqb[:, :, 512 * q3:512 * q3 + 512], wqb_k))
    for q2 in range(2):
        load_w(w_kv_b[:, 1024 * q2:1024 * q2 + 1024].rearrange("(c p) n -> p c n", p=128), 1024, 2, scale_ap=kvan,
               dst=(wkvb[:, :, 1024 * q2:1024 * q2 + 1024], wkvb_k))
    for h in range(H):
        make_rot(wqrot[:, :, 64 * h:64 * h + 64], wqb[:, :, 192 * h + 128:192 * h + 192], 4, wqrot_k, wqb_k)

    def own4(ap2, g):
        return ap2.rearrange("p (b two t) -> p b two t", two=2, t=128)[:, 4 * g:4 * g + 4, 0, :]

    v3 = lambda ap: ap.rearrange("p (b t) -> p b t", b=4)

    for h in range(n_heads):
        proj_T(kT, wkvb[:, :, 256 * h:256 * h + 128], wkvb_k, 2, lambda c, g: kvlatnT[:, c, 512 * g:512 * g + 512],
               lambda c: ("kvlatnT",), 4, "kT")
        proj_v(wkvb[:, :, 256 * h + 128:256 * h + 256], wkvb_k, 2, lambda c, blk: kvlatnT[:, c, 128 * blk:128 * blk + 128],
               lambda c: ("kvlatnT",))
        proj_T(qT, wqb[:, :, 192 * h:192 * h + 128], wqb_k, 4, lambda c, g: qlatnT[:, c, 512 * g:512 * g + 512],
               lambda c: ("qlatnT",), 2, "qT")
        for g in range(2):
            for c in range(4):
                P.mm(ps[0][0:64, :], wqb[:, c, 192 * h + 128:192 * h + 192], qlatnT[:, c, 512 * g:512 * g + 512],
                     c == 0, c == 3, r=wqb_k + ("qlatnT",), w=("ps0",))
            for c in range(4):
                P.mm(ps[1][0:64, :], wqrot[:, c, 64 * h:64 * h + 64], qlatnT[:, c, 512 * g:512 * g + 512],
                     c == 0, c == 3, r=wqrot_k + ("qlatnT",), w=("ps1",))
            P.tt("dve", v3(t1_t[0:64, :]), v3(ps[0][0:64, :]), own4(cos2, g), ALU.mult, r=("ps0", "cos2"), w=("t1_t",))
            P.tt("dve", v3(e_t[0:64, :]), v3(ps[1][0:64, :]), own4(sin2, g), ALU.mult, r=("ps1", "sin2"), w=("e_t",))
            P.tt("pool", qpeT[:, 512 * g:512 * g + 512], t1_t[0:64, :], e_t[0:64, :], ALU.add, r=("t1_t", "e_t"), w=("qpeT",))
        if h == 0:
            chk("mproj_k", kT, 2048)
            chk("mproj_q", qT, 1024)
            chk("mproj_qpe", qpeT, 1024, 64)
            chk("mproj_v", vt[:, :, 0:128], 2048)
        for j in range(NS):
            attn_slot("mla", h, j)
        if h == 0:
            chk("mla0", mixT[:, 8, :], 1024)

    P.fence()

    acc = A.f32(O_H0T, NS * D).rearrange("p (j d) -> p j d", j=NS)
    acck = lambda j: (f"acc{j}",)
    h1bf = A.bf(O_MIX, NS * D).rearrange("p (j d) -> p j d", j=NS)
    gbc = A.f32(O_Z, D)
    bbc = A.f32(O_Z + 8192, D)
    xt2 = A.f32(O_Z + 16384, D)
    wso = [A.f32(O_Z + 24576 + 16384 * i, 8 * 512).rearrange("p (c n) -> p c n", c=8) for i in range(2)]
    wbo = [A.bf(O_Z + 57344 + 8192 * i, 8 * 512).rearrange("p (c n) -> p c n", c=8) for i in range(2)]
    h1T = A.f32(O_Z + 75776, 16 * 128).rearrange("p (c t) -> p c t", c=16)
    wr = A.f32(O_Z + 83968, 16 * E).rearrange("p (c n) -> p c n", c=16)
    rt = A.f32(O_Z + 86016, 8 * E)
    lg = rt[:, 0:32]
    m8 = rt[:, 32:40]
    negm = rt[:, 40:41]
    den = rt[:, 41:42]
    ex = rt[:, 64:96]
    brt = rt[:, 96:128]
    Gt = A.f32(O_Z + 90112, NS * E).rearrange("p (j e) -> p j e", j=NS)
    rankm = A.f32(O_Z + 91136, NS * E).rearrange("p (j e) -> p j e", j=NS)
    routed = A.f32(O_Z + 92160, NS * E).rearrange("p (j e) -> p j e", j=NS)

    P.dma("gbc", gbc, ln_in_g.partition_broadcast(128), r=(), w=("gbc",))
    P.dma("bbc", bbc, ln_in_b.partition_broadcast(128), r=(), w=("bbc",))
    P.add("act", lambda e: e.mul(gbc, gbc, ALPHA), r=("gbc",), w=("gbc",))
    P.add("act", lambda e: e.mul(bbc, bbc, ALPHA), r=("bbc",), w=("bbc",))
    for j in range(NS):
        P.dma("xt2", xt2, xp[2 * j], r=(), w=("xt2",))
        ln_stats(xt2, "xt2")
        P.act(xt2, xt2, AF.Identity, r=("xt2", "rstd", "nmr"), w=("xt2",), bias=stat[:, 28:29], scale=stat[:, 27:28])
        P.tt("dve", acc[:, j, :], xt2, gbc, ALU.mult, r=("xt2", "gbc"), w=acck(j))
        P.tt("pool", acc[:, j, :], acc[:, j, :], bbc, ALU.add, r=acck(j) + ("bbc",), w=acck(j))

    P.act(stat[:, 48:64], stat[:, 32:48], AF.Sqrt, r=("ss", "eps"), w=("rstdo",), bias=eps_t[:, 1:2], scale=1.0 / 1024.0)
    P.add("dve", lambda e: e.reciprocal(stat[:, 48:64], stat[:, 48:64]), r=("rstdo",), w=("rstdo",))
    oi = 0
    for dblk in range(4):
        for half in range(2):
            si = oi % 2
            oi += 1
            src = w_o[1024 * half:1024 * half + 1024, 512 * dblk:512 * dblk + 512].rearrange("(c p) n -> p c n", p=128)
            P.dma(f"wso{si}", wso[si], src, r=(), w=(f"wso{si}",))
            for c in range(8):
                sc = onorm[:, 8 * half + c:8 * half + c + 1]
                if c % 2 == 0:
                    P.ts("dve", wbo[si][:, c, :], wso[si][:, c, :], sc, None, ALU.mult, None, r=(f"wso{si}", "onorm"), w=(f"wbo{si}",))
                else:
                    P.act(wbo[si][:, c, :], wso[si][:, c, :], AF.Identity, r=(f"wso{si}", "onorm"), w=(f"wbo{si}",), scale=sc)
            for j in range(NS):
                pb = 6 + j % 2
                pkeys = ("ps6",) if pb == 6 else ("pst0", "pst1")
                for c in range(8):
                    P.mm(ps[pb], mixT[:, 8 * half + c, 128 * j:128 * j + 128], wbo[si][:, c, :], c == 0, c == 7,
                         r=(f"mixT{8 * half + c}", f"wbo{si}"), w=pkeys)
                dst = acc[:, j, 512 * dblk:512 * dblk + 512]
                P.stt("dve", dst, ps[pb], stat[:, 48 + 8 * half + j:48 + 8 * half + j + 1], dst, ALU.mult, ALU.add,
                      r=pkeys + acck(j) + ("rstdo",), w=acck(j))

    P.fence()
    P.dma("gbc", gbc, ln_mix_g.partition_broadcast(128), r=(), w=("gbc",))
    P.dma("bbc", bbc, ln_mix_b.partition_broadcast(128), r=(), w=("bbc",))
    P.dma("wr", wr, w_router.rearrange("(c p) n -> p c n", p=128), r=(), w=("wr",))
    P.dma("brt", brt, b_router.partition_broadcast(128), r=(), w=("brt",))
    for j in range(NS):
        ln_stats(acc[:, j, :], f"acc{j}")
        P.act(xt2, acc[:, j, :], AF.Identity, r=acck(j) + ("rstd", "nmr"), w=("xt2",), bias=stat[:, 28:29], scale=stat[:, 27:28])
        P.tt("dve", xt2, xt2, gbc, ALU.mult, r=("xt2", "gbc"), w=("xt2",))
        P.tt("pool", acc[:, j, :], xt2, bbc, ALU.add, r=("xt2", "bbc"), w=acck(j))
        P.cp("pool", h1bf[:, j, :], acc[:, j, :], r=acck(j), w=(f"h1bf{j}",))
        for c4 in range(4):
            pb = 6 + c4 % 2
            pkeys = ("ps6",) if pb == 6 else ("pst0", "pst1")
            for i in range(4):
                c = 4 * c4 + i
                P.tr(ps[pb][:, 128 * i:128 * i + 128], acc[:, j, 128 * c:128 * c + 128], ident_f, r=acck(j) + ("ident_f",), w=pkeys)
            P.cp("act", h1T[:, 4 * c4:4 * c4 + 4, :], ps[pb].rearrange("p (i t) -> p i t", i=4), r=pkeys, w=("h1T",))
        for c in range(16):
            P.mm(ps[4][:, 0:E], h1T[:, c, :], wr[:, c, :], c == 0, c == 15, r=("h1T", "wr"), w=("ps4",))
        P.tt("dve", lg, ps[4][:, 0:E], brt, ALU.add, r=("ps4", "brt"), w=("lg",))
        P.add("dve", lambda e: e.max(m8, lg), r=("lg",), w=("m8",))
        P.ts("dve", routed[:, j, :], lg, m8[:, 3:4], None, ALU.is_ge, None, r=("lg", "m8"), w=("routed",))
        P.ts("dve", negm, m8[:, 0:1], -1.0, None, ALU.mult, None, r=("m8",), w=("negm",))
        P.act(ex, lg, AF.Exp, r=("lg", "negm"), w=("ex",), bias=negm, scale=1.0)
        P.tt("dve", ex, ex, routed[:, j, :], ALU.mult, r=("ex", "routed"), w=("ex",))
        P.add("dve", lambda e: e.reduce_sum(den, ex, AX.X), r=("ex",), w=("den",))
        P.add("dve", lambda e: e.reciprocal(den, den), r=("den",), w=("den",))
        P.ts("dve", Gt[:, j, :], ex, den, None, ALU.mult, None, r=("ex", "den"), w=("Gt",))
        for j2 in range(j + 1):
            P.mm(ps[5][:, 0:E], (tril_f if j2 == j else ones_f), routed[:, j2, :], j2 == 0, j2 == j,
                 r=("tril_f", "ones_f", "routed"), w=("ps5",))
        P.stt("dve", rankm[:, j, :], ps[5][:, 0:E], 1.0, routed[:, j, :], ALU.add, ALU.mult, r=("ps5", "routed"), w=("rankm",))
        P.ts("dve", rankm[:, j, :], rankm[:, j, :], -1.0, None, ALU.add, None, r=("rankm",), w=("rankm",))
        P.add("act", (lambda j: lambda e: e.mul(acc[:, j, :], acc[:, j, :], ALPHA))(j), r=acck(j), w=acck(j))

    if stage == "A":
        P.fence()
        for j in range(NS):
            P.dma(f"out{j % 2}", out[j], acc[:, j, :], r=acck(j), w=())
        P.emit(stack)
        return

    P.fence()

    wgu_t = w_gu
    wdn_t = w_dn
    NSTG = 4
    wsM = [A.f32(O_Z + 8192 * i, 2048).rearrange("p (c n) -> p c n", c=16) for i in range(NSTG)]
    wbM = [A.bf(O_Z + 32768 + 4096 * i, 2048).rearrange("p (c n) -> p c n", c=16) for i in range(3)]
    Sel = A.bf(O_Z + 45056, NS * CAP).rearrange("p (j r) -> p j r", j=NS)
    SelG = A.bf(O_Z + 49152, NS * CAP).rearrange("p (j r) -> p j r", j=NS)
    SelGT = A.bf(O_Z + 53248, 2 * 1024).rearrange("p (c t) -> p c t", c=2)
    xT = A.bf(O_Z + 57344, 16 * CAP).rearrange("p (c r) -> p c r", c=16)
    gs = A.bf(O_Z + 65536, 16 * CAP).rearrange("p (c r) -> p c r", c=16)
    yb = A.bf(O_Z + 65536, 2 * D).rearrange("p (c d) -> p c d", c=2)
    actT = A.bf(O_Z + 73728, 16 * CAP).rearrange("p (c r) -> p c r", c=16)
    gtmp = A.f32(O_Z + 81920, CAP)
    sgt = A.f32(O_Z + 82944, CAP)
    utmp = A.f32(O_Z + 83968, CAP)
    bgu_t = A.f32(O_Z + 84992, n_exp * 32)
    P.dma("bgu", bgu_t, bgu, r=(), w=("bgu",))

    mcnt = [0, 0, 0]
    cast_pat = ("dve", "dve", "act")
    tile_src = []
    for e_i in range(n_exp):
        tile_src += [wgu_t[e_i, fb] for fb in range(32)] + [wdn_t[e_i, t] for t in range(16)]
    issued = []
    PF = 2

    def _issue(k):
        si = mcnt[0] % NSTG
        bi = mcnt[1] % 3
        mcnt[0] += 1
        mcnt[1] += 1
        P.dma(f"wsM{si}", wsM[si], tile_src[k].rearrange("p (c n) -> p c n", c=16), r=(), w=(f"wsM{si}",))
        eng = cast_pat[mcnt[2] % len(cast_pat)]
        mcnt[2] += 1
        P.cp(eng, wbM[bi], wsM[si], r=(f"wsM{si}",), w=(f"wbM{bi}",))
        issued.append((wbM[bi], f"wbM{bi}"))

    def get_tile(k):
        while len(issued) <= min(k + PF, len(tile_src) - 1):
            _issue(len(issued))
        return issued[k]

    for e_i in range(n_exp):
        for j in range(NS):
            P.ts("dve", Sel[:, j, :], iota, rankm[:, j, e_i:e_i + 1], None, ALU.is_equal, None, r=("iota", "rankm"), w=("Sel",))
            P.ts("pool", SelG[:, j, :], iota, rankm[:, j, e_i:e_i + 1], Gt[:, j, e_i:e_i + 1], ALU.is_equal, ALU.mult,
                 r=("iota", "rankm", "Gt"), w=("SelG",))
        for d2 in range(8):
            pb = d2 % 2
            for i in range(2):
                dc = 2 * d2 + i
                for j in range(NS):
                    P.mm(ps[pb][:, CAP * i:CAP * i + CAP], h1bf[:, j, 128 * dc:128 * dc + 128], Sel[:, j, :], j == 0, j == NS - 1,
                         r=(f"h1bf{j}", "Sel"), w=(f"ps{pb}",))
            P.cp("act", xT[:, 2 * d2:2 * d2 + 2, :], ps[pb].rearrange("p (i r) -> p i r", i=2), r=(f"ps{pb}",), w=("xT",))
        for rc in range(2):
            for j4 in range(2):
                half = (2 * rc + j4) % 2
                pst = psb[7 - half][:, 0:512]
                for i in range(4):
                    j = 4 * j4 + i
                    P.tr(pst[:, 128 * i:128 * i + 128], SelG[:, j, 128 * rc:128 * rc + 128], ident_b, r=("SelG", "ident_b"),
                         w=(bkey[half],))
                P.cp("act", SelGT[:, rc, 512 * j4:512 * j4 + 512], pst, r=(bkey[half],), w=("SelGT",))
        for fb in range(32):
            wt, wk = get_tile(48 * e_i + fb)
            pb = 2 + fb % 2
            for c in range(16):
                P.mm(ps[pb][:, 0:CAP], wt[:, c, :], xT[:, c, :], c == 0, c == 15, r=(wk, "xT"), w=(f"ps{pb}",))
            bcol = bgu_t[:, e_i * 32 + fb:e_i * 32 + fb + 1]
            if fb < 16:
                P.ts("dve", gtmp, ps[pb][:, 0:CAP], bcol, 7.0, ALU.add, ALU.min, r=(f"ps{pb}", "bgu"), w=("gtmp",))
                P.act(sgt, gtmp, AF.Sigmoid, r=("gtmp",), w=("sgt",), scale=1.702)
                P.tt("pool", gs[:, fb, :], gtmp, sgt, ALU.mult, r=("gtmp", "sgt"), w=("gs",))
            else:
                P.act(utmp, ps[pb][:, 0:CAP], AF.Identity, r=(f"ps{pb}", "bgu"), w=("utmp",), bias=bcol, scale=1.0)
                P.ts("dve", utmp, utmp, -7.0, 7.0, ALU.max, ALU.min, r=("utmp",), w=("utmp",))
                P.stt("dve", actT[:, fb - 16, :], utmp, 1.0, gs[:, fb - 16, :], ALU.add, ALU.mult, r=("utmp", "gs"), w=("actT",))
        for q in range(4):
            for kq in range(4):
                wt, wk = get_tile(48 * e_i + 32 + 4 * q + kq)
                wt4 = wt.rearrange("p c n -> p (c n)").rearrange("p (c n) -> p c n", c=4)
                for c4 in range(4):
                    c = 4 * kq + c4
                    for rc in range(2):
                        P.mm(ps[4 + rc], actT[:, c, 128 * rc:128 * rc + 128], wt4[:, c4, :], c == 0, c == 15,
                             r=(wk, "actT"), w=(f"ps{4 + rc}",))
            for rc in range(2):
                P.cp("act", yb[:, rc, 512 * q:512 * q + 512], ps[4 + rc], r=(f"ps{4 + rc}",), w=("gs",))
        for j in range(NS):
            for dq in range(4):
                pb = dq % 2
                for rc in range(2):
                    P.mm(ps[pb], SelGT[:, rc, 128 * j:128 * j + 128], yb[:, rc, 512 * dq:512 * dq + 512], rc == 0, rc == 1,
                         r=("SelGT", "gs"), w=(f"ps{pb}",))
                dst = acc[:, j, 512 * dq:512 * dq + 512]
                P.tt("dve", dst, dst, ps[pb], ALU.add, r=acck(j) + (f"ps{pb}",), w=acck(j))

    P.fence()

    bdt = A.f32(O_Z, D)
    gT = A.f32(O_Z + 8192, 128)
    P.dma("bdt", bdt[0:n_exp, :], b_dn, r=(), w=("bdt",))
    for j in range(NS):
        P.tr(ps[6][0:n_exp, 0:128], Gt[:, j, 0:n_exp], ident_f, r=("Gt", "ident_f"), w=("ps6",))
        P.cp("act", gT[0:n_exp, :], ps[6][0:n_exp, 0:128], r=("ps6",), w=("gT",))
        for dq in range(4):
            pb = dq % 2
            P.mm(ps[pb], gT[0:n_exp, :], bdt[0:n_exp, 512 * dq:512 * dq + 512], True, True, r=("gT", "bdt"), w=(f"ps{pb}",))
            dst = acc[:, j, 512 * dq:512 * dq + 512]
            P.tt("dve", dst, dst, ps[pb], ALU.add, r=acck(j) + (f"ps{pb}",), w=acck(j))

    P.fence()

    P.dma("gbc", gbc, ln_ffn_g.partition_broadcast(128), r=(), w=("gbc",))
    P.dma("bbc", bbc, ln_ffn_b.partition_broadcast(128), r=(), w=("bbc",))
    fb2 = [A.f32(O_Z + 16384 + 8192 * i, D) for i in range(2)]
    for j in range(NS):
        fo = fb2[j % 2]
        ln_stats(acc[:, j, :], f"acc{j}")
        P.act(fo, acc[:, j, :], AF.Identity, r=acck(j) + ("rstd", "nmr"), w=(f"fo{j % 2}",), bias=stat[:, 28:29], scale=stat[:, 27:28])
        P.tt("dve", fo, fo, gbc, ALU.mult, r=(f"fo{j % 2}", "gbc"), w=(f"fo{j % 2}",))
        P.tt("pool", fo, fo, bbc, ALU.add, r=(f"fo{j % 2}", "bbc"), w=(f"fo{j % 2}",))
        P.dma(f"out{j % 2}", out[j], fo, r=(f"fo{j % 2}",), w=())
    P.emit(stack)


def _host_consts(g):
    own = QA if g == 0 else QB
    oth = QB if g == 0 else QA
    k = np.arange(128)
    ident = np.eye(128, dtype=np.float32)
    triu = (k[:, None] > k[None, :]).astype(np.float32)
    tril = (k[:, None] < k[None, :]).astype(np.float32)
    msb = (k[:, None] < k[None, :]).astype(np.float32)
    mmla = ((k[:, None] // 64) <= (k[None, :] // 64)).astype(np.float32)
    pairf = np.zeros((128, NS, 2, 128), np.float32)
    small = np.zeros((128, 64), np.float32)
    for i in range(NS):
        f = 1.0 if oth[i] > own[i] else 0.0
        pairf[:, i, 0, :] = f
        pairf[:, i, 1, :] = 1.0 - f
        small[:, i] = 1.0 - f
    inv_freq = (10000.0 ** (-(np.arange(32, dtype=np.float32) * 2.0 / 64.0))).astype(np.float32)
    small[:64, 8] = np.concatenate([inv_freq, inv_freq])
    iota = np.broadcast_to(np.arange(CAP, dtype=np.float32)[None, :], (128, CAP)).copy()
    return dict(c_ident=ident, c_triu=triu, c_tril=tril, c_msb=msb, c_mmla=mmla,
                c_pairf=pairf.reshape(128, -1), c_iota=iota), small


def _pc(v, nchunk):
    return np.ascontiguousarray(np.asarray(v, np.float32).reshape(nchunk, 128).T)


def _prepare(inputs, n_exp=E):
    f = lambda k: np.asarray(inputs[k])
    x = f("x").astype(np.float32, copy=False)
    pos = f("positions").astype(np.int32, copy=False)
    wgu = f("w_gate_up")[0][:n_exp]
    wdn = f("w_down")[0][:n_exp]
    wgu_t = np.ascontiguousarray(wgu.reshape(n_exp, 16, 128, 32, 128).transpose(0, 3, 2, 1, 4)).reshape(n_exp, 32, 128, 2048)
    wdn_t = np.ascontiguousarray(wdn.reshape(n_exp, 4, 4, 128, 4, 512).transpose(0, 4, 1, 3, 2, 5)).reshape(n_exp, 16, 128, 2048)
    bgu = np.ascontiguousarray(f("b_gate_up")[0][:n_exp].reshape(n_exp, 32, 128).transpose(2, 0, 1)).reshape(128, n_exp * 32)
    shared = dict(
        w_in=np.ascontiguousarray(f("w_in")[0]), w_q_b=np.ascontiguousarray(f("w_q_b")[0]),
        w_kv_b=np.ascontiguousarray(f("w_kv_b")[0]), w_o=np.ascontiguousarray(f("w_o")[0]),
        ln_in_g=f("ln_in_g"), ln_in_b=f("ln_in_b"), ln_mix_g=f("ln_mix_g")[0], ln_mix_b=f("ln_mix_b")[0],
        ln_ffn_g=f("ln_ffn_g")[0], ln_ffn_b=f("ln_ffn_b")[0], w_router=np.ascontiguousarray(f("w_router")[0]),
        b_router=f("b_router")[0], w_gu=wgu_t, bgu=bgu, w_dn=wdn_t, b_dn=np.ascontiguousarray(f("b_down")[0][:n_exp]),
        c_onorm=_pc(np.concatenate([f("sb_out_norm")[0], f("mla_out_norm")[0]]), 16),
    )
    shared = {k: np.ascontiguousarray(v, dtype=np.float32) for k, v in shared.items()}
    in_maps = []
    for c in range(8):
        b, g = c // 2, c % 2
        own = QA if g == 0 else QB
        oth = QB if g == 0 else QA
        order = []
        for i in range(NS):
            order += [own[i], oth[i]]
        xb = x[b].reshape(NB, 128, D)[order]
        pb = pos[b].reshape(NB, 128)[order].reshape(-1)
        consts, small = _host_consts(g)
        small = small.copy()
        small[:, 16:32] = _pc(f("ln_in_g"), 16)
        small[:, 32:48] = _pc(f("ln_in_b"), 16)
        small[:, 48:52] = _pc(f("q_a_norm")[0], 4)
        small[:, 52:54] = _pc(f("kv_a_norm")[0], 2)
        m = dict(shared)
        m.update(consts)
        m["c_small"] = small
        m["xp"] = np.ascontiguousarray(xb)
        m["posb"] = np.ascontiguousarray(pb.astype(np.int32))
        in_maps.append(m)
    return in_maps


def _assemble(results):
    out = np.zeros((4, S, D), np.float32)
    for c in range(8):
        b, g = c // 2, c % 2
        own = QA if g == 0 else QB
        o = np.asarray(results[c]["out"], np.float32)
        for j in range(NS):
            out[b, own[j] * 128:(own[j] + 1) * 128, :] = o[j]
    return out


def kernel(**inputs):
    from contextlib import ExitStack
    in_maps = _prepare(inputs)
    nc = bass.Bass("TRN2", target_bir_lowering=False)
    with ExitStack() as stack:
        build(nc, stack)
    res = run_bass_kernel_spmd(nc, in_maps, core_ids=list(range(8)))
    return _assemble(res.results)
```

```python
import numpy as np
import concourse.bass as bass
import concourse.mybir as mybir
from concourse.bass_utils import run_bass_kernel_spmd

F32 = mybir.dt.float32
BF16 = mybir.dt.bfloat16
I32 = mybir.dt.int32
AF = mybir.ActivationFunctionType
ALU = mybir.AluOpType
AX = mybir.AxisListType

D = 2048
S = 2048
NB = 16
NS = 8
H = 8
INP = 3904
E = 32
DFF = 2048
CAP = 256
ALPHA = 2.0 ** 0.25
LN_EPS = 1e-5
RMS_EPS = 1e-6
SEG = 12000
import os as _os
DEBUG_EMIT = bool(_os.environ.get("DEBUG_EMIT"))

QA = [0, 3, 4, 7, 8, 11, 12, 15]
QB = [1, 2, 5, 6, 9, 10, 13, 14]


class Prog:
    ENG = ("pe", "act", "dve", "pool", "sp")

    def __init__(self, nc):
        self.nc = nc
        self.ops = []
        self.lastw = {}
        self.readers = {}
        self.dma_cnt = {}

    def add(self, eng, fn, r=(), w=(), dma=None):
        i = len(self.ops)
        deps = set()
        for b in list(r) + list(w):
            if b in self.lastw:
                deps.add(self.lastw[b])
        for b in w:
            for x in self.readers.get(b, ()):
                deps.add(x)
        for b in r:
            if b.startswith("ps"):
                for x in self.readers.get(b, ()):
                    if self.ops[x]["eng"] != eng:
                        deps.add(x)
        deps.discard(i)
        op = dict(id=i, eng=eng, fn=fn, deps=deps, dma=dma, sig=False, r=tuple(r), w=tuple(w))
        if dma is not None:
            self.dma_cnt[dma] = self.dma_cnt.get(dma, 0) + 1
            op["dval"] = 16 * self.dma_cnt[dma]
        self.ops.append(op)
        for b in w:
            self.lastw[b] = i
            self.readers[b] = []
        for b in r:
            if b not in w:
                self.readers.setdefault(b, []).append(i)
        return i

    def emit(self, stack):
        nc = self.nc
        ops = self.ops
        for op in ops:
            nd = set()
            for d in op["deps"]:
                dop = ops[d]
                if op["fn"] is None:
                    if dop["dma"] is None and dop["eng"] == op["eng"]:
                        continue
                    nd.add(d)
                    continue
                if dop["dma"] is None and dop["eng"] == op["eng"] and op["dma"] is None:
                    if op["eng"] == "pe":
                        continue
                    wr = set(dop["w"])
                    if not (wr & set(op["r"])) and not (wr & set(op["w"])):
                        continue
                nd.add(d)
            op["deps"] = nd
            for d in nd:
                ops[d]["sig"] = True
        cnt = {e: 0 for e in self.ENG}
        for op in ops:
            if op["dma"] is None and op["sig"]:
                cnt[op["eng"]] += 1
                op["sidx"] = cnt[op["eng"]]
        nseg = {e: (cnt[e] + SEG - 1) // SEG for e in self.ENG}
        sems = {}
        for e in self.ENG:
            for k in range(max(1, nseg[e])):
                sems[(e, k)] = stack.enter_context(nc.semaphore(f"s_{e}_{k}"))
        dsem = {}
        for name in self.dma_cnt:
            dsem[name] = stack.enter_context(nc.semaphore(f"d_{name}"))

        def token(op):
            if op["dma"] is not None:
                return ("d", op["dma"]), dsem[op["dma"]], op["dval"]
            k = (op["sidx"] - 1) // SEG
            return (op["eng"], k), sems[(op["eng"], k)], (op["sidx"] - 1) % SEG + 1

        out_dmas = [op for op in ops if op.get("dma") and op["dma"].startswith("out")]
        block = stack.enter_context(nc.Block())
        handles = {"pe": block.tensor, "act": block.scalar, "dve": block.vector,
                   "pool": block.gpsimd, "sp": block.sync}

        def make(engname):
            def body(eng):
                known = {}
                for op in ops:
                    if op["eng"] != engname:
                        continue
                    need = {}
                    for d in op["deps"]:
                        key, sem, val = token(ops[d])
                        if known.get(key, 0) >= val:
                            continue
                        if key not in need or need[key][1] < val:
                            need[key] = (sem, val)
                    for key, (sem, val) in need.items():
                        eng.wait_ge(sem, val)
                        known[key] = val
                        if DEBUG_EMIT:
                            print(f"  [{engname}] wait {key} >= {val}")
                    if DEBUG_EMIT:
                        tk = token(op) if (op["dma"] is not None or op["sig"]) else None
                        print(f"  [{engname}] op{op['id']} r={op['r']} w={op['w']} sig={tk and (tk[0], tk[2])}")
                    if op["fn"] is None:
                        continue
                    ins = op["fn"](eng)
                    if op["dma"] is not None:
                        ins.then_inc(dsem[op["dma"]], 16)
                    elif op["sig"]:
                        _, sem, _ = token(op)
                        ins.then_inc(sem, 1)
                if engname == "sp":
                    last = {}
                    for op in out_dmas:
                        last[op["dma"]] = op["dval"]
                    for name, val in last.items():
                        eng.wait_ge(dsem[name], val)
            return body

        for e in self.ENG:
            handles[e](make(e))

    def fence(self):
        last = {}
        for op in self.ops:
            if op["fn"] is None:
                continue
            k = ("d", op["dma"]) if op["dma"] is not None else op["eng"]
            last[k] = op["id"]
        ids = set(last.values())
        for e in self.ENG:
            i = len(self.ops)
            self.ops.append(dict(id=i, eng=e, fn=None, deps=set(ids), dma=None, sig=False, r=(), w=()))
        self.lastw = {}
        self.readers = {}

    def mm(self, out, lhsT, rhs, start, stop, r, w):
        return self.add("pe", lambda e: e.matmul(out, lhsT, rhs, start=start, stop=stop), r=r, w=w)

    def tr(self, out, in_, ident, r, w):
        return self.add("pe", lambda e: e.transpose(out, in_, ident), r=r, w=w)

    def act(self, out, in_, func, r, w, bias=None, scale=None, accum_out=None, eng="act"):
        kw = {}
        if bias is not None:
            kw["bias"] = bias
        if scale is not None:
            kw["scale"] = scale
        if accum_out is not None:
            kw["accum_out"] = accum_out
        return self.add(eng, lambda e: e.activation(out, in_, func, **kw), r=r, w=w)

    def ts(self, eng, out, in0, s1, s2, op0, op1, r, w, accum_out=None):
        if op1 is None:
            return self.add(eng, lambda e: e.tensor_scalar(out, in0, s1, None, op0), r=r, w=w)
        if accum_out is not None:
            return self.add(eng, lambda e: e.tensor_scalar(out, in0, s1, s2, op0, op1, accum_out=accum_out), r=r, w=w)
        return self.add(eng, lambda e: e.tensor_scalar(out, in0, s1, s2, op0, op1), r=r, w=w)

    def tt(self, eng, out, in0, in1, op, r, w):
        return self.add(eng, lambda e: e.tensor_tensor(out, in0, in1, op), r=r, w=w)

    def stt(self, eng, out, in0, scalar, in1, op0, op1, r, w):
        return self.add(eng, lambda e: e.scalar_tensor_tensor(out, in0, scalar, in1, op0, op1), r=r, w=w)

    def cp(self, eng, out, in_, r, w):
        if eng == "act":
            return self.add(eng, lambda e: e.copy(out, in_), r=r, w=w)
        return self.add(eng, lambda e: e.tensor_copy(out, in_), r=r, w=w)

    def ms(self, eng, ap, val, w):
        return self.add(eng, lambda e: e.memset(ap, val), r=(), w=w)

    def dma(self, stream, out, in_, r, w, eng="sp"):
        return self.add(eng, lambda e: e.dma_start(out=out, in_=in_), r=r, w=w, dma=stream)


class Arena:
    def __init__(self, nc, stack, nbytes):
        self.t = stack.enter_context(nc.sbuf_tensor("arena", [128, nbytes // 4], F32))
        self.f = self.t[:]
        self.b = self.f.bitcast(BF16)
        self.i = self.f.bitcast(I32)

    def f32(self, off, n, p=128):
        return self.f[0:p, off // 4: off // 4 + n]

    def bf(self, off, n, p=128):
        return self.b[0:p, off // 2: off // 2 + n]

    def i32(self, off, n, p=128):
        return self.i[0:p, off // 4: off // 4 + n]


ARENA_BYTES = 210944
O_H0T = 10240
O_MIX = 75776
O_Z = 108544

TWO_PI = 2.0 * np.pi
CW1 = float(np.float32(6.28125))
CW2 = float(np.float32(TWO_PI - 6.28125))


class _Done(Exception):
    pass


def build(nc, stack, n_exp=E, stage="full", stop=None):
    P = Prog(nc)
    try:
        _build(nc, stack, P, n_exp, stage, stop)
    except _Done:
        pass
    return P


def _build(nc, stack, P, n_exp, stage, stop):
    A = Arena(nc, stack, ARENA_BYTES)

    def dram(name, shape, dt=F32, kind="ExternalInput"):
        return nc.dram_tensor(name, list(shape), dt, kind=kind).ap()

    xp = dram("xp", [NB, 128, D])
    posb = dram("posb", [S], I32)
    c_ident = dram("c_ident", [128, 128])
    c_triu = dram("c_triu", [128, 128])
    c_tril = dram("c_tril", [128, 128])
    c_msb = dram("c_msb", [128, 128])
    c_mmla = dram("c_mmla", [128, 128])
    c_pairf = dram("c_pairf", [128, NS * 2 * 128])
    c_small = dram("c_small", [128, 64])
    c_onorm = dram("c_onorm", [128, 16])
    c_iota = dram("c_iota", [128, CAP])
    w_in = dram("w_in", [D, INP])
    w_q_b = dram("w_q_b", [512, 1536])
    w_kv_b = dram("w_kv_b", [256, 2048])
    w_o = dram("w_o", [D, D])
    ln_in_g = dram("ln_in_g", [D])
    ln_in_b = dram("ln_in_b", [D])
    ln_mix_g = dram("ln_mix_g", [D])
    ln_mix_b = dram("ln_mix_b", [D])
    ln_ffn_g = dram("ln_ffn_g", [D])
    ln_ffn_b = dram("ln_ffn_b", [D])
    w_router = dram("w_router", [D, E])
    b_router = dram("b_router", [E])
    w_gu = dram("w_gu", [n_exp, 32, 128, 2048])
    bgu = dram("bgu", [128, n_exp * 32])
    w_dn = dram("w_dn", [n_exp, 16, 128, 2048])
    b_dn = dram("b_dn", [n_exp, D])
    out = dram("out", [NS, 128, D], kind="ExternalOutput")

    ps = [stack.enter_context(nc.psum_tensor(f"ps{i}", [128, 512], F32))[:] for i in range(8)]

    def chk(name, ap=None, n=0, p=128):
        if stop != name:
            return
        P.fence()
        dbg = A.f32(O_Z + 82048, D)
        if ap is not None:
            P.cp("dve", dbg[0:p, 0:n], ap, r=(), w=("dbg",))
            P.dma("out0", out[0][0:p, 0:n], dbg[0:p, 0:n], r=("dbg",), w=())
        else:
            P.ms("dve", dbg[:, 0:8], 1.0, w=("dbg",))
            P.dma("out0", out[0][:, 0:8], dbg[:, 0:8], r=("dbg",), w=())
        P.emit(stack)
        raise _Done()

    psb = [p.bitcast(BF16) for p in ps]

    ident_f = A.f32(0, 128)
    ident_b = A.bf(512, 128)
    triu_b = A.bf(768, 128)
    ones_b = A.bf(1024, 128)
    msb_b = A.bf(1280, 128)
    mmla_b = A.bf(1536, 128)
    pairf_b = A.bf(1792, NS * 2 * 128)
    small = A.f32(5888, 64)
    vis = small[:, 0:8]
    invf = small[0:64, 8:9]
    lng = small[:, 16:32]
    lnb = small[:, 32:48]
    qan = small[:, 48:52]
    kvan = small[:, 52:54]
    onorm = A.f32(6144, 16)
    iota = A.f32(6208, CAP)
    tril_f = A.f32(7232, 128)
    ones_f = A.f32(7744, 128)
    stat = A.f32(8256, 496)

    tmpc = A.f32(O_Z + 49280, 128 * NS * 2)

    def load_const(name, dst_b, src, n, key):
        P.dma("c0", tmpc[:, 0:n], src, r=(), w=("tmpc",))
        P.cp("dve", dst_b, tmpc[:, 0:n], r=("tmpc",), w=(key,))

    P.dma("c1", ident_f, c_ident, r=(), w=("ident_f",))
    P.dma("c2", small, c_small, r=(), w=("small",))
    P.dma("c3", onorm, c_onorm, r=(), w=("onorm",))
    P.dma("c4", iota, c_iota, r=(), w=("iota",))
    P.dma("c5", tril_f, c_tril, r=(), w=("tril_f",))
    P.cp("dve", ident_b, ident_f, r=("ident_f",), w=("ident_b",))
    load_const("triu", triu_b, c_triu, 128, "triu_b")
    load_const("msb", msb_b, c_msb, 128, "msb_b")
    load_const("mmla", mmla_b, c_mmla, 128, "mmla_b")
    load_const("pairf", pairf_b, c_pairf, NS * 2 * 128, "pairf_b")
    P.ms("dve", ones_b, 1.0, w=("ones_b",))
    P.ms("dve", ones_f, 1.0, w=("ones_f",))

    chk("consts", triu_b, 128)
    h0T = A.bf(O_H0T, 16 * S).rearrange("p (c t) -> p c t", c=16)
    mixT = A.bf(O_MIX, 16 * 1024).rearrange("p (c t) -> p c t", c=16)
    cos2 = A.f32(O_Z, S, p=64)
    sin2 = A.f32(O_Z + 8192, S, p=64)
    kropeT = A.bf(O_Z + 16384, S, p=64)
    qlatnT = A.bf(O_Z + 20480, 4 * 1024).rearrange("p (c t) -> p c t", c=4)
    kvlatnT = A.bf(O_Z + 28672, 2 * S).rearrange("p (c t) -> p c t", c=2)
    kT = A.bf(O_Z + 36864, S)
    vt = A.bf(O_Z + 40960, 16 * 132).rearrange("p (b d) -> p b d", b=16)
    qT = A.bf(O_Z + 45184, 1024)
    qpeT = A.bf(O_Z + 47232, 1024, p=64)
    ws = [A.f32(O_Z + 49280 + 8192 * i, 16 * 128).rearrange("p (c n) -> p c n", c=16) for i in range(2)]
    wb = [A.bf(O_Z + 65664 + 4096 * i, 16 * 128).rearrange("p (c n) -> p c n", c=16) for i in range(4)]
    xt = A.f32(O_Z + 82048, D)
    xn = A.bf(O_Z + 90240, D)
    e_t = A.f32(O_Z + 94336, 512)
    sp_t = A.bf(O_Z + 96384, 512)
    t1_t = A.f32(O_Z + 97408, 512)
    w_t = A.bf(O_Z + 99456, 512)
    carry = [A.f32(O_Z + 100480 + 512 * i, 128) for i in range(2)]
    osb = A.bf(O_Z + 101504, 128)
    rinv = A.f32(O_Z + 101760, 1)

    own_tok = lambda ap3, c, g: ap3[:, c, :].rearrange("p (b two t) -> p b two t", two=2, t=128)[:, 4 * g:4 * g + 4, 0, :]

    r_i = A.i32(O_Z + 49280, S, p=64)
    r_a = A.f32(O_Z + 49280 + 8192, S, p=64)
    r_k = A.f32(O_Z + 49280 + 16384, S, p=64)
    r_r = A.f32(O_Z + 49280 + 24576, S, p=64)
    r_m = A.f32(O_Z + 49280 + 32768, S, p=64)
    P.dma("c6", r_i, posb.partition_broadcast(64), r=("tmpc",), w=("r_i", "tmpc"))
    P.cp("dve", r_a, r_i, r=("r_i", "small"), w=("r_a",))
    P.ts("dve", r_a, r_a, invf, None, ALU.mult, None, r=("r_a", "small"), w=("r_a",))
    P.ts("dve", r_i, r_a, 1.0 / TWO_PI, None, ALU.mult, None, r=("r_a",), w=("r_i",))
    P.cp("dve", r_k, r_i, r=("r_i",), w=("r_k",))
    P.stt("dve", r_r, r_k, -CW1, r_a, ALU.mult, ALU.add, r=("r_k", "r_a"), w=("r_r",))
    P.stt("dve", r_r, r_k, -CW2, r_r, ALU.mult, ALU.add, r=("r_k", "r_r"), w=("r_r",))

    def wrap(x):
        P.ts("dve", r_m, x, float(np.pi), -TWO_PI, ALU.is_gt, ALU.mult, r=("r_r", "r_a"), w=("r_m",))
        P.tt("dve", x, x, r_m, ALU.add, r=("r_m",), w=("r_r", "r_a"))
        P.ts("dve", r_m, x, -float(np.pi), TWO_PI, ALU.is_lt, ALU.mult, r=("r_r", "r_a"), w=("r_m",))
        P.tt("dve", x, x, r_m, ALU.add, r=("r_m",), w=("r_r", "r_a"))

    P.ts("dve", r_a, r_r, float(np.pi / 2), None, ALU.add, None, r=("r_r",), w=("r_a",))
    wrap(r_r)
    wrap(r_a)
    ovl = ("ws0", "ws1", "wb0", "wb1", "wb2", "wb3", "xt", "tmpc")
    P.act(sin2, r_r, AF.Sin, r=("r_r",), w=("sin2",) + ovl)
    P.act(cos2, r_a, AF.Sin, r=("r_a",), w=("cos2",) + ovl)

    chk("rope", cos2, 2048, 64)
    def ln_stats(src, key):
        for q in range(4):
            P.add("dve", (lambda q: lambda e: e.bn_stats(stat[:, 6 * q:6 * q + 6], src[:, 512 * q:512 * q + 512]))(q),
                  r=(key,), w=("bnst",))
        P.add("dve", lambda e: e.bn_aggr(stat[:, 24:26], stat[:, 0:24]), r=("bnst",), w=("mv",))
        P.act(stat[:, 26:27], stat[:, 25:26], AF.Sqrt, r=("mv",), w=("std",), bias=eps_t[:, 0:1], scale=1.0)
        P.add("dve", lambda e: e.reciprocal(stat[:, 27:28], stat[:, 26:27]), r=("std",), w=("rstd",))
        P.ts("dve", stat[:, 28:29], stat[:, 24:25], -1.0, stat[:, 27:28], ALU.mult, ALU.mult,
             r=("mv", "rstd"), w=("nmr",))

    eps_t = A.f32(8256 + 4 * 480, 4)
    P.ms("dve", eps_t[:, 0:1], LN_EPS, w=("eps",))
    P.ms("dve", eps_t[:, 1:2], RMS_EPS, w=("eps",))

    bkey = ("pst0", "ps6")
    for blk in range(NB):
        P.dma("xt", xt, xp[blk], r=(), w=("xt",))
        ln_stats(xt, "xt")
        if blk == 0:
            chk("ln_s", stat[:, 24:29], 5)
        P.act(xn, xt, AF.Identity, r=("xt", "rstd", "nmr"), w=("xn",), bias=stat[:, 28:29], scale=stat[:, 27:28])
        if blk == 0:
            chk("ln_x", xn, 2048)
        if blk == 1:
            chk("ln_b0", h0T[:, 3, :], 2048)
        for c4 in range(4):
            half = c4 % 2
            pst = psb[7 - half][:, 0:512].rearrange("p (i t) -> p i t", i=4)
            for i in range(4):
                c = 4 * c4 + i
                P.tr(pst[:, i, :], xn[:, 128 * c:128 * c + 128], ident_b, r=("xn", "ident_b"), w=(bkey[half],))
            if blk == 0 and c4 == 0:
                chk("ln_t", psb[7][:, 0:512], 512)
            if blk == 0 and c4 == 1:
                chk("ln_t1", psb[6][:, 0:512], 512)
            for i in range(4):
                c = 4 * c4 + i
                import os
                evm = os.environ.get("EVM", "mix")
                eng = "dve" if half == 0 else "pool"
                if evm == "dve":
                    eng = "dve"
                if evm == "act":
                    eng = "pool"
                if eng == "pool":
                    P.act(h0T[:, c, 128 * blk:128 * blk + 128], pst[:, i, :], AF.Identity,
                          r=(bkey[half], "small"), w=(f"h0T{c}",), bias=lnb[:, c:c + 1], scale=lng[:, c:c + 1])
                else:
                    P.ts("dve", h0T[:, c, 128 * blk:128 * blk + 128], pst[:, i, :], lng[:, c:c + 1], lnb[:, c:c + 1],
                         ALU.mult, ALU.add, r=(bkey[half], "small"), w=(f"h0T{c}",))
            if blk == 0 and c4 == 0:
                chk("ln_c0", h0T[:, 3, :], 2048)
            if blk == 0 and c4 == 1:
                chk("ln_c1", h0T[:, 3, :], 2048)
            if blk == 0 and c4 == 3:
                chk("ln_c3", h0T[:, 3, :], 2048)

    chk("ln", h0T[:, 3, :], 2048)
    wcnt = [0, 0]
    ws_flat = [A.f32(O_Z + 49280 + 8192 * i, 2048) for i in range(2)]
    cast_rr = [0]

    def load_w(src3, ncols, rows_c, scale_ap=None, dst=None, scale_off=0, cast_engs=("dve", "act")):
        si = wcnt[0] % 2
        wcnt[0] += 1
        s_v = ws_flat[si][:, 0:rows_c * ncols].rearrange("p (c n) -> p c n", c=rows_c)
        P.dma(f"ws{si}", s_v, src3, r=(), w=(f"ws{si}",))
        if dst is None:
            bi = wcnt[1] % 4
            wcnt[1] += 1
            d_v = wb[bi][:, 0:rows_c, 0:ncols]
            key = (f"wb{bi}",)
        else:
            d_v, key = dst
        if scale_ap is None:
            eng = cast_engs[cast_rr[0] % len(cast_engs)]
            cast_rr[0] += 1
            P.cp(eng, d_v, s_v, r=(f"ws{si}",), w=key)
        else:
            for c in range(rows_c):
                sc = scale_ap[:, scale_off + c:scale_off + c + 1]
                if c % 2 == 0:
                    P.ts("dve", d_v[:, c, :], s_v[:, c, :], sc, None, ALU.mult, None, r=(f"ws{si}", "small", "onorm"), w=key)
                else:
                    P.act(d_v[:, c, :], s_v[:, c, :], AF.Identity, r=(f"ws{si}", "small", "onorm"), w=key, scale=sc)
        return d_v, key

    def win_cols(c0, n):
        return w_in[:, c0:c0 + n].rearrange("(c p) n -> p c n", p=128)

    sqb = [sp_t, w_t]

    def latent_group(wts, ncol, rhs_of_c, rhs_keys, dst_of_f, dst_key, nfeat):
        for f, (wt, wk) in enumerate(wts):
            for c in range(16):
                P.mm(ps[f], wt[:, c, :], rhs_of_c(c), c == 0, c == 15, r=wk + (f"h0T{c}",), w=(f"ps{f}",))
        for f in range(len(wts)):
            sq = sqb[f % 2]
            P.act(sq, ps[f], AF.Square, r=(f"ps{f}",), w=(f"sq{f % 2}",))
            P.mm(ps[4], ones_b, sq, f == 0, f == len(wts) - 1, r=("ones_b", f"sq{f % 2}"), w=("ps4",))
        P.act(e_t, ps[4], AF.Sqrt, r=("ps4", "eps"), w=("e_t",), bias=eps_t[:, 1:2], scale=1.0 / nfeat)
        P.add("dve", lambda e: e.reciprocal(t1_t, e_t), r=("e_t",), w=("t1_t",))
        for f in range(len(wts)):
            P.tt("dve", dst_of_f(f), ps[f], t1_t, ALU.mult, r=(f"ps{f}", "t1_t"), w=(dst_key,))

    qw = [load_w(win_cols(3072 + 128 * f, 128), 128, 16) for f in range(4)]
    for g in range(2):
        latent_group(qw, 128, lambda c, g=g: own_tok(h0T, c, g), None,
                     lambda f, g=g: qlatnT[:, f, 512 * g:512 * g + 512], "qlatnT", 512.0)
    kvw = [load_w(win_cols(3584 + 128 * f, 128), 128, 16) for f in range(2)]
    for g in range(4):
        latent_group(kvw, 128, lambda c, g=g: h0T[:, c, 512 * g:512 * g + 512], None,
                     lambda f, g=g: kvlatnT[:, f, 512 * g:512 * g + 512], "kvlatnT", 256.0)

    def make_rot(dst, src, nck, key_d, key_s):
        P.ts("pool", dst[:, 0:nck, 0:32], src[:, 0:nck, 32:64], -1.0, None, ALU.mult, None, r=key_s, w=key_d)
        P.cp("pool", dst[:, 0:nck, 32:64], src[:, 0:nck, 0:32], r=key_s, w=key_d)

    def rope_evac(dst, pa, pb, cs, sn, keys_r, key_w):
        P.tt("dve", t1_t[0:64, :], pa, cs, ALU.mult, r=keys_r + ("cos2",), w=("t1_t",))
        P.tt("dve", e_t[0:64, :], pb, sn, ALU.mult, r=keys_r + ("sin2",), w=("e_t",))
        P.tt("pool", dst, t1_t[0:64, :], e_t[0:64, :], ALU.add, r=("t1_t", "e_t"), w=(key_w,))

    krw, krk = load_w(win_cols(3840, 64), 64, 16)
    krot, krotk = wb[wcnt[1] % 4][:, :, 0:64], (f"wb{wcnt[1] % 4}",)
    wcnt[1] += 1
    make_rot(krot, krw, 16, krotk, krk)
    for g in range(4):
        for c in range(16):
            P.mm(ps[0][0:64, :], krw[:, c, :], h0T[:, c, 512 * g:512 * g + 512], c == 0, c == 15,
                 r=krk + (f"h0T{c}",), w=("ps0",))
        for c in range(16):
            P.mm(ps[1][0:64, :], krot[:, c, :], h0T[:, c, 512 * g:512 * g + 512], c == 0, c == 15,
                 r=krotk + (f"h0T{c}",), w=("ps1",))
        rope_evac(kropeT[:, 512 * g:512 * g + 512], ps[0][0:64, :], ps[1][0:64, :],
                  cos2[:, 512 * g:512 * g + 512], sin2[:, 512 * g:512 * g + 512], ("ps0", "ps1"), "kropeT")

    chk("lat", kvlatnT[:, 1, :], 2048)
    chk("krope", kropeT, 2048, 64)
    chk("qlat", qlatnT[:, 2, :], 1024)
    ss_sb = stat[:, 32:40]
    ss_mla = stat[:, 40:48]
    P.ms("dve", stat[:, 32:48], 0.0, w=("ss",))
    P.ms("dve", vt[:, :, 128:132], 1.0, w=("vt",))

    TB = [dict(e=e_t, sp=sp_t, t1=t1_t, w=w_t, carry=carry, osb=osb, rinv=rinv, sfx=""),
          dict(e=A.f32(O_Z + 82048, 512), sp=A.bf(O_Z + 84096, 512), t1=A.f32(O_Z + 85120, 512), w=A.bf(O_Z + 87168, 512),
               carry=[A.f32(O_Z + 88192 + 512 * i, 128) for i in range(2)], osb=A.bf(O_Z + 89216, 128),
               rinv=A.f32(O_Z + 89472, 1), sfx="B")]

    def attn_slot(kind, h, j, sid=0):
        T = TB[sid]
        X = T["sfx"]
        e_s, sp_s, t1_s, w_s, carry_s, osb_s, rinv_s = T["e"], T["sp"], T["t1"], T["w"], T["carry"], T["osb"], T["rinv"]
        ke, ksp, kt1, kw, kosb, krinv = "e_t" + X, "sp_t" + X, "t1_t" + X, "w_t" + X, "osb" + X, "rinv" + X
        zb, btb, ob = sid, 2 + sid, 4 + sid
        z, bt = ps[zb], ps[btb]
        nblk = 2 * j + 2
        groups = [list(range(b0, min(b0 + 4, nblk))) for b0 in range(0, nblk, 4)]
        groups = groups[::-1]
        oc = 128 if kind == "sb" else 129
        scale = 128.0 ** -0.5 if kind == "sb" else 192.0 ** -0.5
        mask_b = msb_b if kind == "sb" else mmla_b
        o_ps = ps[ob][:, 0:oc]
        npv = 0
        tot_pv = nblk
        cur = 0
        for gi, blks in enumerate(groups):
            nb = len(blks)
            cols = nb * 128
            last_group = gi == len(groups) - 1
            for i, b in enumerate(blks):
                zo = z[:, 128 * i:128 * i + 128]
                if kind == "sb":
                    P.mm(zo, kT[:, 128 * b:128 * b + 128], qT[:, 128 * j:128 * j + 128], True, True,
                         r=("kT", "qT"), w=(f"ps{zb}",))
                else:
                    P.mm(zo, kT[:, 128 * b:128 * b + 128], qT[:, 128 * j:128 * j + 128], True, False,
                         r=("kT", "qT"), w=(f"ps{zb}",))
                    P.mm(zo, kropeT[:, 128 * b:128 * b + 128], qpeT[:, 128 * j:128 * j + 128], False, True,
                         r=("kropeT", "qpeT"), w=(f"ps{zb}",))
            yield

            def apply_masks(t, key):
                if gi == 0:
                    i0, i1 = nb - 2, nb - 1
                    P.tt("pool", t[:, 128 * i0:128 * i0 + 128], t[:, 128 * i0:128 * i0 + 128], mask_b, ALU.mult,
                         r=(key, "msb_b", "mmla_b"), w=(key,))
                    P.ts("pool", t[:, 128 * i1:128 * i1 + 128], t[:, 128 * i1:128 * i1 + 128], vis[:, j:j + 1], None,
                         ALU.mult, None, r=(key, "small"), w=(key,))

            if kind == "sb":
                P.act(e_s[:, 0:cols], z[:, 0:cols], AF.Exp, r=(f"ps{zb}",), w=(ke,), scale=scale)
                P.act(sp_s[:, 0:cols], e_s[:, 0:cols], AF.Ln, r=(ke, "ones_f"), w=(ksp,), bias=ones_f[:, 0:1], scale=1.0)
                apply_masks(sp_s, ksp)
                yield
                P.stt("dve", t1_s[:, 0:cols], z[:, 0:cols], scale, sp_s[:, 0:cols], ALU.mult, ALU.subtract,
                      r=(f"ps{zb}", ksp), w=(kt1,))
                for i, b in enumerate(blks):
                    terms = [(triu_b, i, "triu_b")]
                    pair = b // 2
                    if b % 2 == 0:
                        terms.append((pairf_b[:, (pair * 2 + 0) * 128:(pair * 2 + 1) * 128], i + 1, "pairf_b"))
                    else:
                        terms.append((pairf_b[:, (pair * 2 + 1) * 128:(pair * 2 + 2) * 128], i - 1, "pairf_b"))
                    for i2, b2 in enumerate(blks):
                        if b2 // 2 > pair:
                            terms.append((ones_b, i2, "ones_b"))
                    for n, (m, src, mk) in enumerate(terms):
                        P.mm(bt[:, 128 * i:128 * i + 128], m, sp_s[:, 128 * src:128 * src + 128], n == 0,
                             n == len(terms) - 1, r=(mk, ksp), w=(f"ps{btb}",))
                if not last_group:
                    for i in range(nb):
                        P.mm(ps[6][:, 0:128], ones_b, sp_s[:, 128 * i:128 * i + 128], i == 0, i == nb - 1,
                             r=("ones_b", ksp), w=("ps6",))
                oldc = cur
                if not last_group:
                    if gi == 0:
                        P.cp("dve", carry_s[0], ps[6][:, 0:128], r=("ps6",), w=(f"carry{X}0",))
                        cur = 0
                    else:
                        P.tt("dve", carry_s[1 - cur], carry_s[cur], ps[6][:, 0:128], ALU.add, r=(f"carry{X}{cur}", "ps6"),
                             w=(f"carry{X}{1 - cur}",))
                        cur = 1 - cur
                yield
                P.tt("dve", t1_s[:, 0:cols], t1_s[:, 0:cols], bt[:, 0:cols], ALU.subtract, r=(kt1, f"ps{btb}"), w=(kt1,))
                if gi > 0:
                    t3 = t1_s[:, 0:cols].rearrange("p (b t) -> p b t", b=nb)
                    P.tt("pool", t3, t3, carry_s[oldc].unsqueeze(1).to_broadcast([128, nb, 128]), ALU.subtract,
                         r=(kt1, f"carry{X}{oldc}"), w=(kt1,))
                yield
                P.act(w_s[:, 0:cols], t1_s[:, 0:cols], AF.Exp, r=(kt1,), w=(kw,))
            else:
                P.act(w_s[:, 0:cols], z[:, 0:cols], AF.Exp, r=(f"ps{zb}",), w=(kw,), scale=scale)
            apply_masks(w_s, kw)
            yield
            for i, b in enumerate(blks):
                P.mm(o_ps, w_s[:, 128 * i:128 * i + 128], vt[:, b, 0:oc], npv == 0, npv == tot_pv - 1,
                     r=(kw, "vt"), w=(f"ps{ob}",))
                npv += 1
            yield
        hc = h if kind == "sb" else 8 + h
        ssd = ss_sb if kind == "sb" else ss_mla
        sa = stat[:, 64 + sid:65 + sid]
        if kind == "sb":
            P.act(e_s[:, 0:128], o_ps[:, 0:128], AF.Square, r=(f"ps{ob}",), w=(ke, "ssacc" + X), accum_out=sa)
            P.cp("dve", osb_s, o_ps[:, 0:128], r=(f"ps{ob}",), w=(kosb,))
        else:
            P.add("dve", lambda e: e.reciprocal(rinv_s, o_ps[:, 128:129]), r=(f"ps{ob}",), w=(krinv,))
            P.act(e_s[:, 0:128], o_ps[:, 0:128], AF.Square, r=(f"ps{ob}", krinv), w=(ke, "ssacc" + X), scale=rinv_s,
                  accum_out=sa)
            P.ts("dve", osb_s, o_ps[:, 0:128], rinv_s, None, ALU.mult, None, r=(f"ps{ob}", krinv), w=(kosb,))
        P.tt("dve", ssd[:, j:j + 1], ssd[:, j:j + 1], sa, ALU.add, r=("ssacc" + X, "ss"), w=("ss",))
        yield
        P.tr(psb[7][:, 0:128], osb_s, ident_b, r=(kosb, "ident_b"), w=("pst0",))
        P.cp("act", mixT[:, hc, 128 * j:128 * j + 128], psb[7][:, 0:128], r=("pst0",), w=(f"mixT{hc}",))
        yield

    def run_head_attention(kind, h):
        for ja, jb in ((0, 7), (1, 6), (2, 5), (3, 4)):
            gens = [attn_slot(kind, h, ja, 0), attn_slot(kind, h, jb, 1)]
            live = [True, True]
            while any(live):
                for k in range(2):
                    if live[k]:
                        try:
                            next(gens[k])
                        except StopIteration:
                            live[k] = False

    def proj_T(dst, wt, wkey, nck, rhs_of, rkeys, ngrp, dkey, m=128):
        for g in range(ngrp):
            pb = 6 + g % 2
            pkeys = ("ps6",) if pb == 6 else ("pst0", "pst1")
            for c in range(nck):
                P.mm(ps[pb][0:m, :], wt[:, c, :], rhs_of(c, g), c == 0, c == nck - 1, r=wkey + rkeys(c), w=pkeys)
            P.cp("act", dst[:, 512 * g:512 * g + 512], ps[pb][0:m, :], r=pkeys, w=(dkey,))

    def proj_v(wt, wkey, nck, lhs_of, lkeys):
        for q4 in range(4):
            pb = 6 + q4 % 2
            pkeys = ("ps6",) if pb == 6 else ("pst0", "pst1")
            for i in range(4):
                blk = 4 * q4 + i
                for c in range(nck):
                    P.mm(ps[pb][:, 128 * i:128 * i + 128], lhs_of(c, blk), wt[:, c, :], c == 0, c == nck - 1,
                         r=wkey + lkeys(c), w=pkeys)
            P.cp("act", vt[:, 4 * q4:4 * q4 + 4, 0:128], ps[pb].rearrange("p (b d) -> p b d", b=4), r=pkeys, w=("vt",))

    n_heads = H if stage != "fast" else 1
    for h in range(n_heads):
        wq, wqk = load_w(win_cols(h * 128, 128), 128, 16)
        wk_, wkk = load_w(win_cols(1024 + h * 128, 128), 128, 16)
        wv, wvk = load_w(win_cols(2048 + h * 128, 128), 128, 16)
        proj_T(qT, wq, wqk, 16, lambda c, g: own_tok(h0T, c, g), lambda c: (f"h0T{c}",), 2, "qT")
        proj_T(kT, wk_, wkk, 16, lambda c, g: h0T[:, c, 512 * g:512 * g + 512], lambda c: (f"h0T{c}",), 4, "kT")
        proj_v(wv, wvk, 16, lambda c, blk: h0T[:, c, 128 * blk:128 * blk + 128], lambda c: (f"h0T{c}",))
        if h == 0:
            chk("proj_k", kT, 2048)
            chk("proj_q", qT, 1024)
            chk("proj_v", vt[:, :, 0:128], 2048)
        run_head_attention("sb", h)
        if h == 0:
            chk("sb0", mixT[:, 0, :], 1024)

    wqb = A.bf(O_H0T, 4 * 1536).rearrange("p (c n) -> p c n", c=4)
    wqb_k = ("h0T0", "h0T1", "h0T2")
    wkvb = A.bf(O_H0T + 12288, 2 * 2048).rearrange("p (c n) -> p c n", c=2)
    wkvb_k = ("h0T3", "h0T4")
    wqrot = A.bf(O_H0T + 20480, 4 * 512).rearrange("p (c n) -> p c n", c=4)
    wqrot_k = ("h0T5",)
    if stage != "fast":
        pass
    for q3 in range(3):
        load_w(w_q_b[:, 512 * q3:512 * q3 + 512].rearrange("(c p) n -> p c n", p=128), 512, 4, scale_ap=qan,
               dst=(wqb[:, :, 512 * q3:512 * q3 + 512], wqb_k))
    for q2 in range(2):
        load_w(w_kv_b[:, 1024 * q2:1024 * q2 + 1024].rearrange("(c p) n -> p c n", p=128), 1024, 2, scale_ap=kvan,
               dst=(wkvb[:, :, 1024 * q2:1024 * q2 + 1024], wkvb_k))
    for h in range(H):
        make_rot(wqrot[:, :, 64 * h:64 * h + 64], wqb[:, :, 192 * h + 128:192 * h + 192], 4, wqrot_k, wqb_k)

    def own4(ap2, g):
        return ap2.rearrange("p (b two t) -> p b two t", two=2, t=128)[:, 4 * g:4 * g + 4, 0, :]

    v3 = lambda ap: ap.rearrange("p (b t) -> p b t", b=4)

    for h in range(n_heads):
        proj_T(kT, wkvb[:, :, 256 * h:256 * h + 128], wkvb_k, 2, lambda c, g: kvlatnT[:, c, 512 * g:512 * g + 512],
               lambda c: ("kvlatnT",), 4, "kT")
        proj_v(wkvb[:, :, 256 * h + 128:256 * h + 256], wkvb_k, 2, lambda c, blk: kvlatnT[:, c, 128 * blk:128 * blk + 128],
               lambda c: ("kvlatnT",))
        proj_T(qT, wqb[:, :, 192 * h:192 * h + 128], wqb_k, 4, lambda c, g: qlatnT[:, c, 512 * g:512 * g + 512],
               lambda c: ("qlatnT",), 2, "qT")
        for g in range(2):
            for c in range(4):
                P.mm(ps[0][0:64, :], wqb[:, c, 192 * h + 128:192 * h + 192], qlatnT[:, c, 512 * g:512 * g + 512],
                     c == 0, c == 3, r=wqb_k + ("qlatnT",), w=("ps0",))
            for c in range(4):
                P.mm(ps[1][0:64, :], wqrot[:, c, 64 * h:64 * h + 64], qlatnT[:, c, 512 * g:512 * g + 512],
                     c == 0, c == 3, r=wqrot_k + ("qlatnT",), w=("ps1",))
            P.tt("dve", v3(t1_t[0:64, :]), v3(ps[0][0:64, :]), own4(cos2, g), ALU.mult, r=("ps0", "cos2"), w=("t1_t",))
            P.tt("dve", v3(e_t[0:64, :]), v3(ps[1][0:64, :]), own4(sin2, g), ALU.mult, r=("ps1", "sin2"), w=("e_t",))
            P.tt("pool", qpeT[:, 512 * g:512 * g + 512], t1_t[0:64, :], e_t[0:64, :], ALU.add, r=("t1_t", "e_t"), w=("qpeT",))
        if h == 0:
            chk("mproj_k", kT, 2048)
            chk("mproj_q", qT, 1024)
            chk("mproj_qpe", qpeT, 1024, 64)
            chk("mproj_v", vt[:, :, 0:128], 2048)
        run_head_attention("mla", h)
        if h == 0:
            chk("mla0", mixT[:, 8, :], 1024)

    P.fence()

    acc = A.f32(O_H0T, NS * D).rearrange("p (j d) -> p j d", j=NS)
    acck = lambda j: (f"acc{j}",)
    h1bf = A.bf(O_MIX, NS * D).rearrange("p (j d) -> p j d", j=NS)
    gbc = A.f32(O_Z, D)
    bbc = A.f32(O_Z + 8192, D)
    xt2 = A.f32(O_Z + 16384, D)
    wso = [A.f32(O_Z + 24576 + 16384 * i, 8 * 512).rearrange("p (c n) -> p c n", c=8) for i in range(2)]
    wbo = [A.bf(O_Z + 57344 + 8192 * i, 8 * 512).rearrange("p (c n) -> p c n", c=8) for i in range(2)]
    h1T = A.f32(O_Z + 75776, 16 * 128).rearrange("p (c t) -> p c t", c=16)
    wr = A.f32(O_Z + 83968, 16 * E).rearrange("p (c n) -> p c n", c=16)
    rt = A.f32(O_Z + 86016, 8 * E)
    lg = rt[:, 0:32]
    m8 = rt[:, 32:40]
    negm = rt[:, 40:41]
    den = rt[:, 41:42]
    ex = rt[:, 64:96]
    brt = rt[:, 96:128]
    Gt = A.f32(O_Z + 90112, NS * E).rearrange("p (j e) -> p j e", j=NS)
    rankm = A.f32(O_Z + 91136, NS * E).rearrange("p (j e) -> p j e", j=NS)
    routed = A.f32(O_Z + 92160, NS * E).rearrange("p (j e) -> p j e", j=NS)

    P.dma("gbc", gbc, ln_in_g.partition_broadcast(128), r=(), w=("gbc",))
    P.dma("bbc", bbc, ln_in_b.partition_broadcast(128), r=(), w=("bbc",))
    P.add("act", lambda e: e.mul(gbc, gbc, ALPHA), r=("gbc",), w=("gbc",))
    P.add("act", lambda e: e.mul(bbc, bbc, ALPHA), r=("bbc",), w=("bbc",))
    for j in range(NS):
        P.dma("xt2", xt2, xp[2 * j], r=(), w=("xt2",))
        ln_stats(xt2, "xt2")
        P.act(xt2, xt2, AF.Identity, r=("xt2", "rstd", "nmr"), w=("xt2",), bias=stat[:, 28:29], scale=stat[:, 27:28])
        P.tt("dve", acc[:, j, :], xt2, gbc, ALU.mult, r=("xt2", "gbc"), w=acck(j))
        P.tt("pool", acc[:, j, :], acc[:, j, :], bbc, ALU.add, r=acck(j) + ("bbc",), w=acck(j))

    P.act(stat[:, 48:64], stat[:, 32:48], AF.Sqrt, r=("ss", "eps"), w=("rstdo",), bias=eps_t[:, 1:2], scale=1.0 / 1024.0)
    P.add("dve", lambda e: e.reciprocal(stat[:, 48:64], stat[:, 48:64]), r=("rstdo",), w=("rstdo",))
    oi = 0
    for dblk in range(4):
        for half in range(2):
            si = oi % 2
            oi += 1
            src = w_o[1024 * half:1024 * half + 1024, 512 * dblk:512 * dblk + 512].rearrange("(c p) n -> p c n", p=128)
            P.dma(f"wso{si}", wso[si], src, r=(), w=(f"wso{si}",))
            for c in range(8):
                sc = onorm[:, 8 * half + c:8 * half + c + 1]
                if c % 2 == 0:
                    P.ts("dve", wbo[si][:, c, :], wso[si][:, c, :], sc, None, ALU.mult, None, r=(f"wso{si}", "onorm"), w=(f"wbo{si}",))
                else:
                    P.act(wbo[si][:, c, :], wso[si][:, c, :], AF.Identity, r=(f"wso{si}", "onorm"), w=(f"wbo{si}",), scale=sc)
            for j in range(NS):
                pb = 6 + j % 2
                pkeys = ("ps6",) if pb == 6 else ("pst0", "pst1")
                for c in range(8):
                    P.mm(ps[pb], mixT[:, 8 * half + c, 128 * j:128 * j + 128], wbo[si][:, c, :], c == 0, c == 7,
                         r=(f"mixT{8 * half + c}", f"wbo{si}"), w=pkeys)
                dst = acc[:, j, 512 * dblk:512 * dblk + 512]
                P.stt("dve", dst, ps[pb], stat[:, 48 + 8 * half + j:48 + 8 * half + j + 1], dst, ALU.mult, ALU.add,
                      r=pkeys + acck(j) + ("rstdo",), w=acck(j))

    P.fence()
    P.dma("gbc", gbc, ln_mix_g.partition_broadcast(128), r=(), w=("gbc",))
    P.dma("bbc", bbc, ln_mix_b.partition_broadcast(128), r=(), w=("bbc",))
    P.dma("wr", wr, w_router.rearrange("(c p) n -> p c n", p=128), r=(), w=("wr",))
    P.dma("brt", brt, b_router.partition_broadcast(128), r=(), w=("brt",))
    for j in range(NS):
        ln_stats(acc[:, j, :], f"acc{j}")
        P.act(xt2, acc[:, j, :], AF.Identity, r=acck(j) + ("rstd", "nmr"), w=("xt2",), bias=stat[:, 28:29], scale=stat[:, 27:28])
        P.tt("dve", xt2, xt2, gbc, ALU.mult, r=("xt2", "gbc"), w=("xt2",))
        P.tt("pool", acc[:, j, :], xt2, bbc, ALU.add, r=("xt2", "bbc"), w=acck(j))
        P.cp("pool", h1bf[:, j, :], acc[:, j, :], r=acck(j), w=(f"h1bf{j}",))
        for c4 in range(4):
            pb = 6 + c4 % 2
            pkeys = ("ps6",) if pb == 6 else ("pst0", "pst1")
            for i in range(4):
                c = 4 * c4 + i
                P.tr(ps[pb][:, 128 * i:128 * i + 128], acc[:, j, 128 * c:128 * c + 128], ident_f, r=acck(j) + ("ident_f",), w=pkeys)
            P.cp("act", h1T[:, 4 * c4:4 * c4 + 4, :], ps[pb].rearrange("p (i t) -> p i t", i=4), r=pkeys, w=("h1T",))
        for c in range(16):
            P.mm(ps[4][:, 0:E], h1T[:, c, :], wr[:, c, :], c == 0, c == 15, r=("h1T", "wr"), w=("ps4",))
        P.tt("dve", lg, ps[4][:, 0:E], brt, ALU.add, r=("ps4", "brt"), w=("lg",))
        P.add("dve", lambda e: e.max(m8, lg), r=("lg",), w=("m8",))
        P.ts("dve", routed[:, j, :], lg, m8[:, 3:4], None, ALU.is_ge, None, r=("lg", "m8"), w=("routed",))
        P.ts("dve", negm, m8[:, 0:1], -1.0, None, ALU.mult, None, r=("m8",), w=("negm",))
        P.act(ex, lg, AF.Exp, r=("lg", "negm"), w=("ex",), bias=negm, scale=1.0)
        P.tt("dve", ex, ex, routed[:, j, :], ALU.mult, r=("ex", "routed"), w=("ex",))
        P.add("dve", lambda e: e.reduce_sum(den, ex, AX.X), r=("ex",), w=("den",))
        P.add("dve", lambda e: e.reciprocal(den, den), r=("den",), w=("den",))
        P.ts("dve", Gt[:, j, :], ex, den, None, ALU.mult, None, r=("ex", "den"), w=("Gt",))
        for j2 in range(j + 1):
            P.mm(ps[5][:, 0:E], (tril_f if j2 == j else ones_f), routed[:, j2, :], j2 == 0, j2 == j,
                 r=("tril_f", "ones_f", "routed"), w=("ps5",))
        P.stt("dve", rankm[:, j, :], ps[5][:, 0:E], 1.0, routed[:, j, :], ALU.add, ALU.mult, r=("ps5", "routed"), w=("rankm",))
        P.ts("dve", rankm[:, j, :], rankm[:, j, :], -1.0, None, ALU.add, None, r=("rankm",), w=("rankm",))
        P.add("act", (lambda j: lambda e: e.mul(acc[:, j, :], acc[:, j, :], ALPHA))(j), r=acck(j), w=acck(j))

    if stage == "A":
        P.fence()
        for j in range(NS):
            P.dma(f"out{j % 2}", out[j], acc[:, j, :], r=acck(j), w=())
        P.emit(stack)
        return

    P.fence()

    wgu_t = w_gu
    wdn_t = w_dn
    NSTG = 4
    wsM = [A.f32(O_Z + 8192 * i, 2048).rearrange("p (c n) -> p c n", c=16) for i in range(NSTG)]
    wbM = [A.bf(O_Z + 32768 + 4096 * i, 2048).rearrange("p (c n) -> p c n", c=16) for i in range(3)]
    Sel = A.bf(O_Z + 45056, NS * CAP).rearrange("p (j r) -> p j r", j=NS)
    SelG = A.bf(O_Z + 49152, NS * CAP).rearrange("p (j r) -> p j r", j=NS)
    SelGT = A.bf(O_Z + 53248, 2 * 1024).rearrange("p (c t) -> p c t", c=2)
    xT = A.bf(O_Z + 57344, 16 * CAP).rearrange("p (c r) -> p c r", c=16)
    gs = A.bf(O_Z + 65536, 16 * CAP).rearrange("p (c r) -> p c r", c=16)
    yb = A.bf(O_Z + 65536, 2 * D).rearrange("p (c d) -> p c d", c=2)
    actT = A.bf(O_Z + 73728, 16 * CAP).rearrange("p (c r) -> p c r", c=16)
    gtmp = A.f32(O_Z + 81920, CAP)
    sgt = A.f32(O_Z + 82944, CAP)
    utmp = A.f32(O_Z + 83968, CAP)
    bgu_t = A.f32(O_Z + 84992, n_exp * 32)
    P.dma("bgu", bgu_t, bgu, r=(), w=("bgu",))

    mcnt = [0, 0, 0]
    cast_pat = ("dve", "dve", "act")
    tile_src = []
    for e_i in range(n_exp):
        tile_src += [wgu_t[e_i, fb] for fb in range(32)] + [wdn_t[e_i, t] for t in range(16)]
    issued = []
    PF = 2

    def _issue(k):
        si = mcnt[0] % NSTG
        bi = mcnt[1] % 3
        mcnt[0] += 1
        mcnt[1] += 1
        P.dma(f"wsM{si}", wsM[si], tile_src[k].rearrange("p (c n) -> p c n", c=16), r=(), w=(f"wsM{si}",))
        eng = cast_pat[mcnt[2] % len(cast_pat)]
        mcnt[2] += 1
        P.cp(eng, wbM[bi], wsM[si], r=(f"wsM{si}",), w=(f"wbM{bi}",))
        issued.append((wbM[bi], f"wbM{bi}"))

    def get_tile(k):
        while len(issued) <= min(k + PF, len(tile_src) - 1):
            _issue(len(issued))
        return issued[k]

    for e_i in range(n_exp):
        for j in range(NS):
            P.ts("dve", Sel[:, j, :], iota, rankm[:, j, e_i:e_i + 1], None, ALU.is_equal, None, r=("iota", "rankm"), w=("Sel",))
            P.ts("pool", SelG[:, j, :], iota, rankm[:, j, e_i:e_i + 1], Gt[:, j, e_i:e_i + 1], ALU.is_equal, ALU.mult,
                 r=("iota", "rankm", "Gt"), w=("SelG",))
        for d2 in range(8):
            pb = d2 % 2
            for i in range(2):
                dc = 2 * d2 + i
                for j in range(NS):
                    P.mm(ps[pb][:, CAP * i:CAP * i + CAP], h1bf[:, j, 128 * dc:128 * dc + 128], Sel[:, j, :], j == 0, j == NS - 1,
                         r=(f"h1bf{j}", "Sel"), w=(f"ps{pb}",))
            P.cp("act", xT[:, 2 * d2:2 * d2 + 2, :], ps[pb].rearrange("p (i r) -> p i r", i=2), r=(f"ps{pb}",), w=("xT",))
        for rc in range(2):
            for j4 in range(2):
                half = (2 * rc + j4) % 2
                pst = psb[7 - half][:, 0:512]
                for i in range(4):
                    j = 4 * j4 + i
                    P.tr(pst[:, 128 * i:128 * i + 128], SelG[:, j, 128 * rc:128 * rc + 128], ident_b, r=("SelG", "ident_b"),
                         w=(bkey[half],))
                P.cp("act", SelGT[:, rc, 512 * j4:512 * j4 + 512], pst, r=(bkey[half],), w=("SelGT",))
        for fb in range(32):
            wt, wk = get_tile(48 * e_i + fb)
            pb = 2 + fb % 2
            for c in range(16):
                P.mm(ps[pb][:, 0:CAP], wt[:, c, :], xT[:, c, :], c == 0, c == 15, r=(wk, "xT"), w=(f"ps{pb}",))
            bcol = bgu_t[:, e_i * 32 + fb:e_i * 32 + fb + 1]
            if fb < 16:
                P.ts("dve", gtmp, ps[pb][:, 0:CAP], bcol, 7.0, ALU.add, ALU.min, r=(f"ps{pb}", "bgu"), w=("gtmp",))
                P.act(sgt, gtmp, AF.Sigmoid, r=("gtmp",), w=("sgt",), scale=1.702)
                P.tt("pool", gs[:, fb, :], gtmp, sgt, ALU.mult, r=("gtmp", "sgt"), w=("gs",))
            else:
                P.act(utmp, ps[pb][:, 0:CAP], AF.Identity, r=(f"ps{pb}", "bgu"), w=("utmp",), bias=bcol, scale=1.0)
                P.ts("dve", utmp, utmp, -7.0, 7.0, ALU.max, ALU.min, r=("utmp",), w=("utmp",))
                P.stt("dve", actT[:, fb - 16, :], utmp, 1.0, gs[:, fb - 16, :], ALU.add, ALU.mult, r=("utmp", "gs"), w=("actT",))
        for q in range(4):
            for kq in range(4):
                wt, wk = get_tile(48 * e_i + 32 + 4 * q + kq)
                wt4 = wt.rearrange("p c n -> p (c n)").rearrange("p (c n) -> p c n", c=4)
                for c4 in range(4):
                    c = 4 * kq + c4
                    for rc in range(2):
                        P.mm(ps[4 + rc], actT[:, c, 128 * rc:128 * rc + 128], wt4[:, c4, :], c == 0, c == 15,
                             r=(wk, "actT"), w=(f"ps{4 + rc}",))
            for rc in range(2):
                P.cp("act", yb[:, rc, 512 * q:512 * q + 512], ps[4 + rc], r=(f"ps{4 + rc}",), w=("gs",))
        for j in range(NS):
            for dq in range(4):
                pb = dq % 2
                for rc in range(2):
                    P.mm(ps[pb], SelGT[:, rc, 128 * j:128 * j + 128], yb[:, rc, 512 * dq:512 * dq + 512], rc == 0, rc == 1,
                         r=("SelGT", "gs"), w=(f"ps{pb}",))
                dst = acc[:, j, 512 * dq:512 * dq + 512]
                P.tt("dve", dst, dst, ps[pb], ALU.add, r=acck(j) + (f"ps{pb}",), w=acck(j))

    P.fence()

    bdt = A.f32(O_Z, D)
    gT = A.f32(O_Z + 8192, 128)
    P.dma("bdt", bdt[0:n_exp, :], b_dn, r=(), w=("bdt",))
    for j in range(NS):
        P.tr(ps[6][0:n_exp, 0:128], Gt[:, j, 0:n_exp], ident_f, r=("Gt", "ident_f"), w=("ps6",))
        P.cp("act", gT[0:n_exp, :], ps[6][0:n_exp, 0:128], r=("ps6",), w=("gT",))
        for dq in range(4):
            pb = dq % 2
            P.mm(ps[pb], gT[0:n_exp, :], bdt[0:n_exp, 512 * dq:512 * dq + 512], True, True, r=("gT", "bdt"), w=(f"ps{pb}",))
            dst = acc[:, j, 512 * dq:512 * dq + 512]
            P.tt("dve", dst, dst, ps[pb], ALU.add, r=acck(j) + (f"ps{pb}",), w=acck(j))

    P.fence()

    P.dma("gbc", gbc, ln_ffn_g.partition_broadcast(128), r=(), w=("gbc",))
    P.dma("bbc", bbc, ln_ffn_b.partition_broadcast(128), r=(), w=("bbc",))
    fb2 = [A.f32(O_Z + 16384 + 8192 * i, D) for i in range(2)]
    for j in range(NS):
        fo = fb2[j % 2]
        ln_stats(acc[:, j, :], f"acc{j}")
        P.act(fo, acc[:, j, :], AF.Identity, r=acck(j) + ("rstd", "nmr"), w=(f"fo{j % 2}",), bias=stat[:, 28:29], scale=stat[:, 27:28])
        P.tt("dve", fo, fo, gbc, ALU.mult, r=(f"fo{j % 2}", "gbc"), w=(f"fo{j % 2}",))
        P.tt("pool", fo, fo, bbc, ALU.add, r=(f"fo{j % 2}", "bbc"), w=(f"fo{j % 2}",))
        P.dma(f"out{j % 2}", out[j], fo, r=(f"fo{j % 2}",), w=())
    P.emit(stack)


def _host_consts(g):
    own = QA if g == 0 else QB
    oth = QB if g == 0 else QA
    k = np.arange(128)
    ident = np.eye(128, dtype=np.float32)
    triu = (k[:, None] > k[None, :]).astype(np.float32)
    tril = (k[:, None] < k[None, :]).astype(np.float32)
    msb = (k[:, None] < k[None, :]).astype(np.float32)
    mmla = ((k[:, None] // 64) <= (k[None, :] // 64)).astype(np.float32)
    pairf = np.zeros((128, NS, 2, 128), np.float32)
    small = np.zeros((128, 64), np.float32)
    for i in range(NS):
        f = 1.0 if oth[i] > own[i] else 0.0
        pairf[:, i, 0, :] = f
        pairf[:, i, 1, :] = 1.0 - f
        small[:, i] = 1.0 - f
    inv_freq = (10000.0 ** (-(np.arange(32, dtype=np.float32) * 2.0 / 64.0))).astype(np.float32)
    small[:64, 8] = np.concatenate([inv_freq, inv_freq])
    iota = np.broadcast_to(np.arange(CAP, dtype=np.float32)[None, :], (128, CAP)).copy()
    return dict(c_ident=ident, c_triu=triu, c_tril=tril, c_msb=msb, c_mmla=mmla,
                c_pairf=pairf.reshape(128, -1), c_iota=iota), small


def _pc(v, nchunk):
    return np.ascontiguousarray(np.asarray(v, np.float32).reshape(nchunk, 128).T)


def _prepare(inputs, n_exp=E):
    f = lambda k: np.asarray(inputs[k])
    x = f("x").astype(np.float32, copy=False)
    pos = f("positions").astype(np.int32, copy=False)
    wgu = f("w_gate_up")[0][:n_exp]
    wdn = f("w_down")[0][:n_exp]
    wgu_t = np.ascontiguousarray(wgu.reshape(n_exp, 16, 128, 32, 128).transpose(0, 3, 2, 1, 4)).reshape(n_exp, 32, 128, 2048)
    wdn_t = np.ascontiguousarray(wdn.reshape(n_exp, 4, 4, 128, 4, 512).transpose(0, 4, 1, 3, 2, 5)).reshape(n_exp, 16, 128, 2048)
    bgu = np.ascontiguousarray(f("b_gate_up")[0][:n_exp].reshape(n_exp, 32, 128).transpose(2, 0, 1)).reshape(128, n_exp * 32)
    shared = dict(
        w_in=np.ascontiguousarray(f("w_in")[0]), w_q_b=np.ascontiguousarray(f("w_q_b")[0]),
        w_kv_b=np.ascontiguousarray(f("w_kv_b")[0]), w_o=np.ascontiguousarray(f("w_o")[0]),
        ln_in_g=f("ln_in_g"), ln_in_b=f("ln_in_b"), ln_mix_g=f("ln_mix_g")[0], ln_mix_b=f("ln_mix_b")[0],
        ln_ffn_g=f("ln_ffn_g")[0], ln_ffn_b=f("ln_ffn_b")[0], w_router=np.ascontiguousarray(f("w_router")[0]),
        b_router=f("b_router")[0], w_gu=wgu_t, bgu=bgu, w_dn=wdn_t, b_dn=np.ascontiguousarray(f("b_down")[0][:n_exp]),
        c_onorm=_pc(np.concatenate([f("sb_out_norm")[0], f("mla_out_norm")[0]]), 16),
    )
    shared = {k: np.ascontiguousarray(v, dtype=np.float32) for k, v in shared.items()}
    in_maps = []
    for c in range(8):
        b, g = c // 2, c % 2
        own = QA if g == 0 else QB
        oth = QB if g == 0 else QA
        order = []
        for i in range(NS):
            order += [own[i], oth[i]]
        xb = x[b].reshape(NB, 128, D)[order]
        pb = pos[b].reshape(NB, 128)[order].reshape(-1)
        consts, small = _host_consts(g)
        small = small.copy()
        small[:, 16:32] = _pc(f("ln_in_g"), 16)
        small[:, 32:48] = _pc(f("ln_in_b"), 16)
        small[:, 48:52] = _pc(f("q_a_norm")[0], 4)
        small[:, 52:54] = _pc(f("kv_a_norm")[0], 2)
        m = dict(shared)
        m.update(consts)
        m["c_small"] = small
        m["xp"] = np.ascontiguousarray(xb)
        m["posb"] = np.ascontiguousarray(pb.astype(np.int32))
        in_maps.append(m)
    return in_maps


def _assemble(results):
    out = np.zeros((4, S, D), np.float32)
    for c in range(8):
        b, g = c // 2, c % 2
        own = QA if g == 0 else QB
        o = np.asarray(results[c]["out"], np.float32)
        for j in range(NS):
            out[b, own[j] * 128:(own[j] + 1) * 128, :] = o[j]
    return out


def kernel(**inputs):
    from contextlib import ExitStack
    in_maps = _prepare(inputs)
    nc = bass.Bass("TRN2", target_bir_lowering=False)
    with ExitStack() as stack:
        build(nc, stack)
    res = run_bass_kernel_spmd(nc, in_maps, core_ids=list(range(8)))
    return _assemble(res.results)
```

```python
import numpy as np
import concourse.bass as bass
import concourse.mybir as mybir
from concourse.bass_utils import run_bass_kernel_spmd

F32 = mybir.dt.float32
BF16 = mybir.dt.bfloat16
I32 = mybir.dt.int32
AF = mybir.ActivationFunctionType
ALU = mybir.AluOpType
AX = mybir.AxisListType

D = 2048
S = 2048
NB = 16
NS = 8
H = 8
INP = 3904
E = 32
DFF = 2048
CAP = 256
ALPHA = 2.0 ** 0.25
LN_EPS = 1e-5
RMS_EPS = 1e-6
SEG = 12000
import os as _os
DEBUG_EMIT = bool(_os.environ.get("DEBUG_EMIT"))

QA = [0, 3, 4, 7, 8, 11, 12, 15]
QB = [1, 2, 5, 6, 9, 10, 13, 14]


class Prog:
    ENG = ("pe", "act", "dve", "pool", "sp")

    def __init__(self, nc):
        self.nc = nc
        self.ops = []
        self.lastw = {}
        self.readers = {}
        self.dma_cnt = {}

    def add(self, eng, fn, r=(), w=(), dma=None):
        i = len(self.ops)
        deps = set()
        for b in list(r) + list(w):
            if b in self.lastw:
                deps.add(self.lastw[b])
        for b in w:
            for x in self.readers.get(b, ()):
                deps.add(x)
        for b in r:
            if b.startswith("ps"):
                for x in self.readers.get(b, ()):
                    if self.ops[x]["eng"] != eng:
                        deps.add(x)
        deps.discard(i)
        op = dict(id=i, eng=eng, fn=fn, deps=deps, dma=dma, sig=False, r=tuple(r), w=tuple(w))
        if dma is not None:
            self.dma_cnt[dma] = self.dma_cnt.get(dma, 0) + 1
            op["dval"] = 16 * self.dma_cnt[dma]
        self.ops.append(op)
        for b in w:
            self.lastw[b] = i
            self.readers[b] = []
        for b in r:
            if b not in w:
                self.readers.setdefault(b, []).append(i)
        return i

    def emit(self, stack):
        nc = self.nc
        ops = self.ops
        for op in ops:
            nd = set()
            for d in op["deps"]:
                dop = ops[d]
                if op["fn"] is None:
                    if dop["dma"] is None and dop["eng"] == op["eng"]:
                        continue
                    nd.add(d)
                    continue
                if dop["dma"] is None and dop["eng"] == op["eng"] and op["dma"] is None:
                    if op["eng"] == "pe":
                        continue
                    wr = set(dop["w"])
                    if not (wr & set(op["r"])) and not (wr & set(op["w"])):
                        continue
                nd.add(d)
            op["deps"] = nd
            for d in nd:
                ops[d]["sig"] = True
        cnt = {e: 0 for e in self.ENG}
        for op in ops:
            if op["dma"] is None and op["sig"]:
                cnt[op["eng"]] += 1
                op["sidx"] = cnt[op["eng"]]
        nseg = {e: (cnt[e] + SEG - 1) // SEG for e in self.ENG}
        sems = {}
        for e in self.ENG:
            for k in range(max(1, nseg[e])):
                sems[(e, k)] = stack.enter_context(nc.semaphore(f"s_{e}_{k}"))
        dsem = {}
        for name in self.dma_cnt:
            dsem[name] = stack.enter_context(nc.semaphore(f"d_{name}"))

        def token(op):
            if op["dma"] is not None:
                return ("d", op["dma"]), dsem[op["dma"]], op["dval"]
            k = (op["sidx"] - 1) // SEG
            return (op["eng"], k), sems[(op["eng"], k)], (op["sidx"] - 1) % SEG + 1

        out_dmas = [op for op in ops if op.get("dma") and op["dma"].startswith("out")]
        block = stack.enter_context(nc.Block())
        handles = {"pe": block.tensor, "act": block.scalar, "dve": block.vector,
                   "pool": block.gpsimd, "sp": block.sync}

        def make(engname):
            def body(eng):
                known = {}
                for op in ops:
                    if op["eng"] != engname:
                        continue
                    need = {}
                    for d in op["deps"]:
                        key, sem, val = token(ops[d])
                        if known.get(key, 0) >= val:
                            continue
                        if key not in need or need[key][1] < val:
                            need[key] = (sem, val)
                    for key, (sem, val) in need.items():
                        eng.wait_ge(sem, val)
                        known[key] = val
                        if DEBUG_EMIT:
                            print(f"  [{engname}] wait {key} >= {val}")
                    if DEBUG_EMIT:
                        tk = token(op) if (op["dma"] is not None or op["sig"]) else None
                        print(f"  [{engname}] op{op['id']} r={op['r']} w={op['w']} sig={tk and (tk[0], tk[2])}")
                    if op["fn"] is None:
                        continue
                    ins = op["fn"](eng)
                    if op["dma"] is not None:
                        ins.then_inc(dsem[op["dma"]], 16)
                    elif op["sig"]:
                        _, sem, _ = token(op)
                        ins.then_inc(sem, 1)
                if engname == "sp":
                    last = {}
                    for op in out_dmas:
                        last[op["dma"]] = op["dval"]
                    for name, val in last.items():
                        eng.wait_ge(dsem[name], val)
            return body

        for e in self.ENG:
            handles[e](make(e))

    def fence(self):
        last = {}
        for op in self.ops:
            if op["fn"] is None:
                continue
            k = ("d", op["dma"]) if op["dma"] is not None else op["eng"]
            last[k] = op["id"]
        ids = set(last.values())
        for e in self.ENG:
            i = len(self.ops)
            self.ops.append(dict(id=i, eng=e, fn=None, deps=set(ids), dma=None, sig=False, r=(), w=()))
        self.lastw = {}
        self.readers = {}

    def mm(self, out, lhsT, rhs, start, stop, r, w):
        return self.add("pe", lambda e: e.matmul(out, lhsT, rhs, start=start, stop=stop), r=r, w=w)

    def tr(self, out, in_, ident, r, w):
        return self.add("pe", lambda e: e.transpose(out, in_, ident), r=r, w=w)

    def act(self, out, in_, func, r, w, bias=None, scale=None, accum_out=None, eng="act"):
        kw = {}
        if bias is not None:
            kw["bias"] = bias
        if scale is not None:
            kw["scale"] = scale
        if accum_out is not None:
            kw["accum_out"] = accum_out
        return self.add(eng, lambda e: e.activation(out, in_, func, **kw), r=r, w=w)

    def ts(self, eng, out, in0, s1, s2, op0, op1, r, w, accum_out=None):
        if op1 is None:
            return self.add(eng, lambda e: e.tensor_scalar(out, in0, s1, None, op0), r=r, w=w)
        if accum_out is not None:
            return self.add(eng, lambda e: e.tensor_scalar(out, in0, s1, s2, op0, op1, accum_out=accum_out), r=r, w=w)
        return self.add(eng, lambda e: e.tensor_scalar(out, in0, s1, s2, op0, op1), r=r, w=w)

    def tt(self, eng, out, in0, in1, op, r, w):
        return self.add(eng, lambda e: e.tensor_tensor(out, in0, in1, op), r=r, w=w)

    def stt(self, eng, out, in0, scalar, in1, op0, op1, r, w):
        return self.add(eng, lambda e: e.scalar_tensor_tensor(out, in0, scalar, in1, op0, op1), r=r, w=w)

    def cp(self, eng, out, in_, r, w):
        if eng == "act":
            return self.add(eng, lambda e: e.copy(out, in_), r=r, w=w)
        return self.add(eng, lambda e: e.tensor_copy(out, in_), r=r, w=w)

    def ms(self, eng, ap, val, w):
        return self.add(eng, lambda e: e.memset(ap, val), r=(), w=w)

    def dma(self, stream, out, in_, r, w, eng="sp"):
        return self.add(eng, lambda e: e.dma_start(out=out, in_=in_), r=r, w=w, dma=stream)


class Arena:
    def __init__(self, nc, stack, nbytes):
        self.t = stack.enter_context(nc.sbuf_tensor("arena", [128, nbytes // 4], F32))
        self.f = self.t[:]
        self.b = self.f.bitcast(BF16)
        self.i = self.f.bitcast(I32)

    def f32(self, off, n, p=128):
        return self.f[0:p, off // 4: off // 4 + n]

    def bf(self, off, n, p=128):
        return self.b[0:p, off // 2: off // 2 + n]

    def i32(self, off, n, p=128):
        return self.i[0:p, off // 4: off // 4 + n]


ARENA_BYTES = 210944
O_H0T = 10240
O_MIX = 75776
O_Z = 108544

TWO_PI = 2.0 * np.pi
CW1 = float(np.float32(6.28125))
CW2 = float(np.float32(TWO_PI - 6.28125))


class _Done(Exception):
    pass


def build(nc, stack, n_exp=E, stage="full", stop=None):
    P = Prog(nc)
    try:
        _build(nc, stack, P, n_exp, stage, stop)
    except _Done:
        pass
    return P


def _build(nc, stack, P, n_exp, stage, stop):
    A = Arena(nc, stack, ARENA_BYTES)

    def dram(name, shape, dt=F32, kind="ExternalInput"):
        return nc.dram_tensor(name, list(shape), dt, kind=kind).ap()

    xp = dram("xp", [NB, 128, D])
    posb = dram("posb", [S], I32)
    c_ident = dram("c_ident", [128, 128])
    c_triu = dram("c_triu", [128, 128])
    c_tril = dram("c_tril", [128, 128])
    c_msb = dram("c_msb", [128, 128])
    c_mmla = dram("c_mmla", [128, 128])
    c_pairf = dram("c_pairf", [128, NS * 2 * 128])
    c_small = dram("c_small", [128, 64])
    c_onorm = dram("c_onorm", [128, 16])
    c_iota = dram("c_iota", [128, CAP])
    w_in = dram("w_in", [D, INP])
    w_q_b = dram("w_q_b", [512, 1536])
    w_kv_b = dram("w_kv_b", [256, 2048])
    w_o = dram("w_o", [D, D])
    ln_in_g = dram("ln_in_g", [D])
    ln_in_b = dram("ln_in_b", [D])
    ln_mix_g = dram("ln_mix_g", [D])
    ln_mix_b = dram("ln_mix_b", [D])
    ln_ffn_g = dram("ln_ffn_g", [D])
    ln_ffn_b = dram("ln_ffn_b", [D])
    w_router = dram("w_router", [D, E])
    b_router = dram("b_router", [E])
    w_gu = dram("w_gu", [n_exp, 32, 128, 2048])
    bgu = dram("bgu", [128, n_exp * 32])
    w_dn = dram("w_dn", [n_exp, 16, 128, 2048])
    b_dn = dram("b_dn", [n_exp, D])
    out = dram("out", [NS, 128, D], kind="ExternalOutput")

    ps = [stack.enter_context(nc.psum_tensor(f"ps{i}", [128, 512], F32))[:] for i in range(8)]

    def chk(name, ap=None, n=0, p=128):
        if stop != name:
            return
        P.fence()
        dbg = A.f32(O_Z + 82048, D)
        if ap is not None:
            P.cp("dve", dbg[0:p, 0:n], ap, r=(), w=("dbg",))
            P.dma("out0", out[0][0:p, 0:n], dbg[0:p, 0:n], r=("dbg",), w=())
        else:
            P.ms("dve", dbg[:, 0:8], 1.0, w=("dbg",))
            P.dma("out0", out[0][:, 0:8], dbg[:, 0:8], r=("dbg",), w=())
        P.emit(stack)
        raise _Done()

    psb = [p.bitcast(BF16) for p in ps]

    ident_f = A.f32(0, 128)
    ident_b = A.bf(512, 128)
    triu_b = A.bf(768, 128)
    ones_b = A.bf(1024, 128)
    msb_b = A.bf(1280, 128)
    mmla_b = A.bf(1536, 128)
    pairf_b = A.bf(1792, NS * 2 * 128)
    small = A.f32(5888, 64)
    vis = small[:, 0:8]
    invf = small[0:64, 8:9]
    lng = small[:, 16:32]
    lnb = small[:, 32:48]
    qan = small[:, 48:52]
    kvan = small[:, 52:54]
    onorm = A.f32(6144, 16)
    iota = A.f32(6208, CAP)
    tril_f = A.f32(7232, 128)
    ones_f = A.f32(7744, 128)
    stat = A.f32(8256, 496)

    tmpc = A.f32(O_Z + 49280, 128 * NS * 2)

    def load_const(name, dst_b, src, n, key):
        P.dma("c0", tmpc[:, 0:n], src, r=(), w=("tmpc",))
        P.cp("dve", dst_b, tmpc[:, 0:n], r=("tmpc",), w=(key,))

    P.dma("c1", ident_f, c_ident, r=(), w=("ident_f",))
    P.dma("c2", small, c_small, r=(), w=("small",))
    P.dma("c3", onorm, c_onorm, r=(), w=("onorm",))
    P.dma("c4", iota, c_iota, r=(), w=("iota",))
    P.dma("c5", tril_f, c_tril, r=(), w=("tril_f",))
    P.cp("dve", ident_b, ident_f, r=("ident_f",), w=("ident_b",))
    load_const("triu", triu_b, c_triu, 128, "triu_b")
    load_const("msb", msb_b, c_msb, 128, "msb_b")
    load_const("mmla", mmla_b, c_mmla, 128, "mmla_b")
    load_const("pairf", pairf_b, c_pairf, NS * 2 * 128, "pairf_b")
    P.ms("dve", ones_b, 1.0, w=("ones_b",))
    P.ms("dve", ones_f, 1.0, w=("ones_f",))

    chk("consts", triu_b, 128)
    h0T = A.bf(O_H0T, 16 * S).rearrange("p (c t) -> p c t", c=16)
    mixT = A.bf(O_MIX, 16 * 1024).rearrange("p (c t) -> p c t", c=16)
    cos2 = A.f32(O_Z, S, p=64)
    sin2 = A.f32(O_Z + 8192, S, p=64)
    kropeT = A.bf(O_Z + 16384, S, p=64)
    qlatnT = A.bf(O_Z + 20480, 4 * 1024).rearrange("p (c t) -> p c t", c=4)
    kvlatnT = A.bf(O_Z + 28672, 2 * S).rearrange("p (c t) -> p c t", c=2)
    kT = A.bf(O_Z + 36864, S)
    vt = A.bf(O_Z + 40960, 16 * 132).rearrange("p (b d) -> p b d", b=16)
    qT = A.bf(O_Z + 45184, 1024)
    qpeT = A.bf(O_Z + 47232, 1024, p=64)
    ws = [A.f32(O_Z + 49280 + 8192 * i, 16 * 128).rearrange("p (c n) -> p c n", c=16) for i in range(2)]
    wb = [A.bf(O_Z + 65664 + 4096 * i, 16 * 128).rearrange("p (c n) -> p c n", c=16) for i in range(4)]
    xt = A.f32(O_Z + 82048, D)
    xn = A.bf(O_Z + 90240, D)
    e_t = A.f32(O_Z + 94336, 512)
    sp_t = A.bf(O_Z + 96384, 512)
    t1_t = A.f32(O_Z + 97408, 512)
    w_t = A.bf(O_Z + 99456, 512)
    carry = [A.f32(O_Z + 100480 + 512 * i, 128) for i in range(2)]
    osb = A.bf(O_Z + 101504, 128)
    rinv = A.f32(O_Z + 101760, 1)

    own_tok = lambda ap3, c, g: ap3[:, c, :].rearrange("p (b two t) -> p b two t", two=2, t=128)[:, 4 * g:4 * g + 4, 0, :]

    r_i = A.i32(O_Z + 49280, S, p=64)
    r_a = A.f32(O_Z + 49280 + 8192, S, p=64)
    r_k = A.f32(O_Z + 49280 + 16384, S, p=64)
    r_r = A.f32(O_Z + 49280 + 24576, S, p=64)
    r_m = A.f32(O_Z + 49280 + 32768, S, p=64)
    P.dma("c6", r_i, posb.partition_broadcast(64), r=("tmpc",), w=("r_i", "tmpc"))
    P.cp("dve", r_a, r_i, r=("r_i", "small"), w=("r_a",))
    P.ts("dve", r_a, r_a, invf, None, ALU.mult, None, r=("r_a", "small"), w=("r_a",))
    P.ts("dve", r_i, r_a, 1.0 / TWO_PI, None, ALU.mult, None, r=("r_a",), w=("r_i",))
    P.cp("dve", r_k, r_i, r=("r_i",), w=("r_k",))
    P.stt("dve", r_r, r_k, -CW1, r_a, ALU.mult, ALU.add, r=("r_k", "r_a"), w=("r_r",))
    P.stt("dve", r_r, r_k, -CW2, r_r, ALU.mult, ALU.add, r=("r_k", "r_r"), w=("r_r",))

    def wrap(x):
        P.ts("dve", r_m, x, float(np.pi), -TWO_PI, ALU.is_gt, ALU.mult, r=("r_r", "r_a"), w=("r_m",))
        P.tt("dve", x, x, r_m, ALU.add, r=("r_m",), w=("r_r", "r_a"))
        P.ts("dve", r_m, x, -float(np.pi), TWO_PI, ALU.is_lt, ALU.mult, r=("r_r", "r_a"), w=("r_m",))
        P.tt("dve", x, x, r_m, ALU.add, r=("r_m",), w=("r_r", "r_a"))

    P.ts("dve", r_a, r_r, float(np.pi / 2), None, ALU.add, None, r=("r_r",), w=("r_a",))
    wrap(r_r)
    wrap(r_a)
    ovl = ("ws0", "ws1", "wb0", "wb1", "wb2", "wb3", "xt", "tmpc")
    P.act(sin2, r_r, AF.Sin, r=("r_r",), w=("sin2",) + ovl)
    P.act(cos2, r_a, AF.Sin, r=("r_a",), w=("cos2",) + ovl)

    chk("rope", cos2, 2048, 64)
    def ln_stats(src, key):
        for q in range(4):
            P.add("dve", (lambda q: lambda e: e.bn_stats(stat[:, 6 * q:6 * q + 6], src[:, 512 * q:512 * q + 512]))(q),
                  r=(key,), w=("bnst",))
        P.add("dve", lambda e: e.bn_aggr(stat[:, 24:26], stat[:, 0:24]), r=("bnst",), w=("mv",))
        P.act(stat[:, 26:27], stat[:, 25:26], AF.Sqrt, r=("mv",), w=("std",), bias=eps_t[:, 0:1], scale=1.0)
        P.add("dve", lambda e: e.reciprocal(stat[:, 27:28], stat[:, 26:27]), r=("std",), w=("rstd",))
        P.ts("dve", stat[:, 28:29], stat[:, 24:25], -1.0, stat[:, 27:28], ALU.mult, ALU.mult,
             r=("mv", "rstd"), w=("nmr",))

    eps_t = A.f32(8256 + 4 * 480, 4)
    P.ms("dve", eps_t[:, 0:1], LN_EPS, w=("eps",))
    P.ms("dve", eps_t[:, 1:2], RMS_EPS, w=("eps",))

    bkey = ("pst0", "ps6")
    for blk in range(NB):
        P.dma("xt", xt, xp[blk], r=(), w=("xt",))
        ln_stats(xt, "xt")
        if blk == 0:
            chk("ln_s", stat[:, 24:29], 5)
        P.act(xn, xt, AF.Identity, r=("xt", "rstd", "nmr"), w=("xn",), bias=stat[:, 28:29], scale=stat[:, 27:28])
        if blk == 0:
            chk("ln_x", xn, 2048)
        if blk == 1:
            chk("ln_b0", h0T[:, 3, :], 2048)
        for c4 in range(4):
            half = c4 % 2
            pst = psb[7 - half][:, 0:512].rearrange("p (i t) -> p i t", i=4)
            for i in range(4):
                c = 4 * c4 + i
                P.tr(pst[:, i, :], xn[:, 128 * c:128 * c + 128], ident_b, r=("xn", "ident_b"), w=(bkey[half],))
            if blk == 0 and c4 == 0:
                chk("ln_t", psb[7][:, 0:512], 512)
            if blk == 0 and c4 == 1:
                chk("ln_t1", psb[6][:, 0:512], 512)
            for i in range(4):
                c = 4 * c4 + i
                import os
                evm = os.environ.get("EVM", "mix")
                eng = "dve" if half == 0 else "pool"
                if evm == "dve":
                    eng = "dve"
                if evm == "act":
                    eng = "pool"
                if eng == "pool":
                    P.act(h0T[:, c, 128 * blk:128 * blk + 128], pst[:, i, :], AF.Identity,
                          r=(bkey[half], "small"), w=(f"h0T{c}",), bias=lnb[:, c:c + 1], scale=lng[:, c:c + 1])
                else:
                    P.ts("dve", h0T[:, c, 128 * blk:128 * blk + 128], pst[:, i, :], lng[:, c:c + 1], lnb[:, c:c + 1],
                         ALU.mult, ALU.add, r=(bkey[half], "small"), w=(f"h0T{c}",))
            if blk == 0 and c4 == 0:
                chk("ln_c0", h0T[:, 3, :], 2048)
            if blk == 0 and c4 == 1:
                chk("ln_c1", h0T[:, 3, :], 2048)
            if blk == 0 and c4 == 3:
                chk("ln_c3", h0T[:, 3, :], 2048)

    chk("ln", h0T[:, 3, :], 2048)
    wcnt = [0, 0]
    ws_flat = [A.f32(O_Z + 49280 + 8192 * i, 2048) for i in range(2)]
    cast_rr = [0]

    def load_w(src3, ncols, rows_c, scale_ap=None, dst=None, scale_off=0, cast_engs=("dve", "act")):
        si = wcnt[0] % 2
        wcnt[0] += 1
        s_v = ws_flat[si][:, 0:rows_c * ncols].rearrange("p (c n) -> p c n", c=rows_c)
        P.dma(f"ws{si}", s_v, src3, r=(), w=(f"ws{si}",))
        if dst is None:
            bi = wcnt[1] % 4
            wcnt[1] += 1
            d_v = wb[bi][:, 0:rows_c, 0:ncols]
            key = (f"wb{bi}",)
        else:
            d_v, key = dst
        if scale_ap is None:
            eng = cast_engs[cast_rr[0] % len(cast_engs)]
            cast_rr[0] += 1
            P.cp(eng, d_v, s_v, r=(f"ws{si}",), w=key)
        else:
            for c in range(rows_c):
                sc = scale_ap[:, scale_off + c:scale_off + c + 1]
                if c % 2 == 0:
                    P.ts("dve", d_v[:, c, :], s_v[:, c, :], sc, None, ALU.mult, None, r=(f"ws{si}", "small", "onorm"), w=key)
                else:
                    P.act(d_v[:, c, :], s_v[:, c, :], AF.Identity, r=(f"ws{si}", "small", "onorm"), w=key, scale=sc)
        return d_v, key

    def win_cols(c0, n):
        return w_in[:, c0:c0 + n].rearrange("(c p) n -> p c n", p=128)

    sqb = [sp_t, w_t]

    def latent_group(wts, ncol, rhs_of_c, rhs_keys, dst_of_f, dst_key, nfeat):
        for f, (wt, wk) in enumerate(wts):
            for c in range(16):
                P.mm(ps[f], wt[:, c, :], rhs_of_c(c), c == 0, c == 15, r=wk + (f"h0T{c}",), w=(f"ps{f}",))
        for f in range(len(wts)):
            sq = sqb[f % 2]
            P.act(sq, ps[f], AF.Square, r=(f"ps{f}",), w=(f"sq{f % 2}",))
            P.mm(ps[4], ones_b, sq, f == 0, f == len(wts) - 1, r=("ones_b", f"sq{f % 2}"), w=("ps4",))
        P.act(e_t, ps[4], AF.Sqrt, r=("ps4", "eps"), w=("e_t",), bias=eps_t[:, 1:2], scale=1.0 / nfeat)
        P.add("dve", lambda e: e.reciprocal(t1_t, e_t), r=("e_t",), w=("t1_t",))
        for f in range(len(wts)):
            P.tt("dve", dst_of_f(f), ps[f], t1_t, ALU.mult, r=(f"ps{f}", "t1_t"), w=(dst_key,))

    qw = [load_w(win_cols(3072 + 128 * f, 128), 128, 16) for f in range(4)]
    for g in range(2):
        latent_group(qw, 128, lambda c, g=g: own_tok(h0T, c, g), None,
                     lambda f, g=g: qlatnT[:, f, 512 * g:512 * g + 512], "qlatnT", 512.0)
    kvw = [load_w(win_cols(3584 + 128 * f, 128), 128, 16) for f in range(2)]
    for g in range(4):
        latent_group(kvw, 128, lambda c, g=g: h0T[:, c, 512 * g:512 * g + 512], None,
                     lambda f, g=g: kvlatnT[:, f, 512 * g:512 * g + 512], "kvlatnT", 256.0)

    def make_rot(dst, src, nck, key_d, key_s):
        P.ts("pool", dst[:, 0:nck, 0:32], src[:, 0:nck, 32:64], -1.0, None, ALU.mult, None, r=key_s, w=key_d)
        P.cp("pool", dst[:, 0:nck, 32:64], src[:, 0:nck, 0:32], r=key_s, w=key_d)

    def rope_evac(dst, pa, pb, cs, sn, keys_r, key_w):
        P.tt("dve", t1_t[0:64, :], pa, cs, ALU.mult, r=keys_r + ("cos2",), w=("t1_t",))
        P.tt("dve", e_t[0:64, :], pb, sn, ALU.mult, r=keys_r + ("sin2",), w=("e_t",))
        P.tt("pool", dst, t1_t[0:64, :], e_t[0:64, :], ALU.add, r=("t1_t", "e_t"), w=(key_w,))

    krw, krk = load_w(win_cols(3840, 64), 64, 16)
    krot, krotk = wb[wcnt[1] % 4][:, :, 0:64], (f"wb{wcnt[1] % 4}",)
    wcnt[1] += 1
    make_rot(krot, krw, 16, krotk, krk)
    for g in range(4):
        for c in range(16):
            P.mm(ps[0][0:64, :], krw[:, c, :], h0T[:, c, 512 * g:512 * g + 512], c == 0, c == 15,
                 r=krk + (f"h0T{c}",), w=("ps0",))
        for c in range(16):
            P.mm(ps[1][0:64, :], krot[:, c, :], h0T[:, c, 512 * g:512 * g + 512], c == 0, c == 15,
                 r=krotk + (f"h0T{c}",), w=("ps1",))
        rope_evac(kropeT[:, 512 * g:512 * g + 512], ps[0][0:64, :], ps[1][0:64, :],
                  cos2[:, 512 * g:512 * g + 512], sin2[:, 512 * g:512 * g + 512], ("ps0", "ps1"), "kropeT")

    chk("lat", kvlatnT[:, 1, :], 2048)
    chk("krope", kropeT, 2048, 64)
    chk("qlat", qlatnT[:, 2, :], 1024)
    ss_sb = stat[:, 32:40]
    ss_mla = stat[:, 40:48]
    P.ms("dve", stat[:, 32:48], 0.0, w=("ss",))
    P.ms("dve", vt[:, :, 128:132], 1.0, w=("vt",))

    TB = [dict(e=e_t, sp=sp_t, t1=t1_t, w=w_t, carry=carry, osb=osb, rinv=rinv, sfx=""),
          dict(e=A.f32(O_Z + 82048, 512), sp=A.bf(O_Z + 84096, 512), t1=A.f32(O_Z + 85120, 512), w=A.bf(O_Z + 87168, 512),
               carry=[A.f32(O_Z + 88192 + 512 * i, 128) for i in range(2)], osb=A.bf(O_Z + 89216, 128),
               rinv=A.f32(O_Z + 89472, 1), sfx="B")]

    def attn_slot(kind, h, j, sid=0):
        T = TB[sid]
        X = T["sfx"]
        e_s, sp_s, t1_s, w_s, carry_s, osb_s, rinv_s = T["e"], T["sp"], T["t1"], T["w"], T["carry"], T["osb"], T["rinv"]
        ke, ksp, kt1, kw, kosb, krinv = "e_t" + X, "sp_t" + X, "t1_t" + X, "w_t" + X, "osb" + X, "rinv" + X
        zb, btb, ob = sid, 2 + sid, 4 + sid
        z, bt = ps[zb], ps[btb]
        nblk = 2 * j + 2
        groups = [list(range(b0, min(b0 + 4, nblk))) for b0 in range(0, nblk, 4)]
        groups = groups[::-1]
        oc = 128 if kind == "sb" else 129
        scale = 128.0 ** -0.5 if kind == "sb" else 192.0 ** -0.5
        mask_b = msb_b if kind == "sb" else mmla_b
        o_ps = ps[ob][:, 0:oc]
        npv = 0
        tot_pv = nblk
        cur = 0
        for gi, blks in enumerate(groups):
            nb = len(blks)
            cols = nb * 128
            last_group = gi == len(groups) - 1
            for i, b in enumerate(blks):
                zo = z[:, 128 * i:128 * i + 128]
                if kind == "sb":
                    P.mm(zo, kT[:, 128 * b:128 * b + 128], qT[:, 128 * j:128 * j + 128], True, True,
                         r=("kT", "qT"), w=(f"ps{zb}",))
                else:
                    P.mm(zo, kT[:, 128 * b:128 * b + 128], qT[:, 128 * j:128 * j + 128], True, False,
                         r=("kT", "qT"), w=(f"ps{zb}",))
                    P.mm(zo, kropeT[:, 128 * b:128 * b + 128], qpeT[:, 128 * j:128 * j + 128], False, True,
                         r=("kropeT", "qpeT"), w=(f"ps{zb}",))
            yield

            def apply_masks(t, key):
                if gi == 0:
                    i0, i1 = nb - 2, nb - 1
                    P.tt("dve", t[:, 128 * i0:128 * i0 + 128], t[:, 128 * i0:128 * i0 + 128], mask_b, ALU.mult,
                         r=(key, "msb_b", "mmla_b"), w=(key,))
                    P.ts("dve", t[:, 128 * i1:128 * i1 + 128], t[:, 128 * i1:128 * i1 + 128], vis[:, j:j + 1], None,
                         ALU.mult, None, r=(key, "small"), w=(key,))

            if kind == "sb":
                P.act(e_s[:, 0:cols], z[:, 0:cols], AF.Exp, r=(f"ps{zb}",), w=(ke,), scale=scale)
                P.act(sp_s[:, 0:cols], e_s[:, 0:cols], AF.Ln, r=(ke, "ones_f"), w=(ksp,), bias=ones_f[:, 0:1], scale=1.0)
                apply_masks(sp_s, ksp)
                yield
                P.stt("dve", t1_s[:, 0:cols], z[:, 0:cols], scale, sp_s[:, 0:cols], ALU.mult, ALU.subtract,
                      r=(f"ps{zb}", ksp), w=(kt1,))
                for i, b in enumerate(blks):
                    terms = [(triu_b, i, "triu_b")]
                    pair = b // 2
                    if b % 2 == 0:
                        terms.append((pairf_b[:, (pair * 2 + 0) * 128:(pair * 2 + 1) * 128], i + 1, "pairf_b"))
                    else:
                        terms.append((pairf_b[:, (pair * 2 + 1) * 128:(pair * 2 + 2) * 128], i - 1, "pairf_b"))
                    for i2, b2 in enumerate(blks):
                        if b2 // 2 > pair:
                            terms.append((ones_b, i2, "ones_b"))
                    for n, (m, src, mk) in enumerate(terms):
                        P.mm(bt[:, 128 * i:128 * i + 128], m, sp_s[:, 128 * src:128 * src + 128], n == 0,
                             n == len(terms) - 1, r=(mk, ksp), w=(f"ps{btb}",))
                if not last_group:
                    for i in range(nb):
                        P.mm(ps[6][:, 0:128], ones_b, sp_s[:, 128 * i:128 * i + 128], i == 0, i == nb - 1,
                             r=("ones_b", ksp), w=("ps6",))
                oldc = cur
                if not last_group:
                    if gi == 0:
                        P.cp("dve", carry_s[0], ps[6][:, 0:128], r=("ps6",), w=(f"carry{X}0",))
                        cur = 0
                    else:
                        P.tt("dve", carry_s[1 - cur], carry_s[cur], ps[6][:, 0:128], ALU.add, r=(f"carry{X}{cur}", "ps6"),
                             w=(f"carry{X}{1 - cur}",))
                        cur = 1 - cur
                yield
                P.tt("dve", t1_s[:, 0:cols], t1_s[:, 0:cols], bt[:, 0:cols], ALU.subtract, r=(kt1, f"ps{btb}"), w=(kt1,))
                if gi > 0:
                    t3 = t1_s[:, 0:cols].rearrange("p (b t) -> p b t", b=nb)
                    P.tt("dve", t3, t3, carry_s[oldc].unsqueeze(1).to_broadcast([128, nb, 128]), ALU.subtract,
                         r=(kt1, f"carry{X}{oldc}"), w=(kt1,))
                yield
                P.act(w_s[:, 0:cols], t1_s[:, 0:cols], AF.Exp, r=(kt1,), w=(kw,))
            else:
                P.act(w_s[:, 0:cols], z[:, 0:cols], AF.Exp, r=(f"ps{zb}",), w=(kw,), scale=scale)
            apply_masks(w_s, kw)
            yield
            for i, b in enumerate(blks):
                P.mm(o_ps, w_s[:, 128 * i:128 * i + 128], vt[:, b, 0:oc], npv == 0, npv == tot_pv - 1,
                     r=(kw, "vt"), w=(f"ps{ob}",))
                npv += 1
            yield
        hc = h if kind == "sb" else 8 + h
        ssd = ss_sb if kind == "sb" else ss_mla
        sa = stat[:, 64 + sid:65 + sid]
        if kind == "sb":
            P.act(e_s[:, 0:128], o_ps[:, 0:128], AF.Square, r=(f"ps{ob}",), w=(ke, "ssacc" + X), accum_out=sa)
            P.cp("dve", osb_s, o_ps[:, 0:128], r=(f"ps{ob}",), w=(kosb,))
        else:
            P.add("dve", lambda e: e.reciprocal(rinv_s, o_ps[:, 128:129]), r=(f"ps{ob}",), w=(krinv,))
            P.act(e_s[:, 0:128], o_ps[:, 0:128], AF.Square, r=(f"ps{ob}", krinv), w=(ke, "ssacc" + X), scale=rinv_s,
                  accum_out=sa)
            P.ts("dve", osb_s, o_ps[:, 0:128], rinv_s, None, ALU.mult, None, r=(f"ps{ob}", krinv), w=(kosb,))
        P.tt("dve", ssd[:, j:j + 1], ssd[:, j:j + 1], sa, ALU.add, r=("ssacc" + X, "ss"), w=("ss",))
        yield
        P.tr(psb[7][:, 0:128], osb_s, ident_b, r=(kosb, "ident_b"), w=("pst0",))
        P.cp("act", mixT[:, hc, 128 * j:128 * j + 128], psb[7][:, 0:128], r=("pst0",), w=(f"mixT{hc}",))
        yield

    def run_head_attention(kind, h):
        for ja, jb in ((0, 7), (1, 6), (2, 5), (3, 4)):
            gens = [attn_slot(kind, h, ja, 0), attn_slot(kind, h, jb, 1)]
            live = [True, True]
            while any(live):
                for k in range(2):
                    if live[k]:
                        try:
                            next(gens[k])
                        except StopIteration:
                            live[k] = False

    def proj_T(dst, wt, wkey, nck, rhs_of, rkeys, ngrp, dkey, m=128):
        for g in range(ngrp):
            pb = 6 + g % 2
            pkeys = ("ps6",) if pb == 6 else ("pst0", "pst1")
            for c in range(nck):
                P.mm(ps[pb][0:m, :], wt[:, c, :], rhs_of(c, g), c == 0, c == nck - 1, r=wkey + rkeys(c), w=pkeys)
            P.cp("act", dst[:, 512 * g:512 * g + 512], ps[pb][0:m, :], r=pkeys, w=(dkey,))

    def proj_v(wt, wkey, nck, lhs_of, lkeys):
        for q4 in range(4):
            pb = 6 + q4 % 2
            pkeys = ("ps6",) if pb == 6 else ("pst0", "pst1")
            for i in range(4):
                blk = 4 * q4 + i
                for c in range(nck):
                    P.mm(ps[pb][:, 128 * i:128 * i + 128], lhs_of(c, blk), wt[:, c, :], c == 0, c == nck - 1,
                         r=wkey + lkeys(c), w=pkeys)
            P.cp("act", vt[:, 4 * q4:4 * q4 + 4, 0:128], ps[pb].rearrange("p (b d) -> p b d", b=4), r=pkeys, w=("vt",))

    n_heads = H if stage != "fast" else 1
    def load_qkv(h):
        return (load_w(win_cols(h * 128, 128), 128, 16), load_w(win_cols(1024 + h * 128, 128), 128, 16),
                load_w(win_cols(2048 + h * 128, 128), 128, 16))

    nxt = load_qkv(0)
    for h in range(n_heads):
        (wq, wqk), (wk_, wkk), (wv, wvk) = nxt
        proj_T(qT, wq, wqk, 16, lambda c, g: own_tok(h0T, c, g), lambda c: (f"h0T{c}",), 2, "qT")
        proj_T(kT, wk_, wkk, 16, lambda c, g: h0T[:, c, 512 * g:512 * g + 512], lambda c: (f"h0T{c}",), 4, "kT")
        proj_v(wv, wvk, 16, lambda c, blk: h0T[:, c, 128 * blk:128 * blk + 128], lambda c: (f"h0T{c}",))
        if h == 0:
            chk("proj_k", kT, 2048)
            chk("proj_q", qT, 1024)
            chk("proj_v", vt[:, :, 0:128], 2048)
        if h + 1 < n_heads:
            nxt = load_qkv(h + 1)
        run_head_attention("sb", h)
        if h == 0:
            chk("sb0", mixT[:, 0, :], 1024)

    wqb = A.bf(O_H0T, 4 * 1536).rearrange("p (c n) -> p c n", c=4)
    wqb_k = ("h0T0", "h0T1", "h0T2")
    wkvb = A.bf(O_H0T + 12288, 2 * 2048).rearrange("p (c n) -> p c n", c=2)
    wkvb_k = ("h0T3", "h0T4")
    wqrot = A.bf(O_H0T + 20480, 4 * 512).rearrange("p (c n) -> p c n", c=4)
    wqrot_k = ("h0T5",)
    if stage != "fast":
        pass
    for q3 in range(3):
        load_w(w_q_b[:, 512 * q3:512 * q3 + 512].rearrange("(c p) n -> p c n", p=128), 512, 4, scale_ap=qan,
               dst=(wqb[:, :, 512 * q3:512 * q3 + 512], wqb_k))
    for q2 in range(2):
        load_w(w_kv_b[:, 1024 * q2:1024 * q2 + 1024].rearrange("(c p) n -> p c n", p=128), 1024, 2, scale_ap=kvan,
               dst=(wkvb[:, :, 1024 * q2:1024 * q2 + 1024], wkvb_k))
    for h in range(H):
        make_rot(wqrot[:, :, 64 * h:64 * h + 64], wqb[:, :, 192 * h + 128:192 * h + 192], 4, wqrot_k, wqb_k)

    def own4(ap2, g):
        return ap2.rearrange("p (b two t) -> p b two t", two=2, t=128)[:, 4 * g:4 * g + 4, 0, :]

    v3 = lambda ap: ap.rearrange("p (b t) -> p b t", b=4)

    for h in range(n_heads):
        proj_T(kT, wkvb[:, :, 256 * h:256 * h + 128], wkvb_k, 2, lambda c, g: kvlatnT[:, c, 512 * g:512 * g + 512],
               lambda c: ("kvlatnT",), 4, "kT")
        proj_v(wkvb[:, :, 256 * h + 128:256 * h + 256], wkvb_k, 2, lambda c, blk: kvlatnT[:, c, 128 * blk:128 * blk + 128],
               lambda c: ("kvlatnT",))
        proj_T(qT, wqb[:, :, 192 * h:192 * h + 128], wqb_k, 4, lambda c, g: qlatnT[:, c, 512 * g:512 * g + 512],
               lambda c: ("qlatnT",), 2, "qT")
        for g in range(2):
            for c in range(4):
                P.mm(ps[0][0:64, :], wqb[:, c, 192 * h + 128:192 * h + 192], qlatnT[:, c, 512 * g:512 * g + 512],
                     c == 0, c == 3, r=wqb_k + ("qlatnT",), w=("ps0",))
            for c in range(4):
                P.mm(ps[1][0:64, :], wqrot[:, c, 64 * h:64 * h + 64], qlatnT[:, c, 512 * g:512 * g + 512],
                     c == 0, c == 3, r=wqrot_k + ("qlatnT",), w=("ps1",))
            P.tt("dve", v3(t1_t[0:64, :]), v3(ps[0][0:64, :]), own4(cos2, g), ALU.mult, r=("ps0", "cos2"), w=("t1_t",))
            P.tt("dve", v3(e_t[0:64, :]), v3(ps[1][0:64, :]), own4(sin2, g), ALU.mult, r=("ps1", "sin2"), w=("e_t",))
            P.tt("pool", qpeT[:, 512 * g:512 * g + 512], t1_t[0:64, :], e_t[0:64, :], ALU.add, r=("t1_t", "e_t"), w=("qpeT",))
        if h == 0:
            chk("mproj_k", kT, 2048)
            chk("mproj_q", qT, 1024)
            chk("mproj_qpe", qpeT, 1024, 64)
            chk("mproj_v", vt[:, :, 0:128], 2048)
        run_head_attention("mla", h)
        if h == 0:
            chk("mla0", mixT[:, 8, :], 1024)

    P.fence()

    acc = A.f32(O_H0T, NS * D).rearrange("p (j d) -> p j d", j=NS)
    acck = lambda j: (f"acc{j}",)
    h1bf = A.bf(O_MIX, NS * D).rearrange("p (j d) -> p j d", j=NS)
    gbc = A.f32(O_Z, D)
    bbc = A.f32(O_Z + 8192, D)
    xt2 = A.f32(O_Z + 16384, D)
    wso = [A.f32(O_Z + 24576 + 16384 * i, 8 * 512).rearrange("p (c n) -> p c n", c=8) for i in range(2)]
    wbo = [A.bf(O_Z + 57344 + 8192 * i, 8 * 512).rearrange("p (c n) -> p c n", c=8) for i in range(2)]
    h1T = A.f32(O_Z + 75776, 16 * 128).rearrange("p (c t) -> p c t", c=16)
    wr = A.f32(O_Z + 83968, 16 * E).rearrange("p (c n) -> p c n", c=16)
    rt = A.f32(O_Z + 86016, 8 * E)
    lg = rt[:, 0:32]
    m8 = rt[:, 32:40]
    negm = rt[:, 40:41]
    den = rt[:, 41:42]
    ex = rt[:, 64:96]
    brt = rt[:, 96:128]
    Gt = A.f32(O_Z + 90112, NS * E).rearrange("p (j e) -> p j e", j=NS)
    rankm = A.f32(O_Z + 91136, NS * E).rearrange("p (j e) -> p j e", j=NS)
    routed = A.f32(O_Z + 92160, NS * E).rearrange("p (j e) -> p j e", j=NS)

    P.dma("gbc", gbc, ln_in_g.partition_broadcast(128), r=(), w=("gbc",))
    P.dma("bbc", bbc, ln_in_b.partition_broadcast(128), r=(), w=("bbc",))
    P.add("act", lambda e: e.mul(gbc, gbc, ALPHA), r=("gbc",), w=("gbc",))
    P.add("act", lambda e: e.mul(bbc, bbc, ALPHA), r=("bbc",), w=("bbc",))
    for j in range(NS):
        P.dma("xt2", xt2, xp[2 * j], r=(), w=("xt2",))
        ln_stats(xt2, "xt2")
        P.act(xt2, xt2, AF.Identity, r=("xt2", "rstd", "nmr"), w=("xt2",), bias=stat[:, 28:29], scale=stat[:, 27:28])
        P.tt("dve", acc[:, j, :], xt2, gbc, ALU.mult, r=("xt2", "gbc"), w=acck(j))
        P.tt("pool", acc[:, j, :], acc[:, j, :], bbc, ALU.add, r=acck(j) + ("bbc",), w=acck(j))

    P.act(stat[:, 48:64], stat[:, 32:48], AF.Sqrt, r=("ss", "eps"), w=("rstdo",), bias=eps_t[:, 1:2], scale=1.0 / 1024.0)
    P.add("dve", lambda e: e.reciprocal(stat[:, 48:64], stat[:, 48:64]), r=("rstdo",), w=("rstdo",))
    def load_wo(idx):
        dblk, half = idx // 2, idx % 2
        si = idx % 2
        src = w_o[1024 * half:1024 * half + 1024, 512 * dblk:512 * dblk + 512].rearrange("(c p) n -> p c n", p=128)
        P.dma(f"wso{si}", wso[si], src, r=(), w=(f"wso{si}",))
        for c in range(8):
            sc = onorm[:, 8 * half + c:8 * half + c + 1]
            if c % 2 == 0:
                P.ts("dve", wbo[si][:, c, :], wso[si][:, c, :], sc, None, ALU.mult, None, r=(f"wso{si}", "onorm"), w=(f"wbo{si}",))
            else:
                P.act(wbo[si][:, c, :], wso[si][:, c, :], AF.Identity, r=(f"wso{si}", "onorm"), w=(f"wbo{si}",), scale=sc)

    load_wo(0)
    for idx in range(8):
        dblk, half = idx // 2, idx % 2
        si = idx % 2
        if idx + 1 < 8:
            load_wo(idx + 1)
        for j in range(NS):
            pb = 6 + j % 2
            pkeys = ("ps6",) if pb == 6 else ("pst0", "pst1")
            for c in range(8):
                P.mm(ps[pb], mixT[:, 8 * half + c, 128 * j:128 * j + 128], wbo[si][:, c, :], c == 0, c == 7,
                     r=(f"mixT{8 * half + c}", f"wbo{si}"), w=pkeys)
            dst = acc[:, j, 512 * dblk:512 * dblk + 512]
            P.stt("dve", dst, ps[pb], stat[:, 48 + 8 * half + j:48 + 8 * half + j + 1], dst, ALU.mult, ALU.add,
                  r=pkeys + acck(j) + ("rstdo",), w=acck(j))

    P.fence()
    P.dma("gbc", gbc, ln_mix_g.partition_broadcast(128), r=(), w=("gbc",))
    P.dma("bbc", bbc, ln_mix_b.partition_broadcast(128), r=(), w=("bbc",))
    P.dma("wr", wr, w_router.rearrange("(c p) n -> p c n", p=128), r=(), w=("wr",))
    P.dma("brt", brt, b_router.partition_broadcast(128), r=(), w=("brt",))
    for j in range(NS):
        ln_stats(acc[:, j, :], f"acc{j}")
        P.act(xt2, acc[:, j, :], AF.Identity, r=acck(j) + ("rstd", "nmr"), w=("xt2",), bias=stat[:, 28:29], scale=stat[:, 27:28])
        P.tt("dve", xt2, xt2, gbc, ALU.mult, r=("xt2", "gbc"), w=("xt2",))
        P.tt("pool", acc[:, j, :], xt2, bbc, ALU.add, r=("xt2", "bbc"), w=acck(j))
        P.cp("pool", h1bf[:, j, :], acc[:, j, :], r=acck(j), w=(f"h1bf{j}",))
        for c4 in range(4):
            pb = 6 + c4 % 2
            pkeys = ("ps6",) if pb == 6 else ("pst0", "pst1")
            for i in range(4):
                c = 4 * c4 + i
                P.tr(ps[pb][:, 128 * i:128 * i + 128], acc[:, j, 128 * c:128 * c + 128], ident_f, r=acck(j) + ("ident_f",), w=pkeys)
            P.cp("act", h1T[:, 4 * c4:4 * c4 + 4, :], ps[pb].rearrange("p (i t) -> p i t", i=4), r=pkeys, w=("h1T",))
        for c in range(16):
            P.mm(ps[4][:, 0:E], h1T[:, c, :], wr[:, c, :], c == 0, c == 15, r=("h1T", "wr"), w=("ps4",))
        P.tt("dve", lg, ps[4][:, 0:E], brt, ALU.add, r=("ps4", "brt"), w=("lg",))
        P.add("dve", lambda e: e.max(m8, lg), r=("lg",), w=("m8",))
        P.ts("dve", routed[:, j, :], lg, m8[:, 3:4], None, ALU.is_ge, None, r=("lg", "m8"), w=("routed",))
        P.ts("dve", negm, m8[:, 0:1], -1.0, None, ALU.mult, None, r=("m8",), w=("negm",))
        P.act(ex, lg, AF.Exp, r=("lg", "negm"), w=("ex",), bias=negm, scale=1.0)
        P.tt("dve", ex, ex, routed[:, j, :], ALU.mult, r=("ex", "routed"), w=("ex",))
        P.add("dve", lambda e: e.reduce_sum(den, ex, AX.X), r=("ex",), w=("den",))
        P.add("dve", lambda e: e.reciprocal(den, den), r=("den",), w=("den",))
        P.ts("dve", Gt[:, j, :], ex, den, None, ALU.mult, None, r=("ex", "den"), w=("Gt",))
        for j2 in range(j + 1):
            P.mm(ps[5][:, 0:E], (tril_f if j2 == j else ones_f), routed[:, j2, :], j2 == 0, j2 == j,
                 r=("tril_f", "ones_f", "routed"), w=("ps5",))
        P.stt("dve", rankm[:, j, :], ps[5][:, 0:E], 1.0, routed[:, j, :], ALU.add, ALU.mult, r=("ps5", "routed"), w=("rankm",))
        P.ts("dve", rankm[:, j, :], rankm[:, j, :], -1.0, None, ALU.add, None, r=("rankm",), w=("rankm",))
        P.add("act", (lambda j: lambda e: e.mul(acc[:, j, :], acc[:, j, :], ALPHA))(j), r=acck(j), w=acck(j))

    if stage == "A":
        P.fence()
        for j in range(NS):
            P.dma(f"out{j % 2}", out[j], acc[:, j, :], r=acck(j), w=())
        P.emit(stack)
        return

    P.fence()

    wgu_t = w_gu
    wdn_t = w_dn
    NSTG = 4
    wsM = [A.f32(O_Z + 8192 * i, 2048).rearrange("p (c n) -> p c n", c=16) for i in range(NSTG)]
    wbM = [A.bf(O_Z + 32768 + 4096 * i, 2048).rearrange("p (c n) -> p c n", c=16) for i in range(3)]
    Sel = A.bf(O_Z + 45056, NS * CAP).rearrange("p (j r) -> p j r", j=NS)
    SelG = A.bf(O_Z + 49152, NS * CAP).rearrange("p (j r) -> p j r", j=NS)
    SelGT = A.bf(O_Z + 53248, 2 * 1024).rearrange("p (c t) -> p c t", c=2)
    xT = A.bf(O_Z + 57344, 16 * CAP).rearrange("p (c r) -> p c r", c=16)
    gs = A.bf(O_Z + 65536, 16 * CAP).rearrange("p (c r) -> p c r", c=16)
    yb = A.bf(O_Z + 65536, 2 * D).rearrange("p (c d) -> p c d", c=2)
    actT = A.bf(O_Z + 73728, 16 * CAP).rearrange("p (c r) -> p c r", c=16)
    gtmp = A.f32(O_Z + 81920, CAP)
    sgt = A.f32(O_Z + 82944, CAP)
    utmp = A.f32(O_Z + 83968, CAP)
    bgu_t = A.f32(O_Z + 84992, n_exp * 32)
    P.dma("bgu", bgu_t, bgu, r=(), w=("bgu",))

    mcnt = [0, 0, 0]
    cast_pat = ("dve", "dve", "act")
    tile_src = []
    for e_i in range(n_exp):
        tile_src += [wgu_t[e_i, fb] for fb in range(32)] + [wdn_t[e_i, t] for t in range(16)]
    issued = []
    PF = 2

    def _issue(k):
        si = mcnt[0] % NSTG
        bi = mcnt[1] % 3
        mcnt[0] += 1
        mcnt[1] += 1
        P.dma(f"wsM{si}", wsM[si], tile_src[k].rearrange("p (c n) -> p c n", c=16), r=(), w=(f"wsM{si}",))
        eng = cast_pat[mcnt[2] % len(cast_pat)]
        mcnt[2] += 1
        P.cp(eng, wbM[bi], wsM[si], r=(f"wsM{si}",), w=(f"wbM{bi}",))
        issued.append((wbM[bi], f"wbM{bi}"))

    def get_tile(k):
        while len(issued) <= min(k + PF, len(tile_src) - 1):
            _issue(len(issued))
        return issued[k]

    for e_i in range(n_exp):
        for j in range(NS):
            P.ts("dve", Sel[:, j, :], iota, rankm[:, j, e_i:e_i + 1], None, ALU.is_equal, None, r=("iota", "rankm"), w=("Sel",))
            P.ts("pool", SelG[:, j, :], iota, rankm[:, j, e_i:e_i + 1], Gt[:, j, e_i:e_i + 1], ALU.is_equal, ALU.mult,
                 r=("iota", "rankm", "Gt"), w=("SelG",))
        for d2 in range(8):
            pb = d2 % 2
            for i in range(2):
                dc = 2 * d2 + i
                for j in range(NS):
                    P.mm(ps[pb][:, CAP * i:CAP * i + CAP], h1bf[:, j, 128 * dc:128 * dc + 128], Sel[:, j, :], j == 0, j == NS - 1,
                         r=(f"h1bf{j}", "Sel"), w=(f"ps{pb}",))
            P.cp("act", xT[:, 2 * d2:2 * d2 + 2, :], ps[pb].rearrange("p (i r) -> p i r", i=2), r=(f"ps{pb}",), w=("xT",))
        for fb in range(32):
            wt, wk = get_tile(48 * e_i + fb)
            pb = 2 + fb % 2
            for c in range(16):
                P.mm(ps[pb][:, 0:CAP], wt[:, c, :], xT[:, c, :], c == 0, c == 15, r=(wk, "xT"), w=(f"ps{pb}",))
            bcol = bgu_t[:, e_i * 32 + fb:e_i * 32 + fb + 1]
            if fb < 16:
                P.ts("dve", gtmp, ps[pb][:, 0:CAP], bcol, 7.0, ALU.add, ALU.min, r=(f"ps{pb}", "bgu"), w=("gtmp",))
                P.act(sgt, gtmp, AF.Sigmoid, r=("gtmp",), w=("sgt",), scale=1.702)
                P.tt("pool", gs[:, fb, :], gtmp, sgt, ALU.mult, r=("gtmp", "sgt"), w=("gs",))
            else:
                P.act(utmp, ps[pb][:, 0:CAP], AF.Identity, r=(f"ps{pb}", "bgu"), w=("utmp",), bias=bcol, scale=1.0)
                P.ts("dve", utmp, utmp, -7.0, 7.0, ALU.max, ALU.min, r=("utmp",), w=("utmp",))
                P.stt("dve", actT[:, fb - 16, :], utmp, 1.0, gs[:, fb - 16, :], ALU.add, ALU.mult, r=("utmp", "gs"), w=("actT",))
        for q in range(4):
            for kq in range(4):
                wt, wk = get_tile(48 * e_i + 32 + 4 * q + kq)
                wt4 = wt.rearrange("p c n -> p (c n)").rearrange("p (c n) -> p c n", c=4)
                for c4 in range(4):
                    c = 4 * kq + c4
                    for rc in range(2):
                        P.mm(ps[4 + rc], actT[:, c, 128 * rc:128 * rc + 128], wt4[:, c4, :], c == 0, c == 15,
                             r=(wk, "actT"), w=(f"ps{4 + rc}",))
            for rc in range(2):
                P.cp("act", yb[:, rc, 512 * q:512 * q + 512], ps[4 + rc], r=(f"ps{4 + rc}",), w=("gs",))
        for rc in range(2):
            for j4 in range(2):
                half = (2 * rc + j4) % 2
                pst = psb[7 - half][:, 0:512]
                for i in range(4):
                    j = 4 * j4 + i
                    P.tr(pst[:, 128 * i:128 * i + 128], SelG[:, j, 128 * rc:128 * rc + 128], ident_b, r=("SelG", "ident_b"),
                         w=(bkey[half],))
                P.cp("act", SelGT[:, rc, 512 * j4:512 * j4 + 512], pst, r=(bkey[half],), w=("SelGT",))
        for j in range(NS):
            for dq in range(4):
                pb = dq % 2
                for rc in range(2):
                    P.mm(ps[pb], SelGT[:, rc, 128 * j:128 * j + 128], yb[:, rc, 512 * dq:512 * dq + 512], rc == 0, rc == 1,
                         r=("SelGT", "gs"), w=(f"ps{pb}",))
                dst = acc[:, j, 512 * dq:512 * dq + 512]
                P.tt("dve", dst, dst, ps[pb], ALU.add, r=acck(j) + (f"ps{pb}",), w=acck(j))

    P.fence()

    bdt = A.f32(O_Z, D)
    gT = A.f32(O_Z + 8192, 128)
    P.dma("bdt", bdt[0:n_exp, :], b_dn, r=(), w=("bdt",))
    for j in range(NS):
        P.tr(ps[6][0:n_exp, 0:128], Gt[:, j, 0:n_exp], ident_f, r=("Gt", "ident_f"), w=("ps6",))
        P.cp("act", gT[0:n_exp, :], ps[6][0:n_exp, 0:128], r=("ps6",), w=("gT",))
        for dq in range(4):
            pb = dq % 2
            P.mm(ps[pb], gT[0:n_exp, :], bdt[0:n_exp, 512 * dq:512 * dq + 512], True, True, r=("gT", "bdt"), w=(f"ps{pb}",))
            dst = acc[:, j, 512 * dq:512 * dq + 512]
            P.tt("dve", dst, dst, ps[pb], ALU.add, r=acck(j) + (f"ps{pb}",), w=acck(j))

    P.fence()

    P.dma("gbc", gbc, ln_ffn_g.partition_broadcast(128), r=(), w=("gbc",))
    P.dma("bbc", bbc, ln_ffn_b.partition_broadcast(128), r=(), w=("bbc",))
    fb2 = [A.f32(O_Z + 16384 + 8192 * i, D) for i in range(2)]
    for j in range(NS):
        fo = fb2[j % 2]
        ln_stats(acc[:, j, :], f"acc{j}")
        P.act(fo, acc[:, j, :], AF.Identity, r=acck(j) + ("rstd", "nmr"), w=(f"fo{j % 2}",), bias=stat[:, 28:29], scale=stat[:, 27:28])
        P.tt("dve", fo, fo, gbc, ALU.mult, r=(f"fo{j % 2}", "gbc"), w=(f"fo{j % 2}",))
        P.tt("pool", fo, fo, bbc, ALU.add, r=(f"fo{j % 2}", "bbc"), w=(f"fo{j % 2}",))
        P.dma(f"out{j % 2}", out[j], fo, r=(f"fo{j % 2}",), w=())
    P.emit(stack)


def _host_consts(g):
    own = QA if g == 0 else QB
    oth = QB if g == 0 else QA
    k = np.arange(128)
    ident = np.eye(128, dtype=np.float32)
    triu = (k[:, None] > k[None, :]).astype(np.float32)
    tril = (k[:, None] < k[None, :]).astype(np.float32)
    msb = (k[:, None] < k[None, :]).astype(np.float32)
    mmla = ((k[:, None] // 64) <= (k[None, :] // 64)).astype(np.float32)
    pairf = np.zeros((128, NS, 2, 128), np.float32)
    small = np.zeros((128, 64), np.float32)
    for i in range(NS):
        f = 1.0 if oth[i] > own[i] else 0.0
        pairf[:, i, 0, :] = f
        pairf[:, i, 1, :] = 1.0 - f
        small[:, i] = 1.0 - f
    inv_freq = (10000.0 ** (-(np.arange(32, dtype=np.float32) * 2.0 / 64.0))).astype(np.float32)
    small[:64, 8] = np.concatenate([inv_freq, inv_freq])
    iota = np.broadcast_to(np.arange(CAP, dtype=np.float32)[None, :], (128, CAP)).copy()
    return dict(c_ident=ident, c_triu=triu, c_tril=tril, c_msb=msb, c_mmla=mmla,
                c_pairf=pairf.reshape(128, -1), c_iota=iota), small


def _pc(v, nchunk):
    return np.ascontiguousarray(np.asarray(v, np.float32).reshape(nchunk, 128).T)


def _prepare(inputs, n_exp=E):
    f = lambda k: np.asarray(inputs[k])
    x = f("x").astype(np.float32, copy=False)
    pos = f("positions").astype(np.int32, copy=False)
    wgu = f("w_gate_up")[0][:n_exp]
    wdn = f("w_down")[0][:n_exp]
    wgu_t = np.ascontiguousarray(wgu.reshape(n_exp, 16, 128, 32, 128).transpose(0, 3, 2, 1, 4)).reshape(n_exp, 32, 128, 2048)
    wdn_t = np.ascontiguousarray(wdn.reshape(n_exp, 4, 4, 128, 4, 512).transpose(0, 4, 1, 3, 2, 5)).reshape(n_exp, 16, 128, 2048)
    bgu = np.ascontiguousarray(f("b_gate_up")[0][:n_exp].reshape(n_exp, 32, 128).transpose(2, 0, 1)).reshape(128, n_exp * 32)
    shared = dict(
        w_in=np.ascontiguousarray(f("w_in")[0]), w_q_b=np.ascontiguousarray(f("w_q_b")[0]),
        w_kv_b=np.ascontiguousarray(f("w_kv_b")[0]), w_o=np.ascontiguousarray(f("w_o")[0]),
        ln_in_g=f("ln_in_g"), ln_in_b=f("ln_in_b"), ln_mix_g=f("ln_mix_g")[0], ln_mix_b=f("ln_mix_b")[0],
        ln_ffn_g=f("ln_ffn_g")[0], ln_ffn_b=f("ln_ffn_b")[0], w_router=np.ascontiguousarray(f("w_router")[0]),
        b_router=f("b_router")[0], w_gu=wgu_t, bgu=bgu, w_dn=wdn_t, b_dn=np.ascontiguousarray(f("b_down")[0][:n_exp]),
        c_onorm=_pc(np.concatenate([f("sb_out_norm")[0], f("mla_out_norm")[0]]), 16),
    )
    shared = {k: np.ascontiguousarray(v, dtype=np.float32) for k, v in shared.items()}
    in_maps = []
    for c in range(8):
        b, g = c // 2, c % 2
        own = QA if g == 0 else QB
        oth = QB if g == 0 else QA
        order = []
        for i in range(NS):
            order += [own[i], oth[i]]
        xb = x[b].reshape(NB, 128, D)[order]
        pb = pos[b].reshape(NB, 128)[order].reshape(-1)
        consts, small = _host_consts(g)
        small = small.copy()
        small[:, 16:32] = _pc(f("ln_in_g"), 16)
        small[:, 32:48] = _pc(f("ln_in_b"), 16)
        small[:, 48:52] = _pc(f("q_a_norm")[0], 4)
        small[:, 52:54] = _pc(f("kv_a_norm")[0], 2)
        m = dict(shared)
        m.update(consts)
        m["c_small"] = small
        m["xp"] = np.ascontiguousarray(xb)
        m["posb"] = np.ascontiguousarray(pb.astype(np.int32))
        in_maps.append(m)
    return in_maps


def _assemble(results):
    out = np.zeros((4, S, D), np.float32)
    for c in range(8):
        b, g = c // 2, c % 2
        own = QA if g == 0 else QB
        o = np.asarray(results[c]["out"], np.float32)
        for j in range(NS):
            out[b, own[j] * 128:(own[j] + 1) * 128, :] = o[j]
    return out


def kernel(**inputs):
    from contextlib import ExitStack
    in_maps = _prepare(inputs)
    nc = bass.Bass("TRN2", target_bir_lowering=False)
    with ExitStack() as stack:
        build(nc, stack)
    res = run_bass_kernel_spmd(nc, in_maps, core_ids=list(range(8)))
    return _assemble(res.results)
```

```python
import numpy as np
import concourse.bass as bass
import concourse.mybir as mybir
from concourse.bass_utils import run_bass_kernel_spmd

F32 = mybir.dt.float32
BF16 = mybir.dt.bfloat16
I32 = mybir.dt.int32
AF = mybir.ActivationFunctionType
ALU = mybir.AluOpType
AX = mybir.AxisListType

D = 2048
S = 2048
NB = 16
NS = 8
H = 8
INP = 3904
E = 32
DFF = 2048
CAP = 256
ALPHA = 2.0 ** 0.25
LN_EPS = 1e-5
RMS_EPS = 1e-6
SEG = 12000
import os as _os
DEBUG_EMIT = bool(_os.environ.get("DEBUG_EMIT"))

QA = [0, 3, 4, 7, 8, 11, 12, 15]
QB = [1, 2, 5, 6, 9, 10, 13, 14]


class Prog:
    ENG = ("pe", "act", "dve", "pool", "sp")

    def __init__(self, nc):
        self.nc = nc
        self.ops = []
        self.lastw = {}
        self.readers = {}
        self.dma_cnt = {}

    def add(self, eng, fn, r=(), w=(), dma=None):
        i = len(self.ops)
        deps = set()
        for b in list(r) + list(w):
            if b in self.lastw:
                deps.add(self.lastw[b])
        for b in w:
            for x in self.readers.get(b, ()):
                deps.add(x)
        for b in r:
            if b.startswith("ps"):
                for x in self.readers.get(b, ()):
                    if self.ops[x]["eng"] != eng:
                        deps.add(x)
        deps.discard(i)
        op = dict(id=i, eng=eng, fn=fn, deps=deps, dma=dma, sig=False, r=tuple(r), w=tuple(w))
        if dma is not None:
            self.dma_cnt[dma] = self.dma_cnt.get(dma, 0) + 1
            op["dval"] = 16 * self.dma_cnt[dma]
        self.ops.append(op)
        for b in w:
            self.lastw[b] = i
            self.readers[b] = []
        for b in r:
            if b not in w:
                self.readers.setdefault(b, []).append(i)
        return i

    def emit(self, stack):
        nc = self.nc
        ops = self.ops
        for op in ops:
            nd = set()
            for d in op["deps"]:
                dop = ops[d]
                if op["fn"] is None:
                    if dop["dma"] is None and dop["eng"] == op["eng"]:
                        continue
                    nd.add(d)
                    continue
                if dop["dma"] is None and dop["eng"] == op["eng"] and op["dma"] is None:
                    if op["eng"] == "pe":
                        continue
                    wr = set(dop["w"])
                    if not (wr & set(op["r"])) and not (wr & set(op["w"])):
                        continue
                nd.add(d)
            op["deps"] = nd
            for d in nd:
                ops[d]["sig"] = True
        cnt = {e: 0 for e in self.ENG}
        for op in ops:
            if op["dma"] is None and op["sig"]:
                cnt[op["eng"]] += 1
                op["sidx"] = cnt[op["eng"]]
        nseg = {e: (cnt[e] + SEG - 1) // SEG for e in self.ENG}
        sems = {}
        for e in self.ENG:
            for k in range(max(1, nseg[e])):
                sems[(e, k)] = stack.enter_context(nc.semaphore(f"s_{e}_{k}"))
        dsem = {}
        for name in self.dma_cnt:
            dsem[name] = stack.enter_context(nc.semaphore(f"d_{name}"))

        def token(op):
            if op["dma"] is not None:
                return ("d", op["dma"]), dsem[op["dma"]], op["dval"]
            k = (op["sidx"] - 1) // SEG
            return (op["eng"], k), sems[(op["eng"], k)], (op["sidx"] - 1) % SEG + 1

        out_dmas = [op for op in ops if op.get("dma") and op["dma"].startswith("out")]
        block = stack.enter_context(nc.Block())
        handles = {"pe": block.tensor, "act": block.scalar, "dve": block.vector,
                   "pool": block.gpsimd, "sp": block.sync}

        def make(engname):
            def body(eng):
                known = {}
                for op in ops:
                    if op["eng"] != engname:
                        continue
                    need = {}
                    for d in op["deps"]:
                        key, sem, val = token(ops[d])
                        if known.get(key, 0) >= val:
                            continue
                        if key not in need or need[key][1] < val:
                            need[key] = (sem, val)
                    for key, (sem, val) in need.items():
                        eng.wait_ge(sem, val)
                        known[key] = val
                        if DEBUG_EMIT:
                            print(f"  [{engname}] wait {key} >= {val}")
                    if DEBUG_EMIT:
                        tk = token(op) if (op["dma"] is not None or op["sig"]) else None
                        print(f"  [{engname}] op{op['id']} r={op['r']} w={op['w']} sig={tk and (tk[0], tk[2])}")
                    if op["fn"] is None:
                        continue
                    ins = op["fn"](eng)
                    if op["dma"] is not None:
                        ins.then_inc(dsem[op["dma"]], 16)
                    elif op["sig"]:
                        _, sem, _ = token(op)
                        ins.then_inc(sem, 1)
                if engname == "sp":
                    last = {}
                    for op in out_dmas:
                        last[op["dma"]] = op["dval"]
                    for name, val in last.items():
                        eng.wait_ge(dsem[name], val)
            return body

        for e in self.ENG:
            handles[e](make(e))

    def fence(self):
        last = {}
        for op in self.ops:
            if op["fn"] is None:
                continue
            k = ("d", op["dma"]) if op["dma"] is not None else op["eng"]
            last[k] = op["id"]
        ids = set(last.values())
        for e in self.ENG:
            i = len(self.ops)
            self.ops.append(dict(id=i, eng=e, fn=None, deps=set(ids), dma=None, sig=False, r=(), w=()))
        self.lastw = {}
        self.readers = {}

    def mm(self, out, lhsT, rhs, start, stop, r, w):
        return self.add("pe", lambda e: e.matmul(out, lhsT, rhs, start=start, stop=stop), r=r, w=w)

    def tr(self, out, in_, ident, r, w):
        return self.add("pe", lambda e: e.transpose(out, in_, ident), r=r, w=w)

    def act(self, out, in_, func, r, w, bias=None, scale=None, accum_out=None, eng="act"):
        kw = {}
        if bias is not None:
            kw["bias"] = bias
        if scale is not None:
            kw["scale"] = scale
        if accum_out is not None:
            kw["accum_out"] = accum_out
        return self.add(eng, lambda e: e.activation(out, in_, func, **kw), r=r, w=w)

    def ts(self, eng, out, in0, s1, s2, op0, op1, r, w, accum_out=None):
        if op1 is None:
            return self.add(eng, lambda e: e.tensor_scalar(out, in0, s1, None, op0), r=r, w=w)
        if accum_out is not None:
            return self.add(eng, lambda e: e.tensor_scalar(out, in0, s1, s2, op0, op1, accum_out=accum_out), r=r, w=w)
        return self.add(eng, lambda e: e.tensor_scalar(out, in0, s1, s2, op0, op1), r=r, w=w)

    def tt(self, eng, out, in0, in1, op, r, w):
        return self.add(eng, lambda e: e.tensor_tensor(out, in0, in1, op), r=r, w=w)

    def stt(self, eng, out, in0, scalar, in1, op0, op1, r, w):
        return self.add(eng, lambda e: e.scalar_tensor_tensor(out, in0, scalar, in1, op0, op1), r=r, w=w)

    def cp(self, eng, out, in_, r, w):
        if eng == "act":
            return self.add(eng, lambda e: e.copy(out, in_), r=r, w=w)
        return self.add(eng, lambda e: e.tensor_copy(out, in_), r=r, w=w)

    def ms(self, eng, ap, val, w):
        return self.add(eng, lambda e: e.memset(ap, val), r=(), w=w)

    def dma(self, stream, out, in_, r, w, eng="sp"):
        return self.add(eng, lambda e: e.dma_start(out=out, in_=in_), r=r, w=w, dma=stream)


class Arena:
    def __init__(self, nc, stack, nbytes):
        self.t = stack.enter_context(nc.sbuf_tensor("arena", [128, nbytes // 4], F32))
        self.f = self.t[:]
        self.b = self.f.bitcast(BF16)
        self.i = self.f.bitcast(I32)

    def f32(self, off, n, p=128):
        return self.f[0:p, off // 4: off // 4 + n]

    def bf(self, off, n, p=128):
        return self.b[0:p, off // 2: off // 2 + n]

    def i32(self, off, n, p=128):
        return self.i[0:p, off // 4: off // 4 + n]


ARENA_BYTES = 210944
O_H0T = 10240
O_MIX = 75776
O_Z = 108544

TWO_PI = 2.0 * np.pi
CW1 = float(np.float32(6.28125))
CW2 = float(np.float32(TWO_PI - 6.28125))


class _Done(Exception):
    pass


def build(nc, stack, n_exp=E, stage="full", stop=None):
    P = Prog(nc)
    try:
        _build(nc, stack, P, n_exp, stage, stop)
    except _Done:
        pass
    return P


def _build(nc, stack, P, n_exp, stage, stop):
    A = Arena(nc, stack, ARENA_BYTES)

    def dram(name, shape, dt=F32, kind="ExternalInput"):
        return nc.dram_tensor(name, list(shape), dt, kind=kind).ap()

    xp = dram("xp", [NB, 128, D])
    posb = dram("posb", [S], I32)
    c_ident = dram("c_ident", [128, 128])
    c_triu = dram("c_triu", [128, 128])
    c_tril = dram("c_tril", [128, 128])
    c_msb = dram("c_msb", [128, 128])
    c_mmla = dram("c_mmla", [128, 128])
    c_pairf = dram("c_pairf", [128, NS * 2 * 128])
    c_small = dram("c_small", [128, 64])
    c_onorm = dram("c_onorm", [128, 16])
    c_iota = dram("c_iota", [128, CAP])
    w_in = dram("w_in", [D, INP])
    w_q_b = dram("w_q_b", [512, 1536])
    w_kv_b = dram("w_kv_b", [256, 2048])
    w_o = dram("w_o", [D, D])
    ln_in_g = dram("ln_in_g", [D])
    ln_in_b = dram("ln_in_b", [D])
    ln_mix_g = dram("ln_mix_g", [D])
    ln_mix_b = dram("ln_mix_b", [D])
    ln_ffn_g = dram("ln_ffn_g", [D])
    ln_ffn_b = dram("ln_ffn_b", [D])
    w_router = dram("w_router", [D, E])
    b_router = dram("b_router", [E])
    w_gu = dram("w_gu", [n_exp, 32, 128, 2048])
    b_gu = dram("b_gu", [n_exp, 2 * DFF])
    w_dn = dram("w_dn", [n_exp, 16, 128, 2048])
    b_dn = dram("b_dn", [n_exp, D])
    out = dram("out", [NS, 128, D], kind="ExternalOutput")

    ps = [stack.enter_context(nc.psum_tensor(f"ps{i}", [128, 512], F32))[:] for i in range(8)]

    def chk(name, ap=None, n=0, p=128):
        if stop != name:
            return
        P.fence()
        dbg = A.f32(O_Z + 82048, D)
        if ap is not None:
            P.cp("dve", dbg[0:p, 0:n], ap, r=(), w=("dbg",))
            P.dma("out0", out[0][0:p, 0:n], dbg[0:p, 0:n], r=("dbg",), w=())
        else:
            P.ms("dve", dbg[:, 0:8], 1.0, w=("dbg",))
            P.dma("out0", out[0][:, 0:8], dbg[:, 0:8], r=("dbg",), w=())
        P.emit(stack)
        raise _Done()

    psb = [p.bitcast(BF16) for p in ps]

    ident_f = A.f32(0, 128)
    ident_b = A.bf(512, 128)
    triu_b = A.bf(768, 128)
    ones_b = A.bf(1024, 128)
    msb_b = A.bf(1280, 128)
    mmla_b = A.bf(1536, 128)
    pairf_b = A.bf(1792, NS * 2 * 128)
    small = A.f32(5888, 64)
    vis = small[:, 0:8]
    invf = small[0:64, 8:9]
    lng = small[:, 16:32]
    lnb = small[:, 32:48]
    qan = small[:, 48:52]
    kvan = small[:, 52:54]
    onorm = A.f32(6144, 16)
    iota = A.f32(6208, CAP)
    tril_f = A.f32(7232, 128)
    ones_f = A.f32(7744, 128)
    stat = A.f32(8256, 496)

    tmpc = A.f32(O_Z + 49280, 128 * NS * 2)

    def load_const(name, dst_b, src, n, key):
        P.dma("c0", tmpc[:, 0:n], src, r=(), w=("tmpc",))
        P.cp("dve", dst_b, tmpc[:, 0:n], r=("tmpc",), w=(key,))

    P.dma("c1", ident_f, c_ident, r=(), w=("ident_f",))
    P.dma("c2", small, c_small, r=(), w=("small",))
    P.dma("c3", onorm, c_onorm, r=(), w=("onorm",))
    P.dma("c4", iota, c_iota, r=(), w=("iota",))
    P.dma("c5", tril_f, c_tril, r=(), w=("tril_f",))
    P.cp("dve", ident_b, ident_f, r=("ident_f",), w=("ident_b",))
    load_const("triu", triu_b, c_triu, 128, "triu_b")
    load_const("msb", msb_b, c_msb, 128, "msb_b")
    load_const("mmla", mmla_b, c_mmla, 128, "mmla_b")
    load_const("pairf", pairf_b, c_pairf, NS * 2 * 128, "pairf_b")
    P.ms("dve", ones_b, 1.0, w=("ones_b",))
    P.ms("dve", ones_f, 1.0, w=("ones_f",))

    chk("consts", triu_b, 128)
    h0T = A.bf(O_H0T, 16 * S).rearrange("p (c t) -> p c t", c=16)
    mixT = A.bf(O_MIX, 16 * 1024).rearrange("p (c t) -> p c t", c=16)
    cos2 = A.f32(O_Z, S, p=64)
    sin2 = A.f32(O_Z + 8192, S, p=64)
    kropeT = A.bf(O_Z + 16384, S, p=64)
    qlatnT = A.bf(O_Z + 20480, 4 * 1024).rearrange("p (c t) -> p c t", c=4)
    kvlatnT = A.bf(O_Z + 28672, 2 * S).rearrange("p (c t) -> p c t", c=2)
    kT = A.bf(O_Z + 36864, S)
    vt = A.bf(O_Z + 40960, 16 * 132).rearrange("p (b d) -> p b d", b=16)
    qT = A.bf(O_Z + 45184, 1024)
    qpeT = A.bf(O_Z + 47232, 1024, p=64)
    ws = [A.f32(O_Z + 49280 + 8192 * i, 16 * 128).rearrange("p (c n) -> p c n", c=16) for i in range(2)]
    wb = [A.bf(O_Z + 65664 + 4096 * i, 16 * 128).rearrange("p (c n) -> p c n", c=16) for i in range(4)]
    xt = A.f32(O_Z + 82048, D)
    xn = A.bf(O_Z + 90240, D)
    e_t = A.f32(O_Z + 94336, 512)
    sp_t = A.bf(O_Z + 96384, 512)
    t1_t = A.f32(O_Z + 97408, 512)
    w_t = A.bf(O_Z + 99456, 512)
    carry = [A.f32(O_Z + 100480 + 512 * i, 128) for i in range(2)]
    osb = A.bf(O_Z + 101504, 128)
    rinv = A.f32(O_Z + 101760, 1)

    own_tok = lambda ap3, c, g: ap3[:, c, :].rearrange("p (b two t) -> p b two t", two=2, t=128)[:, 4 * g:4 * g + 4, 0, :]

    r_i = A.i32(O_Z + 49280, S, p=64)
    r_a = A.f32(O_Z + 49280 + 8192, S, p=64)
    r_k = A.f32(O_Z + 49280 + 16384, S, p=64)
    r_r = A.f32(O_Z + 49280 + 24576, S, p=64)
    r_m = A.f32(O_Z + 49280 + 32768, S, p=64)
    P.dma("c6", r_i, posb.partition_broadcast(64), r=("tmpc",), w=("r_i", "tmpc"))
    P.cp("dve", r_a, r_i, r=("r_i", "small"), w=("r_a",))
    P.ts("dve", r_a, r_a, invf, None, ALU.mult, None, r=("r_a", "small"), w=("r_a",))
    P.ts("dve", r_i, r_a, 1.0 / TWO_PI, None, ALU.mult, None, r=("r_a",), w=("r_i",))
    P.cp("dve", r_k, r_i, r=("r_i",), w=("r_k",))
    P.stt("dve", r_r, r_k, -CW1, r_a, ALU.mult, ALU.add, r=("r_k", "r_a"), w=("r_r",))
    P.stt("dve", r_r, r_k, -CW2, r_r, ALU.mult, ALU.add, r=("r_k", "r_r"), w=("r_r",))

    def wrap(x):
        P.ts("dve", r_m, x, float(np.pi), -TWO_PI, ALU.is_gt, ALU.mult, r=("r_r", "r_a"), w=("r_m",))
        P.tt("dve", x, x, r_m, ALU.add, r=("r_m",), w=("r_r", "r_a"))
        P.ts("dve", r_m, x, -float(np.pi), TWO_PI, ALU.is_lt, ALU.mult, r=("r_r", "r_a"), w=("r_m",))
        P.tt("dve", x, x, r_m, ALU.add, r=("r_m",), w=("r_r", "r_a"))

    P.ts("dve", r_a, r_r, float(np.pi / 2), None, ALU.add, None, r=("r_r",), w=("r_a",))
    wrap(r_r)
    wrap(r_a)
    ovl = ("ws0", "ws1", "wb0", "wb1", "wb2", "wb3", "xt", "tmpc")
    P.act(sin2, r_r, AF.Sin, r=("r_r",), w=("sin2",) + ovl)
    P.act(cos2, r_a, AF.Sin, r=("r_a",), w=("cos2",) + ovl)

    chk("rope", cos2, 2048, 64)
    def ln_stats(src, key):
        for q in range(4):
            P.add("dve", (lambda q: lambda e: e.bn_stats(stat[:, 6 * q:6 * q + 6], src[:, 512 * q:512 * q + 512]))(q),
                  r=(key,), w=("bnst",))
        P.add("dve", lambda e: e.bn_aggr(stat[:, 24:26], stat[:, 0:24]), r=("bnst",), w=("mv",))
        P.act(stat[:, 26:27], stat[:, 25:26], AF.Sqrt, r=("mv",), w=("std",), bias=eps_t[:, 0:1], scale=1.0)
        P.add("dve", lambda e: e.reciprocal(stat[:, 27:28], stat[:, 26:27]), r=("std",), w=("rstd",))
        P.ts("dve", stat[:, 28:29], stat[:, 24:25], -1.0, stat[:, 27:28], ALU.mult, ALU.mult,
             r=("mv", "rstd"), w=("nmr",))

    eps_t = A.f32(8256 + 4 * 480, 4)
    P.ms("dve", eps_t[:, 0:1], LN_EPS, w=("eps",))
    P.ms("dve", eps_t[:, 1:2], RMS_EPS, w=("eps",))

    bkey = ("pst0", "ps6")
    for blk in range(NB):
        P.dma("xt", xt, xp[blk], r=(), w=("xt",))
        ln_stats(xt, "xt")
        if blk == 0:
            chk("ln_s", stat[:, 24:29], 5)
        P.act(xn, xt, AF.Identity, r=("xt", "rstd", "nmr"), w=("xn",), bias=stat[:, 28:29], scale=stat[:, 27:28])
        if blk == 0:
            chk("ln_x", xn, 2048)
        if blk == 1:
            chk("ln_b0", h0T[:, 3, :], 2048)
        for c4 in range(4):
            half = c4 % 2
            pst = psb[7 - half][:, 0:512].rearrange("p (i t) -> p i t", i=4)
            for i in range(4):
                c = 4 * c4 + i
                P.tr(pst[:, i, :], xn[:, 128 * c:128 * c + 128], ident_b, r=("xn", "ident_b"), w=(bkey[half],))
            if blk == 0 and c4 == 0:
                chk("ln_t", psb[7][:, 0:512], 512)
            if blk == 0 and c4 == 1:
                chk("ln_t1", psb[6][:, 0:512], 512)
            for i in range(4):
                c = 4 * c4 + i
                import os
                evm = os.environ.get("EVM", "mix")
                eng = "dve" if half == 0 else "pool"
                if evm == "dve":
                    eng = "dve"
                if evm == "act":
                    eng = "pool"
                if eng == "pool":
                    P.act(h0T[:, c, 128 * blk:128 * blk + 128], pst[:, i, :], AF.Identity,
                          r=(bkey[half], "small"), w=(f"h0T{c}",), bias=lnb[:, c:c + 1], scale=lng[:, c:c + 1])
                else:
                    P.ts("dve", h0T[:, c, 128 * blk:128 * blk + 128], pst[:, i, :], lng[:, c:c + 1], lnb[:, c:c + 1],
                         ALU.mult, ALU.add, r=(bkey[half], "small"), w=(f"h0T{c}",))
            if blk == 0 and c4 == 0:
                chk("ln_c0", h0T[:, 3, :], 2048)
            if blk == 0 and c4 == 1:
                chk("ln_c1", h0T[:, 3, :], 2048)
            if blk == 0 and c4 == 3:
                chk("ln_c3", h0T[:, 3, :], 2048)

    chk("ln", h0T[:, 3, :], 2048)
    wcnt = [0, 0]
    ws_flat = [A.f32(O_Z + 49280 + 8192 * i, 2048) for i in range(2)]
    cast_rr = [0]

    def load_w(src3, ncols, rows_c, scale_ap=None, dst=None, scale_off=0, cast_engs=("dve", "act")):
        si = wcnt[0] % 2
        wcnt[0] += 1
        s_v = ws_flat[si][:, 0:rows_c * ncols].rearrange("p (c n) -> p c n", c=rows_c)
        P.dma(f"ws{si}", s_v, src3, r=(), w=(f"ws{si}",))
        if dst is None:
            bi = wcnt[1] % 4
            wcnt[1] += 1
            d_v = wb[bi][:, 0:rows_c, 0:ncols]
            key = (f"wb{bi}",)
        else:
            d_v, key = dst
        if scale_ap is None:
            eng = cast_engs[cast_rr[0] % len(cast_engs)]
            cast_rr[0] += 1
            P.cp(eng, d_v, s_v, r=(f"ws{si}",), w=key)
        else:
            for c in range(rows_c):
                sc = scale_ap[:, scale_off + c:scale_off + c + 1]
                if c % 2 == 0:
                    P.ts("dve", d_v[:, c, :], s_v[:, c, :], sc, None, ALU.mult, None, r=(f"ws{si}", "small", "onorm"), w=key)
                else:
                    P.act(d_v[:, c, :], s_v[:, c, :], AF.Identity, r=(f"ws{si}", "small", "onorm"), w=key, scale=sc)
        return d_v, key

    def win_cols(c0, n):
        return w_in[:, c0:c0 + n].rearrange("(c p) n -> p c n", p=128)

    sqb = [sp_t, w_t]

    def latent_group(wts, ncol, rhs_of_c, rhs_keys, dst_of_f, dst_key, nfeat):
        for f, (wt, wk) in enumerate(wts):
            for c in range(16):
                P.mm(ps[f], wt[:, c, :], rhs_of_c(c), c == 0, c == 15, r=wk + (f"h0T{c}",), w=(f"ps{f}",))
        for f in range(len(wts)):
            sq = sqb[f % 2]
            P.act(sq, ps[f], AF.Square, r=(f"ps{f}",), w=(f"sq{f % 2}",))
            P.mm(ps[4], ones_b, sq, f == 0, f == len(wts) - 1, r=("ones_b", f"sq{f % 2}"), w=("ps4",))
        P.act(e_t, ps[4], AF.Sqrt, r=("ps4", "eps"), w=("e_t",), bias=eps_t[:, 1:2], scale=1.0 / nfeat)
        P.add("dve", lambda e: e.reciprocal(t1_t, e_t), r=("e_t",), w=("t1_t",))
        for f in range(len(wts)):
            P.tt("dve", dst_of_f(f), ps[f], t1_t, ALU.mult, r=(f"ps{f}", "t1_t"), w=(dst_key,))

    qw = [load_w(win_cols(3072 + 128 * f, 128), 128, 16) for f in range(4)]
    for g in range(2):
        latent_group(qw, 128, lambda c, g=g: own_tok(h0T, c, g), None,
                     lambda f, g=g: qlatnT[:, f, 512 * g:512 * g + 512], "qlatnT", 512.0)
    kvw = [load_w(win_cols(3584 + 128 * f, 128), 128, 16) for f in range(2)]
    for g in range(4):
        latent_group(kvw, 128, lambda c, g=g: h0T[:, c, 512 * g:512 * g + 512], None,
                     lambda f, g=g: kvlatnT[:, f, 512 * g:512 * g + 512], "kvlatnT", 256.0)

    def make_rot(dst, src, nck, key_d, key_s):
        P.ts("pool", dst[:, 0:nck, 0:32], src[:, 0:nck, 32:64], -1.0, None, ALU.mult, None, r=key_s, w=key_d)
        P.cp("pool", dst[:, 0:nck, 32:64], src[:, 0:nck, 0:32], r=key_s, w=key_d)

    def rope_evac(dst, pa, pb, cs, sn, keys_r, key_w):
        P.tt("dve", t1_t[0:64, :], pa, cs, ALU.mult, r=keys_r + ("cos2",), w=("t1_t",))
        P.tt("dve", e_t[0:64, :], pb, sn, ALU.mult, r=keys_r + ("sin2",), w=("e_t",))
        P.tt("pool", dst, t1_t[0:64, :], e_t[0:64, :], ALU.add, r=("t1_t", "e_t"), w=(key_w,))

    krw, krk = load_w(win_cols(3840, 64), 64, 16)
    krot, krotk = wb[wcnt[1] % 4][:, :, 0:64], (f"wb{wcnt[1] % 4}",)
    wcnt[1] += 1
    make_rot(krot, krw, 16, krotk, krk)
    for g in range(4):
        for c in range(16):
            P.mm(ps[0][0:64, :], krw[:, c, :], h0T[:, c, 512 * g:512 * g + 512], c == 0, c == 15,
                 r=krk + (f"h0T{c}",), w=("ps0",))
        for c in range(16):
            P.mm(ps[1][0:64, :], krot[:, c, :], h0T[:, c, 512 * g:512 * g + 512], c == 0, c == 15,
                 r=krotk + (f"h0T{c}",), w=("ps1",))
        rope_evac(kropeT[:, 512 * g:512 * g + 512], ps[0][0:64, :], ps[1][0:64, :],
                  cos2[:, 512 * g:512 * g + 512], sin2[:, 512 * g:512 * g + 512], ("ps0", "ps1"), "kropeT")

    chk("lat", kvlatnT[:, 1, :], 2048)
    chk("krope", kropeT, 2048, 64)
    chk("qlat", qlatnT[:, 2, :], 1024)
    ss_sb = stat[:, 32:40]
    ss_mla = stat[:, 40:48]
    P.ms("dve", stat[:, 32:48], 0.0, w=("ss",))
    P.ms("dve", vt[:, :, 128:132], 1.0, w=("vt",))

    TB = [dict(e=e_t, sp=sp_t, t1=t1_t, w=w_t, carry=carry, osb=osb, rinv=rinv, sfx=""),
          dict(e=A.f32(O_Z + 82048, 512), sp=A.bf(O_Z + 84096, 512), t1=A.f32(O_Z + 85120, 512), w=A.bf(O_Z + 87168, 512),
               carry=[A.f32(O_Z + 88192 + 512 * i, 128) for i in range(2)], osb=A.bf(O_Z + 89216, 128),
               rinv=A.f32(O_Z + 89472, 1), sfx="B")]

    def attn_slot(kind, h, j, sid=0):
        T = TB[sid]
        X = T["sfx"]
        e_s, sp_s, t1_s, w_s, carry_s, osb_s, rinv_s = T["e"], T["sp"], T["t1"], T["w"], T["carry"], T["osb"], T["rinv"]
        ke, ksp, kt1, kw, kosb, krinv = "e_t" + X, "sp_t" + X, "t1_t" + X, "w_t" + X, "osb" + X, "rinv" + X
        zb, btb, ob = sid, 2 + sid, 4 + sid
        z, bt = ps[zb], ps[btb]
        nblk = 2 * j + 2
        groups = [list(range(b0, min(b0 + 4, nblk))) for b0 in range(0, nblk, 4)]
        groups = groups[::-1]
        oc = 128 if kind == "sb" else 129
        scale = 128.0 ** -0.5 if kind == "sb" else 192.0 ** -0.5
        mask_b = msb_b if kind == "sb" else mmla_b
        o_ps = ps[ob][:, 0:oc]
        npv = 0
        tot_pv = nblk
        cur = 0
        for gi, blks in enumerate(groups):
            nb = len(blks)
            cols = nb * 128
            last_group = gi == len(groups) - 1
            for i, b in enumerate(blks):
                zo = z[:, 128 * i:128 * i + 128]
                if kind == "sb":
                    P.mm(zo, kT[:, 128 * b:128 * b + 128], qT[:, 128 * j:128 * j + 128], True, True,
                         r=("kT", "qT"), w=(f"ps{zb}",))
                else:
                    P.mm(zo, kT[:, 128 * b:128 * b + 128], qT[:, 128 * j:128 * j + 128], True, False,
                         r=("kT", "qT"), w=(f"ps{zb}",))
                    P.mm(zo, kropeT[:, 128 * b:128 * b + 128], qpeT[:, 128 * j:128 * j + 128], False, True,
                         r=("kropeT", "qpeT"), w=(f"ps{zb}",))
            yield

            def apply_masks(t, key):
                if gi == 0:
                    i0, i1 = nb - 2, nb - 1
                    P.tt("dve", t[:, 128 * i0:128 * i0 + 128], t[:, 128 * i0:128 * i0 + 128], mask_b, ALU.mult,
                         r=(key, "msb_b", "mmla_b"), w=(key,))
                    P.ts("dve", t[:, 128 * i1:128 * i1 + 128], t[:, 128 * i1:128 * i1 + 128], vis[:, j:j + 1], None,
                         ALU.mult, None, r=(key, "small"), w=(key,))

            if kind == "sb":
                P.act(e_s[:, 0:cols], z[:, 0:cols], AF.Exp, r=(f"ps{zb}",), w=(ke,), scale=scale)
                P.act(sp_s[:, 0:cols], e_s[:, 0:cols], AF.Ln, r=(ke, "ones_f"), w=(ksp,), bias=ones_f[:, 0:1], scale=1.0)
                apply_masks(sp_s, ksp)
                yield
                P.stt("dve", t1_s[:, 0:cols], z[:, 0:cols], scale, sp_s[:, 0:cols], ALU.mult, ALU.subtract,
                      r=(f"ps{zb}", ksp), w=(kt1,))
                for i, b in enumerate(blks):
                    terms = [(triu_b, i, "triu_b")]
                    pair = b // 2
                    if b % 2 == 0:
                        terms.append((pairf_b[:, (pair * 2 + 0) * 128:(pair * 2 + 1) * 128], i + 1, "pairf_b"))
                    else:
                        terms.append((pairf_b[:, (pair * 2 + 1) * 128:(pair * 2 + 2) * 128], i - 1, "pairf_b"))
                    for i2, b2 in enumerate(blks):
                        if b2 // 2 > pair:
                            terms.append((ones_b, i2, "ones_b"))
                    for n, (m, src, mk) in enumerate(terms):
                        P.mm(bt[:, 128 * i:128 * i + 128], m, sp_s[:, 128 * src:128 * src + 128], n == 0,
                             n == len(terms) - 1, r=(mk, ksp), w=(f"ps{btb}",))
                if not last_group:
                    for i in range(nb):
                        P.mm(ps[6][:, 0:128], ones_b, sp_s[:, 128 * i:128 * i + 128], i == 0, i == nb - 1,
                             r=("ones_b", ksp), w=("ps6",))
                oldc = cur
                if not last_group:
                    if gi == 0:
                        P.cp("dve", carry_s[0], ps[6][:, 0:128], r=("ps6",), w=(f"carry{X}0",))
                        cur = 0
                    else:
                        P.tt("dve", carry_s[1 - cur], carry_s[cur], ps[6][:, 0:128], ALU.add, r=(f"carry{X}{cur}", "ps6"),
                             w=(f"carry{X}{1 - cur}",))
                        cur = 1 - cur
                yield
                P.tt("dve", t1_s[:, 0:cols], t1_s[:, 0:cols], bt[:, 0:cols], ALU.subtract, r=(kt1, f"ps{btb}"), w=(kt1,))
                if gi > 0:
                    t3 = t1_s[:, 0:cols].rearrange("p (b t) -> p b t", b=nb)
                    P.tt("dve", t3, t3, carry_s[oldc].unsqueeze(1).to_broadcast([128, nb, 128]), ALU.subtract,
                         r=(kt1, f"carry{X}{oldc}"), w=(kt1,))
                yield
                P.act(w_s[:, 0:cols], t1_s[:, 0:cols], AF.Exp, r=(kt1,), w=(kw,))
            else:
                P.act(w_s[:, 0:cols], z[:, 0:cols], AF.Exp, r=(f"ps{zb}",), w=(kw,), scale=scale)
            apply_masks(w_s, kw)
            yield
            for i, b in enumerate(blks):
                P.mm(o_ps, w_s[:, 128 * i:128 * i + 128], vt[:, b, 0:oc], npv == 0, npv == tot_pv - 1,
                     r=(kw, "vt"), w=(f"ps{ob}",))
                npv += 1
            yield
        hc = h if kind == "sb" else 8 + h
        ssd = ss_sb if kind == "sb" else ss_mla
        sa = stat[:, 64 + sid:65 + sid]
        if kind == "sb":
            P.act(e_s[:, 0:128], o_ps[:, 0:128], AF.Square, r=(f"ps{ob}",), w=(ke, "ssacc" + X), accum_out=sa)
            P.cp("dve", osb_s, o_ps[:, 0:128], r=(f"ps{ob}",), w=(kosb,))
        else:
            P.add("dve", lambda e: e.reciprocal(rinv_s, o_ps[:, 128:129]), r=(f"ps{ob}",), w=(krinv,))
            P.act(e_s[:, 0:128], o_ps[:, 0:128], AF.Square, r=(f"ps{ob}", krinv), w=(ke, "ssacc" + X), scale=rinv_s,
                  accum_out=sa)
            P.ts("dve", osb_s, o_ps[:, 0:128], rinv_s, None, ALU.mult, None, r=(f"ps{ob}", krinv), w=(kosb,))
        P.tt("dve", ssd[:, j:j + 1], ssd[:, j:j + 1], sa, ALU.add, r=("ssacc" + X, "ss"), w=("ss",))
        yield
        P.tr(psb[7][:, 0:128], osb_s, ident_b, r=(kosb, "ident_b"), w=("pst0",))
        P.cp("act", mixT[:, hc, 128 * j:128 * j + 128], psb[7][:, 0:128], r=("pst0",), w=(f"mixT{hc}",))
        yield

    def run_head_attention(kind, h):
        for ja, jb in ((0, 7), (1, 6), (2, 5), (3, 4)):
            gens = [attn_slot(kind, h, ja, 0), attn_slot(kind, h, jb, 1)]
            live = [True, True]
            while any(live):
                for k in range(2):
                    if live[k]:
                        try:
                            next(gens[k])
                        except StopIteration:
                            live[k] = False

    def proj_T(dst, wt, wkey, nck, rhs_of, rkeys, ngrp, dkey, m=128):
        for g in range(ngrp):
            pb = 6 + g % 2
            pkeys = ("ps6",) if pb == 6 else ("pst0", "pst1")
            for c in range(nck):
                P.mm(ps[pb][0:m, :], wt[:, c, :], rhs_of(c, g), c == 0, c == nck - 1, r=wkey + rkeys(c), w=pkeys)
            P.cp("act", dst[:, 512 * g:512 * g + 512], ps[pb][0:m, :], r=pkeys, w=(dkey,))

    def proj_v(wt, wkey, nck, lhs_of, lkeys):
        for q4 in range(4):
            pb = 6 + q4 % 2
            pkeys = ("ps6",) if pb == 6 else ("pst0", "pst1")
            for i in range(4):
                blk = 4 * q4 + i
                for c in range(nck):
                    P.mm(ps[pb][:, 128 * i:128 * i + 128], lhs_of(c, blk), wt[:, c, :], c == 0, c == nck - 1,
                         r=wkey + lkeys(c), w=pkeys)
            P.cp("act", vt[:, 4 * q4:4 * q4 + 4, 0:128], ps[pb].rearrange("p (b d) -> p b d", b=4), r=pkeys, w=("vt",))

    n_heads = H if stage != "fast" else 1
    def load_qkv(h):
        return (load_w(win_cols(h * 128, 128), 128, 16), load_w(win_cols(1024 + h * 128, 128), 128, 16),
                load_w(win_cols(2048 + h * 128, 128), 128, 16))

    nxt = load_qkv(0)
    for h in range(n_heads):
        (wq, wqk), (wk_, wkk), (wv, wvk) = nxt
        proj_T(qT, wq, wqk, 16, lambda c, g: own_tok(h0T, c, g), lambda c: (f"h0T{c}",), 2, "qT")
        proj_T(kT, wk_, wkk, 16, lambda c, g: h0T[:, c, 512 * g:512 * g + 512], lambda c: (f"h0T{c}",), 4, "kT")
        proj_v(wv, wvk, 16, lambda c, blk: h0T[:, c, 128 * blk:128 * blk + 128], lambda c: (f"h0T{c}",))
        if h == 0:
            chk("proj_k", kT, 2048)
            chk("proj_q", qT, 1024)
            chk("proj_v", vt[:, :, 0:128], 2048)
        if h + 1 < n_heads:
            nxt = load_qkv(h + 1)
        run_head_attention("sb", h)
        if h == 0:
            chk("sb0", mixT[:, 0, :], 1024)

    wqb = A.bf(O_H0T, 4 * 1536).rearrange("p (c n) -> p c n", c=4)
    wqb_k = ("h0T0", "h0T1", "h0T2")
    wkvb = A.bf(O_H0T + 12288, 2 * 2048).rearrange("p (c n) -> p c n", c=2)
    wkvb_k = ("h0T3", "h0T4")
    wqrot = A.bf(O_H0T + 20480, 4 * 512).rearrange("p (c n) -> p c n", c=4)
    wqrot_k = ("h0T5",)
    if stage != "fast":
        pass
    for q3 in range(3):
        load_w(w_q_b[:, 512 * q3:512 * q3 + 512].rearrange("(c p) n -> p c n", p=128), 512, 4, scale_ap=qan,
               dst=(wqb[:, :, 512 * q3:512 * q3 + 512], wqb_k))
    for q2 in range(2):
        load_w(w_kv_b[:, 1024 * q2:1024 * q2 + 1024].rearrange("(c p) n -> p c n", p=128), 1024, 2, scale_ap=kvan,
               dst=(wkvb[:, :, 1024 * q2:1024 * q2 + 1024], wkvb_k))
    for h in range(H):
        make_rot(wqrot[:, :, 64 * h:64 * h + 64], wqb[:, :, 192 * h + 128:192 * h + 192], 4, wqrot_k, wqb_k)

    def own4(ap2, g):
        return ap2.rearrange("p (b two t) -> p b two t", two=2, t=128)[:, 4 * g:4 * g + 4, 0, :]

    v3 = lambda ap: ap.rearrange("p (b t) -> p b t", b=4)

    for h in range(n_heads):
        proj_T(kT, wkvb[:, :, 256 * h:256 * h + 128], wkvb_k, 2, lambda c, g: kvlatnT[:, c, 512 * g:512 * g + 512],
               lambda c: ("kvlatnT",), 4, "kT")
        proj_v(wkvb[:, :, 256 * h + 128:256 * h + 256], wkvb_k, 2, lambda c, blk: kvlatnT[:, c, 128 * blk:128 * blk + 128],
               lambda c: ("kvlatnT",))
        proj_T(qT, wqb[:, :, 192 * h:192 * h + 128], wqb_k, 4, lambda c, g: qlatnT[:, c, 512 * g:512 * g + 512],
               lambda c: ("qlatnT",), 2, "qT")
        for g in range(2):
            for c in range(4):
                P.mm(ps[0][0:64, :], wqb[:, c, 192 * h + 128:192 * h + 192], qlatnT[:, c, 512 * g:512 * g + 512],
                     c == 0, c == 3, r=wqb_k + ("qlatnT",), w=("ps0",))
            for c in range(4):
                P.mm(ps[1][0:64, :], wqrot[:, c, 64 * h:64 * h + 64], qlatnT[:, c, 512 * g:512 * g + 512],
                     c == 0, c == 3, r=wqrot_k + ("qlatnT",), w=("ps1",))
            P.tt("dve", v3(t1_t[0:64, :]), v3(ps[0][0:64, :]), own4(cos2, g), ALU.mult, r=("ps0", "cos2"), w=("t1_t",))
            P.tt("dve", v3(e_t[0:64, :]), v3(ps[1][0:64, :]), own4(sin2, g), ALU.mult, r=("ps1", "sin2"), w=("e_t",))
            P.tt("pool", qpeT[:, 512 * g:512 * g + 512], t1_t[0:64, :], e_t[0:64, :], ALU.add, r=("t1_t", "e_t"), w=("qpeT",))
        if h == 0:
            chk("mproj_k", kT, 2048)
            chk("mproj_q", qT, 1024)
            chk("mproj_qpe", qpeT, 1024, 64)
            chk("mproj_v", vt[:, :, 0:128], 2048)
        run_head_attention("mla", h)
        if h == 0:
            chk("mla0", mixT[:, 8, :], 1024)

    P.fence()

    acc = A.f32(O_H0T, NS * D).rearrange("p (j d) -> p j d", j=NS)
    acck = lambda j: (f"acc{j}",)
    h1bf = A.bf(O_MIX, NS * D).rearrange("p (j d) -> p j d", j=NS)
    gbc = A.f32(O_Z, D)
    bbc = A.f32(O_Z + 8192, D)
    xt2 = A.f32(O_Z + 16384, D)
    wso = [A.f32(O_Z + 24576 + 16384 * i, 8 * 512).rearrange("p (c n) -> p c n", c=8) for i in range(2)]
    wbo = [A.bf(O_Z + 57344 + 8192 * i, 8 * 512).rearrange("p (c n) -> p c n", c=8) for i in range(2)]
    h1T = A.f32(O_Z + 75776, 16 * 128).rearrange("p (c t) -> p c t", c=16)
    wr = A.f32(O_Z + 83968, 16 * E).rearrange("p (c n) -> p c n", c=16)
    rt = A.f32(O_Z + 86016, 8 * E)
    lg = rt[:, 0:32]
    m8 = rt[:, 32:40]
    negm = rt[:, 40:41]
    den = rt[:, 41:42]
    ex = rt[:, 64:96]
    brt = rt[:, 96:128]
    Gt = A.f32(O_Z + 90112, NS * E).rearrange("p (j e) -> p j e", j=NS)
    rankm = A.f32(O_Z + 91136, NS * E).rearrange("p (j e) -> p j e", j=NS)
    routed = A.f32(O_Z + 92160, NS * E).rearrange("p (j e) -> p j e", j=NS)

    P.dma("gbc", gbc, ln_in_g.partition_broadcast(128), r=(), w=("gbc",))
    P.dma("bbc", bbc, ln_in_b.partition_broadcast(128), r=(), w=("bbc",))
    P.add("act", lambda e: e.mul(gbc, gbc, ALPHA), r=("gbc",), w=("gbc",))
    P.add("act", lambda e: e.mul(bbc, bbc, ALPHA), r=("bbc",), w=("bbc",))
    for j in range(NS):
        P.dma("xt2", xt2, xp[2 * j], r=(), w=("xt2",))
        ln_stats(xt2, "xt2")
        P.act(xt2, xt2, AF.Identity, r=("xt2", "rstd", "nmr"), w=("xt2",), bias=stat[:, 28:29], scale=stat[:, 27:28])
        P.tt("dve", acc[:, j, :], xt2, gbc, ALU.mult, r=("xt2", "gbc"), w=acck(j))
        P.tt("dve", acc[:, j, :], acc[:, j, :], bbc, ALU.add, r=acck(j) + ("bbc",), w=acck(j))

    P.act(stat[:, 48:64], stat[:, 32:48], AF.Sqrt, r=("ss", "eps"), w=("rstdo",), bias=eps_t[:, 1:2], scale=1.0 / 1024.0)
    P.add("dve", lambda e: e.reciprocal(stat[:, 48:64], stat[:, 48:64]), r=("rstdo",), w=("rstdo",))
    def load_wo(idx):
        dblk, half = idx // 2, idx % 2
        si = idx % 2
        src = w_o[1024 * half:1024 * half + 1024, 512 * dblk:512 * dblk + 512].rearrange("(c p) n -> p c n", p=128)
        P.dma(f"wso{si}", wso[si], src, r=(), w=(f"wso{si}",))
        for c in range(8):
            sc = onorm[:, 8 * half + c:8 * half + c + 1]
            if c % 2 == 0:
                P.ts("dve", wbo[si][:, c, :], wso[si][:, c, :], sc, None, ALU.mult, None, r=(f"wso{si}", "onorm"), w=(f"wbo{si}",))
            else:
                P.act(wbo[si][:, c, :], wso[si][:, c, :], AF.Identity, r=(f"wso{si}", "onorm"), w=(f"wbo{si}",), scale=sc)

    load_wo(0)
    for idx in range(8):
        dblk, half = idx // 2, idx % 2
        si = idx % 2
        if idx + 1 < 8:
            load_wo(idx + 1)
        for j in range(NS):
            pb = 6 + j % 2
            pkeys = ("ps6",) if pb == 6 else ("pst0", "pst1")
            for c in range(8):
                P.mm(ps[pb], mixT[:, 8 * half + c, 128 * j:128 * j + 128], wbo[si][:, c, :], c == 0, c == 7,
                     r=(f"mixT{8 * half + c}", f"wbo{si}"), w=pkeys)
            dst = acc[:, j, 512 * dblk:512 * dblk + 512]
            P.stt("dve", dst, ps[pb], stat[:, 48 + 8 * half + j:48 + 8 * half + j + 1], dst, ALU.mult, ALU.add,
                  r=pkeys + acck(j) + ("rstdo",), w=acck(j))

    P.fence()
    P.dma("gbc", gbc, ln_mix_g.partition_broadcast(128), r=(), w=("gbc",))
    P.dma("bbc", bbc, ln_mix_b.partition_broadcast(128), r=(), w=("bbc",))
    P.dma("wr", wr, w_router.rearrange("(c p) n -> p c n", p=128), r=(), w=("wr",))
    P.dma("brt", brt, b_router.partition_broadcast(128), r=(), w=("brt",))
    for j in range(NS):
        ln_stats(acc[:, j, :], f"acc{j}")
        P.act(xt2, acc[:, j, :], AF.Identity, r=acck(j) + ("rstd", "nmr"), w=("xt2",), bias=stat[:, 28:29], scale=stat[:, 27:28])
        P.tt("dve", xt2, xt2, gbc, ALU.mult, r=("xt2", "gbc"), w=("xt2",))
        P.tt("dve", acc[:, j, :], xt2, bbc, ALU.add, r=("xt2", "bbc"), w=acck(j))
        P.cp("act", h1bf[:, j, :], acc[:, j, :], r=acck(j), w=(f"h1bf{j}",))
        for c4 in range(4):
            pb = 6 + c4 % 2
            pkeys = ("ps6",) if pb == 6 else ("pst0", "pst1")
            for i in range(4):
                c = 4 * c4 + i
                P.tr(ps[pb][:, 128 * i:128 * i + 128], acc[:, j, 128 * c:128 * c + 128], ident_f, r=acck(j) + ("ident_f",), w=pkeys)
            P.cp("act", h1T[:, 4 * c4:4 * c4 + 4, :], ps[pb].rearrange("p (i t) -> p i t", i=4), r=pkeys, w=("h1T",))
        for c in range(16):
            P.mm(ps[4][:, 0:E], h1T[:, c, :], wr[:, c, :], c == 0, c == 15, r=("h1T", "wr"), w=("ps4",))
        P.tt("dve", lg, ps[4][:, 0:E], brt, ALU.add, r=("ps4", "brt"), w=("lg",))
        P.add("dve", lambda e: e.max(m8, lg), r=("lg",), w=("m8",))
        P.ts("dve", routed[:, j, :], lg, m8[:, 3:4], None, ALU.is_ge, None, r=("lg", "m8"), w=("routed",))
        P.ts("dve", negm, m8[:, 0:1], -1.0, None, ALU.mult, None, r=("m8",), w=("negm",))
        P.act(ex, lg, AF.Exp, r=("lg", "negm"), w=("ex",), bias=negm, scale=1.0)
        P.tt("dve", ex, ex, routed[:, j, :], ALU.mult, r=("ex", "routed"), w=("ex",))
        P.add("dve", lambda e: e.reduce_sum(den, ex, AX.X), r=("ex",), w=("den",))
        P.add("dve", lambda e: e.reciprocal(den, den), r=("den",), w=("den",))
        P.ts("dve", Gt[:, j, :], ex, den, None, ALU.mult, None, r=("ex", "den"), w=("Gt",))
        for j2 in range(j + 1):
            P.mm(ps[5][:, 0:E], (tril_f if j2 == j else ones_f), routed[:, j2, :], j2 == 0, j2 == j,
                 r=("tril_f", "ones_f", "routed"), w=("ps5",))
        P.stt("dve", rankm[:, j, :], ps[5][:, 0:E], 1.0, routed[:, j, :], ALU.add, ALU.mult, r=("ps5", "routed"), w=("rankm",))
        P.ts("dve", rankm[:, j, :], rankm[:, j, :], -1.0, None, ALU.add, None, r=("rankm",), w=("rankm",))
        P.add("act", (lambda j: lambda e: e.mul(acc[:, j, :], acc[:, j, :], ALPHA))(j), r=acck(j), w=acck(j))

    if stage == "A":
        P.fence()
        for j in range(NS):
            P.dma(f"out{j % 2}", out[j], acc[:, j, :], r=acck(j), w=())
        P.emit(stack)
        return

    P.fence()

    wgu_t = w_gu
    wdn_t = w_dn
    NSTG = 4
    wsM = [A.f32(O_Z + 8192 * i, 2048).rearrange("p (c n) -> p c n", c=16) for i in range(NSTG)]
    wbM = [A.bf(O_Z + 32768 + 4096 * i, 2048).rearrange("p (c n) -> p c n", c=16) for i in range(3)]
    Sel = A.bf(O_Z + 45056, NS * CAP).rearrange("p (j r) -> p j r", j=NS)
    SelG = A.bf(O_Z + 49152, NS * CAP).rearrange("p (j r) -> p j r", j=NS)
    SelGT = A.bf(O_Z + 53248, 2 * 1024).rearrange("p (c t) -> p c t", c=2)
    xT = A.bf(O_Z + 57344, 16 * CAP).rearrange("p (c r) -> p c r", c=16)
    gs = A.bf(O_Z + 65536, 2 * DFF).rearrange("p (c d) -> p c d", c=2)
    yb = A.bf(O_Z + 65536, 2 * D).rearrange("p (c d) -> p c d", c=2)
    actT = A.bf(O_Z + 73728, 16 * CAP).rearrange("p (c r) -> p c r", c=16)
    actm = A.bf(O_Z + 94208, 2 * DFF).rearrange("p (c d) -> p c d", c=2)
    gt = A.f32(O_Z + 81920, 512)
    sg = A.bf(O_Z + 83968, 512)
    ut = A.f32(O_Z + 84992, 512)
    brow = A.f32(O_Z + 87040, 512, p=1)
    brow_b = A.bf(O_Z + 89088, 512, p=1)

    mcnt = [0, 0, 0]
    cast_pat = ("dve", "dve", "act")
    tile_src = []
    for e_i in range(n_exp):
        tile_src += [wgu_t[e_i, t] for t in range(32)] + [wdn_t[e_i, t] for t in range(16)]
    issued = []
    PF = 2

    def _issue(k):
        si = mcnt[0] % NSTG
        bi = mcnt[1] % 3
        mcnt[0] += 1
        mcnt[1] += 1
        P.dma(f"wsM{si}", wsM[si], tile_src[k].rearrange("p (c n) -> p c n", c=16), r=(), w=(f"wsM{si}",))
        eng = cast_pat[mcnt[2] % len(cast_pat)]
        mcnt[2] += 1
        P.cp(eng, wbM[bi], wsM[si], r=(f"wsM{si}",), w=(f"wbM{bi}",))
        issued.append((wbM[bi], f"wbM{bi}"))

    def get_tile(k):
        while len(issued) <= min(k + PF, len(tile_src) - 1):
            _issue(len(issued))
        return issued[k]

    def build_sel(e_n):
        for j in range(NS):
            P.ts("dve", Sel[:, j, :], iota, rankm[:, j, e_n:e_n + 1], None, ALU.is_equal, None, r=("iota", "rankm"), w=("Sel",))

    def build_selg(e_n):
        for j in range(NS):
            P.ts("pool", SelG[:, j, :], iota, rankm[:, j, e_n:e_n + 1], Gt[:, j, e_n:e_n + 1], ALU.is_equal, ALU.mult,
                 r=("iota", "rankm", "Gt"), w=("SelG",))

    build_sel(0)
    build_selg(0)
    for e_i in range(n_exp):
        for d2 in range(8):
            pb = d2 % 2
            for i in range(2):
                dc = 2 * d2 + i
                for j in range(NS):
                    P.mm(ps[pb][:, CAP * i:CAP * i + CAP], h1bf[:, j, 128 * dc:128 * dc + 128], Sel[:, j, :], j == 0, j == NS - 1,
                         r=(f"h1bf{j}", "Sel"), w=(f"ps{pb}",))
            P.cp("act", xT[:, 2 * d2:2 * d2 + 2, :], ps[pb].rearrange("p (i r) -> p i r", i=2), r=(f"ps{pb}",), w=("xT",))
        for fg in range(8):
            banks = (2, 3) if fg % 2 == 0 else (0, 1)
            P.dma("brow", brow, b_gu[e_i:e_i + 1, 512 * fg:512 * fg + 512], r=(), w=("brow",))
            P.cp("act", brow_b, brow, r=("brow",), w=("brow_b",))
            for kq in range(4):
                wt, wk = get_tile(48 * e_i + 4 * fg + kq)
                wt4 = wt.rearrange("p c n -> p (c n)").rearrange("p (c n) -> p c n", c=4)
                for c4 in range(4):
                    c = 4 * kq + c4
                    for rc in range(2):
                        P.mm(ps[banks[rc]], xT[:, c, 128 * rc:128 * rc + 128], wt4[:, c4, :], c == 0, False,
                             r=(wk, "xT"), w=(f"ps{banks[rc]}",))
            for rc in range(2):
                P.mm(ps[banks[rc]], ones_b[0:1, :], brow_b, False, True, r=("ones_b", "brow_b"), w=(f"ps{banks[rc]}",))
            for rc in range(2):
                pk = f"ps{banks[rc]}"
                if fg < 4:
                    dstg = gs[:, rc, 512 * fg:512 * fg + 512]
                    P.ts("dve", gt, ps[banks[rc]], 7.0, None, ALU.min, None, r=(pk,), w=("gt",))
                    P.act(sg, gt, AF.Sigmoid, r=("gt",), w=("sg",), scale=1.702)
                    P.tt("pool", dstg, gt, sg, ALU.mult, r=("gt", "sg"), w=("gs",))
                else:
                    f0 = 512 * (fg - 4)
                    P.ts("dve", ut, ps[banks[rc]], -7.0, 7.0, ALU.max, ALU.min, r=(pk,), w=("ut",))
                    P.stt("dve", actm[:, rc, f0:f0 + 512], ut, 1.0, gs[:, rc, f0:f0 + 512], ALU.add, ALU.mult,
                          r=("ut", "gs"), w=("actm",))
        for rc in range(2):
            for c4 in range(4):
                half = c4 % 2
                pst = psb[7 - half][:, 0:512]
                for i in range(4):
                    c = 4 * c4 + i
                    P.tr(pst[:, 128 * i:128 * i + 128], actm[:, rc, 128 * c:128 * c + 128], ident_b, r=("actm", "ident_b"),
                         w=(bkey[half],))
                P.cp("act", actT[:, 4 * c4:4 * c4 + 4, 128 * rc:128 * rc + 128], pst.rearrange("p (i t) -> p i t", i=4),
                     r=(bkey[half],), w=("actT",))
        if e_i + 1 < n_exp:
            build_sel(e_i + 1)
        for q in range(4):
            for kq in range(4):
                wt, wk = get_tile(48 * e_i + 32 + 4 * q + kq)
                wt4 = wt.rearrange("p c n -> p (c n)").rearrange("p (c n) -> p c n", c=4)
                for c4 in range(4):
                    c = 4 * kq + c4
                    for rc in range(2):
                        P.mm(ps[4 + rc], actT[:, c, 128 * rc:128 * rc + 128], wt4[:, c4, :], c == 0, c == 15,
                             r=(wk, "actT"), w=(f"ps{4 + rc}",))
            for rc in range(2):
                P.cp("act", yb[:, rc, 512 * q:512 * q + 512], ps[4 + rc], r=(f"ps{4 + rc}",), w=("gs",))
        for rc in range(2):
            for j4 in range(2):
                half = (2 * rc + j4) % 2
                pst = psb[7 - half][:, 0:512]
                for i in range(4):
                    j = 4 * j4 + i
                    P.tr(pst[:, 128 * i:128 * i + 128], SelG[:, j, 128 * rc:128 * rc + 128], ident_b, r=("SelG", "ident_b"),
                         w=(bkey[half],))
                P.cp("act", SelGT[:, rc, 512 * j4:512 * j4 + 512], pst, r=(bkey[half],), w=("SelGT",))
        if e_i + 1 < n_exp:
            build_selg(e_i + 1)
        for j in range(NS):
            for dq in range(4):
                pb = dq
                for rc in range(2):
                    P.mm(ps[pb], SelGT[:, rc, 128 * j:128 * j + 128], yb[:, rc, 512 * dq:512 * dq + 512], rc == 0, rc == 1,
                         r=("SelGT", "gs"), w=(f"ps{pb}",))
                dst = acc[:, j, 512 * dq:512 * dq + 512]
                P.tt("dve", dst, dst, ps[pb], ALU.add, r=acck(j) + (f"ps{pb}",), w=acck(j))

    P.fence()

    bdt = A.f32(O_Z, D)
    gT = A.f32(O_Z + 8192, 128)
    P.dma("bdt", bdt[0:n_exp, :], b_dn, r=(), w=("bdt",))
    for j in range(NS):
        P.tr(ps[6][0:n_exp, 0:128], Gt[:, j, 0:n_exp], ident_f, r=("Gt", "ident_f"), w=("ps6",))
        P.cp("act", gT[0:n_exp, :], ps[6][0:n_exp, 0:128], r=("ps6",), w=("gT",))
        for dq in range(4):
            pb = dq % 2
            P.mm(ps[pb], gT[0:n_exp, :], bdt[0:n_exp, 512 * dq:512 * dq + 512], True, True, r=("gT", "bdt"), w=(f"ps{pb}",))
            dst = acc[:, j, 512 * dq:512 * dq + 512]
            P.tt("dve", dst, dst, ps[pb], ALU.add, r=acck(j) + (f"ps{pb}",), w=acck(j))

    P.fence()

    P.dma("gbc", gbc, ln_ffn_g.partition_broadcast(128), r=(), w=("gbc",))
    P.dma("bbc", bbc, ln_ffn_b.partition_broadcast(128), r=(), w=("bbc",))
    fb2 = [A.f32(O_Z + 16384 + 8192 * i, D) for i in range(2)]
    for j in range(NS):
        fo = fb2[j % 2]
        ln_stats(acc[:, j, :], f"acc{j}")
        P.act(fo, acc[:, j, :], AF.Identity, r=acck(j) + ("rstd", "nmr"), w=(f"fo{j % 2}",), bias=stat[:, 28:29], scale=stat[:, 27:28])
        P.tt("dve", fo, fo, gbc, ALU.mult, r=(f"fo{j % 2}", "gbc"), w=(f"fo{j % 2}",))
        P.tt("dve", fo, fo, bbc, ALU.add, r=(f"fo{j % 2}", "bbc"), w=(f"fo{j % 2}",))
        P.dma(f"out{j % 2}", out[j], fo, r=(f"fo{j % 2}",), w=())
    P.emit(stack)


def _host_consts(g):
    own = QA if g == 0 else QB
    oth = QB if g == 0 else QA
    k = np.arange(128)
    ident = np.eye(128, dtype=np.float32)
    triu = (k[:, None] > k[None, :]).astype(np.float32)
    tril = (k[:, None] < k[None, :]).astype(np.float32)
    msb = (k[:, None] < k[None, :]).astype(np.float32)
    mmla = ((k[:, None] // 64) <= (k[None, :] // 64)).astype(np.float32)
    pairf = np.zeros((128, NS, 2, 128), np.float32)
    small = np.zeros((128, 64), np.float32)
    for i in range(NS):
        f = 1.0 if oth[i] > own[i] else 0.0
        pairf[:, i, 0, :] = f
        pairf[:, i, 1, :] = 1.0 - f
        small[:, i] = 1.0 - f
    inv_freq = (10000.0 ** (-(np.arange(32, dtype=np.float32) * 2.0 / 64.0))).astype(np.float32)
    small[:64, 8] = np.concatenate([inv_freq, inv_freq])
    iota = np.broadcast_to(np.arange(CAP, dtype=np.float32)[None, :], (128, CAP)).copy()
    return dict(c_ident=ident, c_triu=triu, c_tril=tril, c_msb=msb, c_mmla=mmla,
                c_pairf=pairf.reshape(128, -1), c_iota=iota), small


def _pc(v, nchunk):
    return np.ascontiguousarray(np.asarray(v, np.float32).reshape(nchunk, 128).T)


def _prepare(inputs, n_exp=E):
    f = lambda k: np.asarray(inputs[k])
    x = f("x").astype(np.float32, copy=False)
    pos = f("positions").astype(np.int32, copy=False)
    wgu = f("w_gate_up")[0][:n_exp]
    wdn = f("w_down")[0][:n_exp]
    wgu_t = np.ascontiguousarray(wgu.reshape(n_exp, 4, 4, 128, 8, 512).transpose(0, 4, 1, 3, 2, 5)).reshape(n_exp, 32, 128, 2048)
    wdn_t = np.ascontiguousarray(wdn.reshape(n_exp, 4, 4, 128, 4, 512).transpose(0, 4, 1, 3, 2, 5)).reshape(n_exp, 16, 128, 2048)
    bgu = np.ascontiguousarray(f("b_gate_up")[0][:n_exp])
    shared = dict(
        w_in=np.ascontiguousarray(f("w_in")[0]), w_q_b=np.ascontiguousarray(f("w_q_b")[0]),
        w_kv_b=np.ascontiguousarray(f("w_kv_b")[0]), w_o=np.ascontiguousarray(f("w_o")[0]),
        ln_in_g=f("ln_in_g"), ln_in_b=f("ln_in_b"), ln_mix_g=f("ln_mix_g")[0], ln_mix_b=f("ln_mix_b")[0],
        ln_ffn_g=f("ln_ffn_g")[0], ln_ffn_b=f("ln_ffn_b")[0], w_router=np.ascontiguousarray(f("w_router")[0]),
        b_router=f("b_router")[0], w_gu=wgu_t, b_gu=bgu, w_dn=wdn_t, b_dn=np.ascontiguousarray(f("b_down")[0][:n_exp]),
        c_onorm=_pc(np.concatenate([f("sb_out_norm")[0], f("mla_out_norm")[0]]), 16),
    )
    shared = {k: np.ascontiguousarray(v, dtype=np.float32) for k, v in shared.items()}
    in_maps = []
    for c in range(8):
        b, g = c // 2, c % 2
        own = QA if g == 0 else QB
        oth = QB if g == 0 else QA
        order = []
        for i in range(NS):
            order += [own[i], oth[i]]
        xb = x[b].reshape(NB, 128, D)[order]
        pb = pos[b].reshape(NB, 128)[order].reshape(-1)
        consts, small = _host_consts(g)
        small = small.copy()
        small[:, 16:32] = _pc(f("ln_in_g"), 16)
        small[:, 32:48] = _pc(f("ln_in_b"), 16)
        small[:, 48:52] = _pc(f("q_a_norm")[0], 4)
        small[:, 52:54] = _pc(f("kv_a_norm")[0], 2)
        m = dict(shared)
        m.update(consts)
        m["c_small"] = small
        m["xp"] = np.ascontiguousarray(xb)
        m["posb"] = np.ascontiguousarray(pb.astype(np.int32))
        in_maps.append(m)
    return in_maps


def _assemble(results):
    out = np.zeros((4, S, D), np.float32)
    for c in range(8):
        b, g = c // 2, c % 2
        own = QA if g == 0 else QB
        o = np.asarray(results[c]["out"], np.float32)
        for j in range(NS):
            out[b, own[j] * 128:(own[j] + 1) * 128, :] = o[j]
    return out


def kernel(**inputs):
    from contextlib import ExitStack
    in_maps = _prepare(inputs)
    nc = bass.Bass("TRN2", target_bir_lowering=False)
    with ExitStack() as stack:
        build(nc, stack)
    res = run_bass_kernel_spmd(nc, in_maps, core_ids=list(range(8)))
    return _assemble(res.results)
```
